# Optimizing a Trainium2 kernel written in Bass

```python
import math
import jax, jax.numpy as jnp
from jax import lax
import numpy as np

D_MODEL = 1024
BATCH = 16
SEQ = 2048
DEPTH = 2

GRID_W = 64
CTX_LEN = 256
EPS = 1e-6

HEAD_DIM = 64
NA_WIDTH = D_MODEL // 2
NA_HEADS = NA_WIDTH // HEAD_DIM
NA_WIN_H = 8
NA_WIN_W = 16
S5_WIDTH = D_MODEL // 4
S5_GROUP = 16
S5_GROUPS = S5_WIDTH // S5_GROUP
S5_STATE = 64
S5_DT_MIN = 0.001
S5_DT_MAX = 0.1
FNET_WIDTH = D_MODEL // 4
FNET_HEAD_DIM = 64
FNET_HEADS = FNET_WIDTH // FNET_HEAD_DIM
MIX_WIDTH = NA_WIDTH + S5_WIDTH + FNET_WIDTH
IN_COLS = 3 * NA_WIDTH + S5_WIDTH + FNET_WIDTH
IN_SPLITS = (NA_WIDTH, 2 * NA_WIDTH, 3 * NA_WIDTH, 3 * NA_WIDTH + S5_WIDTH)
ATTN_SCALE = HEAD_DIM ** -0.5
ROPE_BASE = 100.0
ROPE_PAIRS_PER_AXIS = HEAD_DIM // 4

N_EXPERTS = 16
N_EXPERT_GROUPS = 4
EXPERTS_PER_GROUP = N_EXPERTS // N_EXPERT_GROUPS
TOP_K = 2
EXPERT_FF = 512

kernel_name = "hybrid_na_s5_fnet_moe_dit"


def rms_norm(x, g):
    xf = x.astype(jnp.float32)
    y = xf * lax.rsqrt(jnp.mean(xf * xf, axis=-1, keepdims=True) + EPS)
    return (y * g.astype(jnp.float32)).astype(x.dtype)


def axial_rope(n_tokens):
    t = jnp.arange(n_tokens, dtype=jnp.int32)
    row = (t // GRID_W).astype(jnp.float32)
    col = (t % GRID_W).astype(jnp.float32)
    inv_freq = ROPE_BASE ** (-jnp.arange(ROPE_PAIRS_PER_AXIS, dtype=jnp.float32) / ROPE_PAIRS_PER_AXIS)
    ang = jnp.concatenate([row[:, None] * inv_freq, col[:, None] * inv_freq], axis=-1)
    return jnp.cos(ang), jnp.sin(ang)


def apply_rope(x, cos, sin):
    half = HEAD_DIM // 2
    x1, x2 = x[..., :half], x[..., half:]
    cs = cos[:, None, :].astype(x.dtype)
    sn = sin[:, None, :].astype(x.dtype)
    return jnp.concatenate([x1 * cs - x2 * sn, x1 * sn + x2 * cs], axis=-1)


def neighbourhood_attention(q, k, v, k_ctx, v_ctx, rpb):
    B, L, H, dh = q.shape
    rows = L // GRID_W
    kh = min(NA_WIN_H, rows)
    kw = NA_WIN_W
    qg = jnp.moveaxis(q.reshape(B, rows, GRID_W, H, dh), 1, 0)
    kg = k.reshape(B, rows, GRID_W, H, dh)
    vg = v.reshape(B, rows, GRID_W, H, dh)
    col = jnp.arange(GRID_W, dtype=jnp.int32)
    col_start = jnp.clip(col - kw // 2, 0, GRID_W - kw)
    col_idx = col_start[:, None] + jnp.arange(kw, dtype=jnp.int32)[None, :]
    col_off = col_idx - col[:, None] + (NA_WIN_W - 1)

    def row_block(args):
        r, q_r = args
        row_start = jnp.clip(r - kh // 2, 0, rows - kh)
        k_rows = lax.dynamic_slice_in_dim(kg, row_start, kh, axis=1)
        v_rows = lax.dynamic_slice_in_dim(vg, row_start, kh, axis=1)
        k_win = k_rows[:, :, col_idx]
        v_win = v_rows[:, :, col_idx]
        row_off = row_start + jnp.arange(kh, dtype=jnp.int32) - r + (NA_WIN_H - 1)
        bias = rpb[:, row_off][:, :, col_off]
        bias = jnp.transpose(bias, (0, 2, 1, 3)).reshape(H, GRID_W, kh * kw)
        s_loc = jnp.einsum('bchd,bicjhd->bhcij', q_r, k_win).reshape(B, H, GRID_W, kh * kw)
        s_loc = s_loc.astype(jnp.float32) * ATTN_SCALE + bias.astype(jnp.float32)
        s_ctx = jnp.einsum('bchd,bjhd->bhcj', q_r, k_ctx).astype(jnp.float32) * ATTN_SCALE
        p = jax.nn.softmax(jnp.concatenate([s_loc, s_ctx], axis=-1), axis=-1).astype(v.dtype)
        p_loc = p[..., :kh * kw].reshape(B, H, GRID_W, kh, kw)
        p_ctx = p[..., kh * kw:]
        return (jnp.einsum('bhcij,bicjhd->bchd', p_loc, v_win)
                + jnp.einsum('bhcj,bjhd->bchd', p_ctx, v_ctx))

    out = lax.map(row_block, (jnp.arange(rows, dtype=jnp.int32), qg))
    return jnp.moveaxis(out, 0, 1).reshape(B, L, H, dh)


def context_attention(qc, kc, vc):
    s = jnp.einsum('bqhd,bkhd->bhqk', qc, kc).astype(jnp.float32) * ATTN_SCALE
    p = jax.nn.softmax(s, axis=-1).astype(vc.dtype)
    return jnp.einsum('bhqk,bkhd->bqhd', p, vc)


def s5_discretise(lam_re, lam_im, log_step, b_re, b_im):
    lam = lax.complex(lam_re.astype(jnp.float32), lam_im.astype(jnp.float32))
    step = jnp.exp(log_step.astype(jnp.float32))[:, None]
    lam_bar = jnp.exp(lam * step)
    b = lax.complex(b_re.astype(jnp.float32), b_im.astype(jnp.float32))
    b_bar = ((lam_bar - 1.0) / lam)[..., None] * b
    return lam_bar, b_bar


def _linear_recurrence(left, right):
    a_l, b_l = left
    a_r, b_r = right
    return a_l * a_r, a_r * b_l + b_r


def s5_scan(u, lam_bar, b_bar, h0, reverse):
    bu = jnp.einsum('blgc,gpc->blgp', u.astype(jnp.complex64), b_bar)
    if reverse:
        bu = jnp.flip(bu, axis=1)
    if h0 is not None:
        bu = bu.at[:, 0].add(lam_bar * h0)
    a = jnp.broadcast_to(lam_bar, (1, bu.shape[1]) + lam_bar.shape)
    _, h = lax.associative_scan(_linear_recurrence, (a, bu), axis=1)
    return jnp.flip(h, axis=1) if reverse else h


def s5_glu(y, w_glu, b_glu):
    g = jax.nn.gelu(y)
    return g * jax.nn.sigmoid(g @ w_glu + b_glu)


def s5_mixer(u, u_ctx, lam_re, lam_im, log_step, b_re, b_im, c_re, c_im, d_skip, w_glu, b_glu, ctx_out):
    B, L, _ = u.shape
    Lc = u_ctx.shape[1]
    ug = u.astype(jnp.float32).reshape(B, L, S5_GROUPS, S5_GROUP)
    ucg = u_ctx.astype(jnp.float32).reshape(B, Lc, S5_GROUPS, S5_GROUP)
    d = d_skip.astype(jnp.float32).reshape(S5_GROUPS, S5_GROUP)
    y = d * ug
    yc = d * ucg if ctx_out else None
    for direction in range(2):
        rev = direction == 1
        lam_bar, b_bar = s5_discretise(lam_re[direction], lam_im[direction], log_step[direction],
                                       b_re[direction], b_im[direction])
        c_mat = lax.complex(c_re[direction].astype(jnp.float32), c_im[direction].astype(jnp.float32))
        h_ctx = s5_scan(ucg, lam_bar, b_bar, None, rev)
        h0 = h_ctx[:, 0] if rev else h_ctx[:, -1]
        h = s5_scan(ug, lam_bar, b_bar, h0, rev)
        y = y + jnp.einsum('blgp,gcp->blgc', h, c_mat).real
        if ctx_out:
            yc = yc + jnp.einsum('blgp,gcp->blgc', h_ctx, c_mat).real
    out = s5_glu(y.reshape(B, L, S5_WIDTH).astype(u.dtype), w_glu, b_glu)
    if not ctx_out:
        return out, None
    out_c = s5_glu(yc.reshape(B, Lc, S5_WIDTH).astype(u.dtype), w_glu, b_glu)
    return out, out_c


def fourier_mixer(u, w_fnet):
    f = jnp.fft.fft2(u.astype(jnp.float32), axes=(1, 3), norm='ortho').real.astype(u.dtype)
    return jnp.einsum('blhd,hde->blhe', f, w_fnet)


def token_mixers(h, hc, rope_cos, rope_sin, w_in, q_norm_g, k_norm_g, rpb,
                 lam_re, lam_im, log_step, b_re, b_im, c_re, c_im, d_skip,
                 w_glu, b_glu, w_fnet, w_out, ctx_out):
    B, L, _ = h.shape
    Lc = hc.shape[1]
    q, k, v, u_s5, u_f = jnp.split(h @ w_in, IN_SPLITS, axis=-1)
    q = apply_rope(rms_norm(q.reshape(B, L, NA_HEADS, HEAD_DIM), q_norm_g), rope_cos, rope_sin)
    k = apply_rope(rms_norm(k.reshape(B, L, NA_HEADS, HEAD_DIM), k_norm_g), rope_cos, rope_sin)
    v = v.reshape(B, L, NA_HEADS, HEAD_DIM)
    if ctx_out:
        qc, kc, vc, uc_s5, uc_f = jnp.split(hc @ w_in, IN_SPLITS, axis=-1)
    else:
        kc, vc, uc_s5 = jnp.split(hc @ w_in[:, NA_WIDTH:3 * NA_WIDTH + S5_WIDTH],
                                  (NA_WIDTH, 2 * NA_WIDTH), axis=-1)
    kc = rms_norm(kc.reshape(B, Lc, NA_HEADS, HEAD_DIM), k_norm_g)
    vc = vc.reshape(B, Lc, NA_HEADS, HEAD_DIM)

    att = neighbourhood_attention(q, k, v, kc, vc, rpb).reshape(B, L, NA_WIDTH)
    ssm, ssm_c = s5_mixer(u_s5, uc_s5, lam_re, lam_im, log_step, b_re, b_im, c_re, c_im, d_skip,
                          w_glu, b_glu, ctx_out)
    fou = fourier_mixer(u_f.reshape(B, L, FNET_HEADS, FNET_HEAD_DIM), w_fnet).reshape(B, L, FNET_WIDTH)
    out = jnp.concatenate([att, ssm, fou], axis=-1) @ w_out
    if not ctx_out:
        return out, None
    qc = rms_norm(qc.reshape(B, Lc, NA_HEADS, HEAD_DIM), q_norm_g)
    att_c = context_attention(qc, kc, vc).reshape(B, Lc, NA_WIDTH)
    fou_c = fourier_mixer(uc_f.reshape(B, Lc, FNET_HEADS, FNET_HEAD_DIM), w_fnet).reshape(B, Lc, FNET_WIDTH)
    out_c = jnp.concatenate([att_c, ssm_c, fou_c], axis=-1) @ w_out
    return out, out_c


def moe_ffn(t, w_router, b_router, w_gate, w_up, w_down):
    n_tok = t.shape[0]
    affinity = jax.nn.sigmoid((t @ w_router).astype(jnp.float32))
    select = (affinity + b_router.astype(jnp.float32)).reshape(n_tok, N_EXPERT_GROUPS, EXPERTS_PER_GROUP)
    group_score = jnp.sum(lax.top_k(select, TOP_K)[0], axis=-1)
    g_idx = jnp.argmax(group_score, axis=-1)
    in_group = jnp.take_along_axis(select, g_idx[:, None, None], axis=1)[:, 0]
    e_idx = g_idx[:, None] * EXPERTS_PER_GROUP + lax.top_k(in_group, TOP_K)[1]
    w = jnp.take_along_axis(affinity, e_idx, axis=-1)
    w = w / jnp.sum(w, axis=-1, keepdims=True)
    gates = jnp.sum(jax.nn.one_hot(e_idx, N_EXPERTS, dtype=jnp.float32) * w[..., None], axis=1).astype(t.dtype)
    y = jnp.zeros_like(t)
    for e in range(N_EXPERTS):
        he = jax.nn.silu(t @ w_gate[e]) * (t @ w_up[e])
        y = y + gates[:, e:e + 1] * (he @ w_down[e])
    return y


def setup_inputs(seed: int = 0) -> dict:
    key = jax.random.key(seed)
    ks = jax.random.split(key, 32)
    f32 = jnp.float32
    D = D_MODEL

    def nrm(k, shape, scale):
        return jax.random.normal(k, shape, f32) * scale

    s5_shape = (DEPTH, 2, S5_GROUPS, S5_STATE)
    return {
        "x": nrm(ks[0], (BATCH, SEQ, D), 1.0),
        "c": nrm(ks[1], (BATCH, D), 1.0),
        "ctx": nrm(ks[2], (BATCH, CTX_LEN, D), 1.0),
        "c_ctx": nrm(ks[3], (D,), 1.0),
        "w_ada": nrm(ks[4], (DEPTH, D, 6 * D), 0.5 * D ** -0.5),
        "b_ada": nrm(ks[5], (DEPTH, 6 * D), 0.01),
        "norm1_g": 1.0 + nrm(ks[6], (DEPTH, D), 0.01),
        "norm2_g": 1.0 + nrm(ks[7], (DEPTH, D), 0.01),
        "w_in": nrm(ks[8], (DEPTH, D, IN_COLS), D ** -0.5),
        "q_norm_g": 1.0 + nrm(ks[9], (DEPTH, HEAD_DIM), 0.01),
        "k_norm_g": 1.0 + nrm(ks[10], (DEPTH, HEAD_DIM), 0.01),
        "rpb": nrm(ks[11], (DEPTH, NA_HEADS, 2 * NA_WIN_H - 1, 2 * NA_WIN_W - 1), 0.02),
        "s5_lam_re": -0.5 + nrm(ks[12], s5_shape, 0.01),
        "s5_lam_im": math.pi * jnp.arange(S5_STATE, dtype=f32) + nrm(ks[13], s5_shape, 0.01),
        "s5_log_step": jax.random.uniform(ks[14], (DEPTH, 2, S5_GROUPS), f32,
                                          math.log(S5_DT_MIN), math.log(S5_DT_MAX)),
        "s5_b_re": nrm(ks[15], s5_shape + (S5_GROUP,), (2 * S5_GROUP) ** -0.5),
        "s5_b_im": nrm(ks[16], s5_shape + (S5_GROUP,), (2 * S5_GROUP) ** -0.5),
        "s5_c_re": nrm(ks[17], (DEPTH, 2, S5_GROUPS, S5_GROUP, S5_STATE), (2 * S5_STATE) ** -0.5),
        "s5_c_im": nrm(ks[18], (DEPTH, 2, S5_GROUPS, S5_GROUP, S5_STATE), (2 * S5_STATE) ** -0.5),
        "s5_d": nrm(ks[19], (DEPTH, S5_WIDTH), 1.0),
        "w_glu": nrm(ks[20], (DEPTH, S5_WIDTH, S5_WIDTH), S5_WIDTH ** -0.5),
        "b_glu": nrm(ks[21], (DEPTH, S5_WIDTH), 0.01),
        "w_fnet": nrm(ks[22], (DEPTH, FNET_HEADS, FNET_HEAD_DIM, FNET_HEAD_DIM), FNET_HEAD_DIM ** -0.5),
        "w_out": nrm(ks[23], (DEPTH, MIX_WIDTH, D), MIX_WIDTH ** -0.5),
        "w_router": nrm(ks[24], (D, N_EXPERTS), D ** -0.5),
        "b_router": nrm(ks[25], (N_EXPERTS,), 0.01),
        "w_gate": nrm(ks[26], (DEPTH, N_EXPERTS, D, EXPERT_FF), D ** -0.5),
        "w_up": nrm(ks[27], (DEPTH, N_EXPERTS, D, EXPERT_FF), D ** -0.5),
        "w_down": nrm(ks[28], (DEPTH, N_EXPERTS, EXPERT_FF, D), EXPERT_FF ** -0.5),
    }


def reference(x, c, ctx, c_ctx, w_ada, b_ada, norm1_g, norm2_g, w_in, q_norm_g, k_norm_g, rpb,
              s5_lam_re, s5_lam_im, s5_log_step, s5_b_re, s5_b_im, s5_c_re, s5_c_im, s5_d,
              w_glu, b_glu, w_fnet, w_out, w_router, b_router, w_gate, w_up, w_down):
    B, L, D = x.shape
    rope_cos, rope_sin = axial_rope(L)
    silu_c = jax.nn.silu(c)
    silu_cc = jax.nn.silu(c_ctx)
    xc = ctx
    for l in range(DEPTH):
        ctx_out = l < DEPTH - 1
        mod = jnp.split((silu_c @ w_ada[l] + b_ada[l])[:, None, :], 6, axis=-1)
        mod_c = jnp.split(silu_cc @ w_ada[l] + b_ada[l], 6, axis=-1)
        h = rms_norm(x, norm1_g[l]) * (1 + mod[1]) + mod[0]
        hc = rms_norm(xc, norm1_g[l]) * (1 + mod_c[1]) + mod_c[0]
        o, oc = token_mixers(h, hc, rope_cos, rope_sin, w_in[l], q_norm_g[l], k_norm_g[l], rpb[l],
                             s5_lam_re[l], s5_lam_im[l], s5_log_step[l], s5_b_re[l], s5_b_im[l],
                             s5_c_re[l], s5_c_im[l], s5_d[l], w_glu[l], b_glu[l], w_fnet[l], w_out[l],
                             ctx_out)
        x = x + mod[2] * o
        h2 = rms_norm(x, norm2_g[l]) * (1 + mod[4]) + mod[3]
        if ctx_out:
            xc = xc + mod_c[2] * oc
            h2c = rms_norm(xc, norm2_g[l]) * (1 + mod_c[4]) + mod_c[3]
            tokens = jnp.concatenate([h2.reshape(-1, D), h2c.reshape(-1, D)], axis=0)
            y = moe_ffn(tokens, w_router, b_router, w_gate[l], w_up[l], w_down[l])
            x = x + mod[5] * y[:B * L].reshape(B, L, D)
            xc = xc + mod_c[5] * y[B * L:].reshape(B, -1, D)
        else:
            y = moe_ffn(h2.reshape(-1, D), w_router, b_router, w_gate[l], w_up[l], w_down[l])
            x = x + mod[5] * y.reshape(B, L, D)
    return x
```

```python
import math
from contextlib import ExitStack
import numpy as np
import ml_dtypes
import concourse.bass as bass
import concourse.mybir as mybir
from concourse.bass_utils import run_bass_kernel_spmd

F32 = mybir.dt.float32
BF16 = mybir.dt.bfloat16
I32 = mybir.dt.int32
AF = mybir.ActivationFunctionType
ALU = mybir.AluOpType
AX = mybir.AxisListType

D = 1024
L = 2048
LC = 256
T = L + LC
NT = T // 128
DEPTH = 2
EPS = 1e-6
NEG = -30000.0
PI = math.pi

ENGS = ("pe", "act", "dve", "pool", "sp")
BF16_INPUTS = ("dftc", "dftns", "dftc_c", "dftns_c")


class Buf:
    __slots__ = ("name", "w", "wx", "r", "dsem", "dcnt")

    def __init__(self, name):
        self.name = name
        self.w = None
        self.wx = []
        self.r = []
        self.dsem = None
        self.dcnt = 0


class Prog:
    def __init__(self, nc):
        self.nc = nc
        self.ops = {e: [] for e in ENGS}
        self.seen = {e: {} for e in ENGS}
        self.marked = {e: set() for e in ENGS}
        self.ndsem = 0
        self.dsem_final = {}
        self.dsem_names = {}

    def _deps(self, eng, reads, writes, pwrites=()):
        deps = []
        for b in reads:
            if b.w is not None:
                deps.append(b.w)
            deps.extend(b.wx)
        for b in writes:
            if b.w is not None:
                t = b.w
                if not (t[0] == "E" and t[1] == eng):
                    deps.append(t)
            for t in list(b.r) + list(b.wx):
                if not (t[0] == "E" and t[1] == eng):
                    deps.append(t)
        for b in pwrites:
            if b.w is not None:
                t = b.w
                if not (t[0] == "E" and t[1] == eng):
                    deps.append(t)
            for t in b.r:
                if not (t[0] == "E" and t[1] == eng):
                    deps.append(t)
        seen = self.seen[eng]
        best = {}
        for t in deps:
            key = (t[0], t[1])
            if seen.get(key, -1) >= t[2]:
                continue
            if best.get(key, -1) < t[2]:
                best[key] = t[2]
        out = []
        for key, v in best.items():
            seen[key] = v
            out.append((key[0], key[1], v))
            if key[0] == "E":
                self.marked[key[1]].add(v)
        return out

    def _commit(self, tok, reads, writes, pwrites=()):
        for b in pwrites:
            b.wx.append(tok)
        for b in reads:
            if len(b.r) > 24:
                last = {}
                for t in b.r:
                    k = (t[0], t[1])
                    if last.get(k, -1) < t[2]:
                        last[k] = t[2]
                b.r = [(k[0], k[1], v) for k, v in last.items()]
            b.r.append(tok)
        for b in writes:
            b.w = tok
            b.wx = []
            b.r = []

    def op(self, eng, fn, reads=(), writes=(), pw=()):
        waits = self._deps(eng, reads, writes, pw)
        seq = len(self.ops[eng])
        tok = ("E", eng, seq)
        self.ops[eng].append((waits, fn, "C", None))
        self._commit(tok, reads, writes, pw)
        return tok

    def dma(self, eng, fn, reads=(), writes=(), semb=None):
        if semb is None:
            semb = writes[0] if writes else reads[0]
        if semb.dsem is None:
            if semb.name not in self.dsem_names:
                self.dsem_names[semb.name] = self.ndsem
                self.ndsem += 1
            semb.dsem = self.dsem_names[semb.name]
        waits = self._deps(eng, reads, writes)
        semb.dcnt = self.dsem_final.get(semb.dsem, 0) + 16
        tok = ("D", semb.dsem, semb.dcnt)
        self.dsem_final[semb.dsem] = semb.dcnt
        self.ops[eng].append((waits, fn, "D", semb.dsem))
        self._commit(tok, reads, writes)
        return tok

    def alias(self, new_bufs, old_bufs):
        toks = []
        for b in old_bufs:
            if b.w is not None:
                toks.append(b.w)
            toks.extend(b.wx)
            toks.extend(b.r)
        last = {}
        for t in toks:
            k = (t[0], t[1])
            if last.get(k, -1) < t[2]:
                last[k] = t[2]
        toks = [(k[0], k[1], v) for k, v in last.items()]
        for nb in new_bufs:
            nb.r = list(nb.r) + toks

    def emit(self):
        nc = self.nc
        with ExitStack() as es:
            esem = {e: es.enter_context(nc.semaphore("sem_" + e)) for e in ENGS}
            dsem = [es.enter_context(nc.semaphore("dsem%d" % i)) for i in range(self.ndsem)]
            block = es.enter_context(nc.Block())
            mcount = {}
            for e in ENGS:
                ms = sorted(self.marked[e])
                mcount[e] = {s: i + 1 for i, s in enumerate(ms)}

            def run(e, engobj):
                mk = self.marked[e]
                for seq, (waits, fn, kind, extra) in enumerate(self.ops[e]):
                    for (k, a, v) in waits:
                        if k == "E":
                            engobj.wait_ge(esem[a], mcount[a][v])
                        else:
                            engobj.wait_ge(dsem[a], v)
                    ins = fn(engobj)
                    if kind == "D":
                        ins.then_inc(dsem[extra], 16)
                    elif seq in mk:
                        ins.then_inc(esem[e], 1)
                if e == "sp":
                    for i, cnt in self.dsem_final.items():
                        engobj.wait_ge(dsem[i], cnt)

            @block.tensor
            def _(eng):
                run("pe", eng)

            @block.scalar
            def _(eng):
                run("act", eng)

            @block.vector
            def _(eng):
                run("dve", eng)

            @block.gpsimd
            def _(eng):
                run("pool", eng)

            @block.sync
            def _(eng):
                run("sp", eng)


def _row_start(r):
    return min(max(r - 4, 0), 24)


def _col_start(c):
    return min(max(c - 8, 0), 48)


BIAS_TILES = [(5, 3), (5, 4), (5, 5), (5, 6), (5, 7)] + [(0, k) for k in range(4)] + [(1, k) for k in range(4)] \
    + [(14, k) for k in range(12, 16)] + [(15, k) for k in range(12, 16)]


def key_tiles(j):
    if j == 0:
        return [0, 1, 2, 3], 5
    if j == 1:
        return [0, 1, 2, 3], 9
    if j == 14:
        return [12, 13, 14, 15], 13
    if j == 15:
        return [12, 13, 14, 15], 17
    return [j - 2, j - 1, j, j + 1, j + 2], 0


def _bias_index():
    idx = np.full((21, 128, 128), 15 * 31, dtype=np.int64)
    for t, (j, jk) in enumerate(BIAS_TILES):
        for qr in range(2):
            r = 2 * j + qr
            rs = _row_start(r)
            for kr in range(2):
                r2 = 2 * jk + kr
                if not (rs <= r2 < rs + 8):
                    continue
                for c in range(64):
                    cs = _col_start(c)
                    c2 = np.arange(cs, cs + 16)
                    idx[t, kr * 64 + c2, qr * 64 + c] = (r2 - r + 7) * 31 + (c2 - c + 15)
    return idx


_CONST_CACHE = {}


def _constants():
    if _CONST_CACHE:
        return _CONST_CACHE
    c = {}
    c["ident"] = np.eye(128, dtype=np.float32)
    c["iota"] = np.tile(np.arange(128, dtype=np.float32)[None, :], (128, 1))
    t = np.arange(L)
    row = (t // 64).astype(np.float32)
    col = (t % 64).astype(np.float32)
    inv = (100.0 ** (-np.arange(16, dtype=np.float32) / 16)).astype(np.float32)
    ang = np.concatenate([row[:, None] * inv, col[:, None] * inv], axis=-1).astype(np.float32)
    c["ropec"] = np.ascontiguousarray(np.cos(ang).astype(np.float32).reshape(16, 128, 32).transpose(1, 0, 2))
    c["ropes"] = np.ascontiguousarray(np.sin(ang).astype(np.float32).reshape(16, 128, 32).transpose(1, 0, 2))
    def dft(n, scale):
        k = np.arange(n, dtype=np.int64)
        m = (k[:, None] * k[None, :]) % n
        a = 2.0 * np.pi * m.astype(np.float64) / n
        return (np.cos(a) * scale).astype(np.float32), (-np.sin(a) * scale).astype(np.float32)
    c["dftc"], c["dftns"] = dft(L, 1.0 / math.sqrt(L * 64))
    c["dftc_c"], c["dftns_c"] = dft(LC, 1.0 / math.sqrt(LC * 64))
    for k_ in BF16_INPUTS:
        c[k_] = np.ascontiguousarray(c[k_].astype(ml_dtypes.bfloat16))
    c64, ns64 = dft(64, 1.0)
    z = np.zeros((128, 128), np.float32)
    z[:64, :64] = c64
    z[64:, 64:] = c64
    c["c64blk"] = z.copy()
    z[:64, :64] = -ns64
    z[64:, 64:] = -ns64
    c["s64blk"] = z.copy()
    sel = np.zeros((16, 16, 128), np.float32)
    for e in range(16):
        sel[e, e, :] = 1.0
    c["sel"] = sel.reshape(16, 16 * 128)
    c["bias_idx"] = _bias_index()
    _CONST_CACHE.update(c)
    return c


def _prep_shared(inp):
    cst = _constants()
    f = lambda a: np.ascontiguousarray(np.asarray(a, dtype=np.float32))
    m = {}
    for k in ("ident", "iota", "ropec", "ropes", "dftc", "dftns", "dftc_c", "dftns_c", "c64blk", "s64blk", "sel"):
        m[k] = cst[k]
    m["w_ada"] = f(inp["w_ada"])
    m["b_adaT"] = f(np.asarray(inp["b_ada"]).reshape(DEPTH, 48, 128).transpose(0, 2, 1))
    m["n1gT"] = f(np.asarray(inp["norm1_g"]).reshape(DEPTH, 8, 128).transpose(0, 2, 1))
    m["n2gT"] = f(np.asarray(inp["norm2_g"]).reshape(DEPTH, 8, 128).transpose(0, 2, 1))
    m["w_in"] = f(inp["w_in"])
    m["w_out"] = f(inp["w_out"])
    m["qng"] = f(inp["q_norm_g"])
    m["kng"] = f(inp["k_norm_g"])
    rpb = np.asarray(inp["rpb"], dtype=np.float32).reshape(DEPTH, 8, 15 * 31)
    rpbp = np.concatenate([rpb, np.full((DEPTH, 8, 1), NEG, np.float32)], axis=-1)
    bt = rpbp[:, :, cst["bias_idx"]]
    m["bt"] = f(bt.transpose(0, 1, 3, 2, 4))
    lre = np.asarray(inp["s5_lam_re"], np.float32)
    lim = np.asarray(inp["s5_lam_im"], np.float32)
    lst = np.asarray(inp["s5_log_step"], np.float32)
    sm = lambda a: a.reshape(DEPTH, 2, 8, 2, 64).transpose(0, 1, 3, 4, 2).reshape(DEPTH, 2, 128, 8)
    lsr = np.repeat(lst[..., None], 64, axis=-1)
    m["s5_sm"] = f(np.stack([sm(lre), sm(lim), sm(lsr)], axis=3))
    m["s5_bc"] = f(np.stack([lre.reshape(DEPTH, 2, 1024), lim.reshape(DEPTH, 2, 1024),
                             lsr.reshape(DEPTH, 2, 1024)], axis=2))
    bre = np.asarray(inp["s5_b_re"], np.float32)
    bim = np.asarray(inp["s5_b_im"], np.float32)
    cre = np.asarray(inp["s5_c_re"], np.float32)
    cim = np.asarray(inp["s5_c_im"], np.float32)
    bz = np.zeros((DEPTH, 2, 2, 128, 8, 128), np.float32)
    cz = np.zeros((DEPTH, 2, 2, 128, 8, 128), np.float32)
    for g in range(16):
        st, two, ch0 = g // 2, g % 2, (g % 8) * 16
        bz[:, :, 0, ch0:ch0 + 16, st, two * 64:(two + 1) * 64] = bre[:, :, g].transpose(0, 1, 3, 2)
        bz[:, :, 1, ch0:ch0 + 16, st, two * 64:(two + 1) * 64] = bim[:, :, g].transpose(0, 1, 3, 2)
        cz[:, :, 0, two * 64:(two + 1) * 64, st, ch0:ch0 + 16] = cre[:, :, g].transpose(0, 1, 3, 2)
        cz[:, :, 1, two * 64:(two + 1) * 64, st, ch0:ch0 + 16] = cim[:, :, g].transpose(0, 1, 3, 2)
    m["s5_bz"] = bz
    m["s5_cz"] = cz
    m["s5_dT"] = f(np.asarray(inp["s5_d"]).reshape(DEPTH, 2, 128).transpose(0, 2, 1))
    m["w_glu"] = f(inp["w_glu"])
    m["b_gluT"] = f(np.asarray(inp["b_glu"]).reshape(DEPTH, 2, 128).transpose(0, 2, 1))
    wf = np.asarray(inp["w_fnet"], np.float32)
    wfz = np.zeros((DEPTH, 2, 128, 256), np.float32)
    for h in range(4):
        kt, hh = h // 2, h % 2
        wfz[:, kt, hh * 64:(hh + 1) * 64, h * 64:(h + 1) * 64] = wf[:, h]
    m["w_fz"] = wfz
    m["w_router"] = f(np.asarray(inp["w_router"]).reshape(8, 128, 16).transpose(1, 0, 2))
    m["b_router"] = f(inp["b_router"])
    m["w_gate"] = f(inp["w_gate"])
    m["w_up"] = f(inp["w_up"])
    m["w_down"] = f(inp["w_down"])
    return m


GROUPS = [(g * 512, 512) for g in range(4)] + [(2048, 256)]


def build_program(shapes, n_batch=2, layers=(0, 1), dbg=None, stop_after=None):
    nc = bass.Bass("TRN2", target_bir_lowering=False)
    P = Prog(nc)
    dr = {}
    for name, shp in shapes.items():
        dr[name] = nc.dram_tensor(name, list(shp), BF16 if name in BF16_INPUTS else F32, kind="ExternalInput").ap()
    out_d = nc.dram_tensor("out", [n_batch, L, D], F32, kind="ExternalOutput").ap()
    dbg_d = {}
    if dbg:
        for name, (shp, dt) in dbg.items():
            dbg_d[name] = nc.dram_tensor("dbg_" + name, list(shp), dt, kind="ExternalOutput").ap()

    class Stop(Exception):
        pass

    es = ExitStack()
    with es:
        ARENA_BYTES = 212000
        arena_t = es.enter_context(nc.sbuf_tensor("arena", [128, ARENA_BYTES // 4], F32))
        ps_t = [es.enter_context(nc.psum_tensor("ps%d" % i, [128, 512], F32)) for i in range(8)]
        PS = [t[:, :] for t in ps_t]
        PSH = [t[:, :].bitcast(BF16) for t in ps_t]
        PSb = [Buf("ps%d" % i) for i in range(8)]

        def view(off, shape, dt):
            n = int(np.prod(shape))
            assert off % 4 == 0
            if dt == F32:
                v = arena_t[:, off // 4: off // 4 + n]
                nb = n * 4
            else:
                assert n % 2 == 0
                v = arena_t[:, off // 4: off // 4 + n // 2].bitcast(BF16)
                nb = n * 2
            if len(shape) == 2:
                v = v.rearrange("p (a b) -> p a b", a=shape[0])
            elif len(shape) == 3:
                v = v.rearrange("p (a b c) -> p a b c", a=shape[0], b=shape[1])
            return v, nb

        class Alloc:
            def __init__(self, base, limit):
                self.off = base
                self.limit = limit

            def take(self, shape, dt):
                v, nb = view(self.off, shape, dt)
                self.off += (nb + 3) // 4 * 4
                assert self.off <= self.limit, (self.off, self.limit)
                return v

        pers = Alloc(0, ARENA_BYTES)
        X = pers.take([NT, D], F32)
        Xb = [Buf("x%d" % i) for i in range(NT)]
        HT_OFF = pers.off
        HT = pers.take([8, T], BF16)
        HT_END = pers.off
        HTb = [Buf("ht%d" % i) for i in range(NT)]
        ident = pers.take([128], F32); b_ident = Buf("ident")
        identb = pers.take([128], BF16); b_identb = Buf("identb")
        ones_f = pers.take([128], F32); b_ones = Buf("ones")
        ones_b = pers.take([128], BF16)
        iota = pers.take([128], F32); b_iota = Buf("iota")
        ropec = pers.take([16, 32], F32); ropes = pers.take([16, 32], F32); b_rope = Buf("rope")
        modT = pers.take([DEPTH * 48, 3], F32); b_modT = Buf("modT")
        n1g = pers.take([DEPTH, 8], F32); n2g = pers.take([DEPTH, 8], F32); b_ng = Buf("ng")
        AB = pers.take([2, 2, 8], F32); b_AB = Buf("AB")
        ssq = pers.take([NT], F32); b_ssq = Buf("ssq")
        rstd = pers.take([NT], F32); b_rstd = Buf("rstd")
        ccol = pers.take([4], F32); b_ccol = Buf("ccol")
        GB = pers.take([2, D], F32); b_GB = Buf("GB")
        diag = pers.take([128], F32); b_diag = Buf("diag")
        PH_BASE = pers.off
        PH_LIMIT = ARENA_BYTES
        state = dict(old=[], cur=[])

        def new_phase():
            state["old"] = state["old"] + state["cur"]
            summ = Buf("summ")
            P.alias([summ], state["old"])
            state["old"] = [summ]
            state["cur"] = []
            state["extra"] = []
            return Alloc(PH_BASE, PH_LIMIT)

        def pbuf(name, extra_old=()):
            b = Buf(name)
            P.alias([b], state["old"] + list(extra_old) + list(state.get("extra", [])))
            state["cur"].append(b)
            return b

        def mm(out, lhsT, rhs, start, stop, reads, writes):
            P.op("pe", lambda e: e.matmul(out=out, lhsT=lhsT, rhs=rhs, start=start, stop=stop), reads, writes)

        def tr(out, in_, idn, reads, writes):
            P.op("pe", lambda e: e.transpose(out=out, in_=in_, identity=idn), reads, writes)

        def act(out, in_, func, reads, writes, bias=None, scale=None, accum=None, pw=()):
            kw = {}
            if bias is not None:
                kw["bias"] = bias
            if scale is not None:
                kw["scale"] = scale
            if accum is not None:
                kw["accum_out"] = accum
            P.op("act", lambda e: e.activation(out=out, in_=in_, func=func, **kw), reads, writes, pw)

        def tt(eng, out, in0, in1, op, reads, writes):
            P.op(eng, lambda e: e.tensor_tensor(out=out, in0=in0, in1=in1, op=op), reads, writes)

        def ts(eng, out, in0, s1, op0, reads, writes, s2=None, op1=None, pw=()):
            if op1 is None:
                P.op(eng, lambda e: e.tensor_scalar(out=out, in0=in0, scalar1=s1, scalar2=None, op0=op0), reads, writes, pw)
            else:
                P.op(eng, lambda e: e.tensor_scalar(out=out, in0=in0, scalar1=s1, scalar2=s2, op0=op0, op1=op1), reads, writes, pw)

        def stt(eng, out, in0, scalar, in1, op0, op1, reads, writes):
            P.op(eng, lambda e: e.scalar_tensor_tensor(out=out, in0=in0, scalar=scalar, in1=in1, op0=op0, op1=op1), reads, writes)

        def red(out, in_, op, reads, writes):
            P.op("dve", lambda e: e.tensor_reduce(out=out, in_=in_, axis=AX.X, op=op), reads, writes)

        def recip(out, in_, reads, writes):
            P.op("dve", lambda e: e.reciprocal(out=out, in_=in_), reads, writes)

        def cp(eng, out, in_, reads, writes, pw=()):
            if eng == "act":
                act(out, in_, AF.Copy, reads, writes, pw=pw)
            else:
                P.op(eng, lambda e: e.tensor_copy(out=out, in_=in_), reads, writes, pw)

        def memset(eng, ap, val, writes):
            P.op(eng, lambda e: e.memset(ap, val), (), writes)

        def dma(q, out, in_, reads, writes, semb=None):
            P.dma(q, lambda e: e.dma_start(out=out, in_=in_), reads, writes, semb)

        def dump(name, src, reads):
            if name in dbg_d:
                dma("sp", dbg_d[name], src, reads, [], semb=reads[0])

        def flat(v):
            return v.rearrange("p a b -> p (a b)")

        def wview(ap2d):
            return ap2d.rearrange("(k p) n -> p k n", p=128)

        dma("sp", ident, dr["ident"][:, :], [], [b_ident])
        dma("sp", iota, dr["iota"][:, :], [], [b_iota])
        dma("sp", ropec, dr["ropec"][:, :, :], [], [b_rope])
        dma("sp", ropes, dr["ropes"][:, :, :], [], [b_rope])
        dma("sp", n1g, dr["n1gT"].rearrange("l p c -> p l c"), [], [b_ng])
        dma("sp", n2g, dr["n2gT"].rearrange("l p c -> p l c"), [], [b_ng])
        cp("dve", identb, ident, [b_ident], [b_identb])
        memset("dve", ones_f, 1.0, [b_ones])
        memset("dve", ones_b, 1.0, [b_ones])
        memset("dve", ccol[:, 0:1], EPS, [b_ccol])
        memset("dve", ccol[:, 1:2], -PI, [b_ccol])
        eps_col = ccol[:, 0:1]
        negpi_col = ccol[:, 1:2]

        ph = new_phase()
        cT = ph.take([8, 3], F32); b_cT = pbuf("cT")
        sT = ph.take([8, 3], BF16); b_sT = pbuf("sT")
        badaT = ph.take([DEPTH, 48], F32); b_bada = pbuf("bada")
        wch = [ph.take([8, 512], BF16) for _ in range(2)]
        b_wch = [pbuf("wch%d" % i) for i in range(2)]
        dma("sp", cT, dr["crowT"][:, :, :], [], [b_cT])
        dma("sp", badaT, dr["b_adaT"].rearrange("l p c -> p l c"), [], [b_bada])
        act(sT, cT, AF.Silu, [b_cT], [b_sT])
        for l in range(DEPTH):
            wv = wview(dr["w_ada"][l])
            for cc in range(12):
                s = (l * 12 + cc) % 2
                dma("pool", wch[s], wv[:, :, cc * 512:(cc + 1) * 512], [], [b_wch[s]])
                for j in range(4):
                    col = (cc * 4 + j) * 3
                    for k in range(8):
                        mm(PS[0][:, col:col + 3], wch[s][:, k, j * 128:(j + 1) * 128], sT[:, k, :], k == 0, k == 7,
                           [b_wch[s], b_sT], [PSb[0]])
            tt("dve", modT[:, l * 48:(l + 1) * 48, :], PS[0][:, 0:144].rearrange("p (c r) -> p c r", r=3),
               badaT[:, l, :].unsqueeze(2).to_broadcast([128, 48, 3]), ALU.add, [PSb[0], b_bada], [b_modT])
        dump("modT", modT, [b_modT])

        s5_scr = {}

        def s5_scratch(l, d):
            if (l, d) not in s5_scr:
                a = nc.dram_tensor("s5scrA_%d_%d" % (l, d), [128, 6144], BF16, kind="Internal").ap()
                b = nc.dram_tensor("s5scrB_%d_%d" % (l, d), [128, 1056], F32, kind="Internal").ap()
                s5_scr[(l, d)] = dict(A=a, B=b, bA1=Buf("s5A1"), bA2=Buf("s5A2"), bB1=Buf("s5B1"), bB2=Buf("s5B2"))
            return s5_scr[(l, d)]

        def modcol(l, vec, chunk, row):
            i = l * 48 + vec * 8 + chunk
            return modT[:, i, row:row + 1]

        try:
            for bi in range(n_batch):
                for i in range(NT):
                    if i < 2:
                        src = dr["ctx"][bi, i * 128:(i + 1) * 128, :]
                    else:
                        src = dr["x"][bi, (i - 2) * 128:(i - 1) * 128, :]
                    dma("sp", X[:, i, :], src, [], [Xb[i]])

                for l in layers:
                    first = (bi == 0 and l == layers[0])

                    def dmp(name, src, reads):
                        if first:
                            dump(name, src, reads)

                    def norm_to_HT(gT, vshift, vscale, ph, router=None):
                        junk = ph.take([D], BF16); b_junk = pbuf("junk")
                        xs = [ph.take([D], F32) for _ in range(2)]
                        b_xs = [pbuf("xs%d" % i) for i in range(2)]
                        for r_i, row in enumerate((bi, 2)):
                            for c in range(8):
                                ts("dve", AB[:, r_i, 0, c:c + 1], modcol(l, vscale, c, row), 1.0, ALU.add,
                                   [b_modT, b_ng], [b_AB], s2=gT[:, l, c:c + 1], op1=ALU.mult)
                                cp("dve", AB[:, r_i, 1, c:c + 1], modcol(l, vshift, c, row), [b_modT], [b_AB])
                        for i in range(NT):
                            act(junk, X[:, i, :], AF.Square, [Xb[i]], [b_junk, b_ssq], accum=ssq[:, i:i + 1])
                        act(rstd, ssq, AF.Sqrt, [b_ssq, b_ccol], [b_rstd], bias=eps_col, scale=1.0 / D)
                        recip(rstd, rstd, [b_rstd], [b_rstd])
                        def mk_xs(i):
                            ts("dve", xs[i % 2], X[:, i, :], rstd[:, i:i + 1], ALU.mult, [Xb[i], b_rstd], [b_xs[i % 2]])

                        mk_xs(0)
                        for i in range(NT):
                            s = i % 2
                            r_i = 1 if i < 2 else 0
                            pb = (i % 2) * 2
                            for c in range(8):
                                bk = pb + c // 4
                                tr(PS[bk][:, (c % 4) * 128:(c % 4 + 1) * 128], xs[s][:, c * 128:(c + 1) * 128], ident,
                                   [b_xs[s], b_ident], [PSb[bk]])
                            if i + 1 < NT:
                                mk_xs(i + 1)
                            if router is not None:
                                router(i, pb, r_i)
                                continue
                            for c in range(8):
                                bk = pb + c // 4
                                src_ = PS[bk][:, (c % 4) * 128:(c % 4 + 1) * 128]
                                dst_ = HT[:, c, i * 128:(i + 1) * 128]
                                wr_, pw_ = ([HTb[i]], ()) if c == 0 else ([], [HTb[i]])
                                if c < 4:
                                    act(dst_, src_, AF.Identity, [PSb[bk], b_AB], wr_,
                                        bias=AB[:, r_i, 1, c:c + 1], scale=AB[:, r_i, 0, c:c + 1], pw=pw_)
                                else:
                                    ts("dve", dst_, src_, AB[:, r_i, 0, c:c + 1], ALU.mult, [PSb[bk], b_AB], wr_,
                                       s2=AB[:, r_i, 1, c:c + 1], op1=ALU.add, pw=pw_)

                    def make_gb(vec):
                        for r_i, row in enumerate((bi, 2)):
                            for c in range(8):
                                ts("dve", diag, ident, modcol(l, vec, c, row), ALU.mult, [b_ident, b_modT], [b_diag])
                                mm(PS[7][:, 0:128], ones_f, diag, True, True, [b_ones, b_diag], [PSb[7]])
                                cp("act", GB[:, r_i, c * 128:(c + 1) * 128], PS[7][:, 0:128], [PSb[7]], [b_GB])

                    def wout_partial(srcT, b_src, nk, wo, b_wo, tmp, b_tmp):
                        for i in range(NT):
                            r_i = 1 if i < 2 else 0
                            for half in range(2):
                                bk = 4 + (i * 2 + half) % 4
                                for kk in range(nk):
                                    mm(PS[bk], srcT[:, kk, i * 128:(i + 1) * 128], wo[:, kk, half * 512:(half + 1) * 512],
                                       kk == 0, kk == nk - 1, [b_src, b_wo], [PSb[bk]])
                                s = (i * 2 + half) % 2
                                tt("dve", tmp[s], PS[bk], GB[:, r_i, half * 512:(half + 1) * 512], ALU.mult,
                                   [PSb[bk], b_GB], [b_tmp[s]])
                                tt("pool", X[:, i, half * 512:(half + 1) * 512], X[:, i, half * 512:(half + 1) * 512], tmp[s],
                                   ALU.add, [b_tmp[s], Xb[i]], [Xb[i]])

                    def proj_fm(dstT, b_dst, wchunk, b_w, ncol_tiles):
                        n = 0
                        for (t0, tn) in GROUPS:
                            tiles = list(range(t0 // 128, (t0 + tn) // 128))
                            for ct in range(ncol_tiles):
                                bk = n % 4
                                n += 1
                                for k in range(8):
                                    mm(PS[bk][:, 0:tn], wchunk[:, k, ct * 128:(ct + 1) * 128], HT[:, k, t0:t0 + tn], k == 0, k == 7,
                                       [b_w] + [HTb[i] for i in tiles], [PSb[bk]])
                                cp("act", dstT[:, ct, t0:t0 + tn], PS[bk][:, 0:tn], [PSb[bk]], [b_dst])

                    ph = new_phase()
                    norm_to_HT(n1g, 0, 1, ph)
                    make_gb(2)
                    dmp("ht1", HT, HTb)
                    if stop_after == "norm1":
                        raise Stop()

                    ph = new_phase()
                    win = ph.take([8, 256], BF16); b_win = pbuf("win")
                    uT = ph.take([2, T], BF16); b_uT = pbuf("uT")
                    dma("pool", win, wview(dr["w_in"][l])[:, :, 1536:1792], [], [b_win])
                    proj_fm(uT, b_uT, win, b_win, 2)
                    dmp("uT", uT, [b_uT])
                    y1T = ph.take([2, T], BF16); b_y1T = pbuf("y1T")
                    wglu = ph.take([2, 256], BF16); b_wglu = pbuf("wglu")
                    dma("pool", wglu, wview(dr["w_glu"][l]), [], [b_wglu])
                    bglu = ph.take([2], F32); dsk = ph.take([2], F32); b_sp = pbuf("s5small")
                    dma("sp", bglu, dr["b_gluT"][l], [], [b_sp])
                    dma("sp", dsk, dr["s5_dT"][l], [], [b_sp])
                    wo_s = ph.take([2, D], BF16); b_wo_s = pbuf("wo_s")
                    dma("pool", wo_s, wview(dr["w_out"][l])[:, 4:6, :], [], [b_wo_s])
                    bz_off = ph.off
                    bzr = ph.take([8, 128], BF16); bzi = ph.take([8, 128], BF16); b_bz = pbuf("bz")
                    czr = ph.take([8, 128], BF16); czi = ph.take([8, 128], BF16); b_cz = pbuf("cz")
                    bzcz_flat = view(bz_off, [4096], BF16)[0]
                    assert ph.off == bz_off + 4 * 2048
                    sm = ph.take([3, 8], F32); b_sm = pbuf("sm")
                    smv = ph.take([40, 8], F32); b_smv = pbuf("smv")
                    dgd = ph.take([2, 128], BF16); b_dgd = pbuf("dgd")
                    carry = ph.take([2, 8], F32); b_carry = pbuf("carry")
                    ytmp = ph.take([2, 128], F32); b_ytmp = pbuf("ytmp")
                    SC = [ph.take([8, 128], F32) for _ in range(11)]
                    b_SC = [pbuf("sc%d" % i) for i in range(11)]
                    def arena_pair(v0):
                        off = v0.offset - arena_t[:, :].offset
                        return arena_t[:, off:off + 2048].rearrange("p (t a b) -> p t a b", t=2, a=8)

                    def halves(v):
                        f = flat(v).bitcast(BF16)
                        return [f[:, 0:1024].rearrange("p (a b) -> p a b", a=8), f[:, 1024:2048].rearrange("p (a b) -> p a b", a=8)]
                    GR, GI, RT0, ZR, ZI = SC[0:5]
                    b_GR, b_GI, b_RT0, b_ZR, b_ZI = b_SC[0:5]
                    BUrb, BUib = halves(SC[5]); b_BUb = b_SC[5]
                    COSb, SINb = halves(SC[6]); b_tabb = b_SC[6]
                    p1, p2 = halves(SC[7]); b_pp = b_SC[7]
                    grb, gib = halves(SC[8]); b_gb = b_SC[8]
                    HRb, HIb = halves(SC[9]); b_Hb = b_SC[9]
                    p3, p4 = halves(SC[10]); b_pq = b_SC[10]
                    sig = [flat(ZR)[:, 0:512], flat(ZR)[:, 512:1024]]; b_sig = b_ZR
                    wtmp = [flat(ZI)[:, 0:512], flat(ZI)[:, 512:1024]]; b_wt5 = b_ZI
                    c127 = ph.take([2, 8], F32); b_c127 = pbuf("c127")

                    def sincos(o_sin, o_cos, th, t1, t2, t3, R_, W_):
                        RW = R_ + W_
                        t1i = t1.bitcast(I32)
                        for (o_, shift) in ((o_sin, 0.0), (o_cos, 0.5 * PI)):
                            if shift == 0.0:
                                src = th
                            else:
                                ts("dve", t3, th, shift, ALU.add, RW, W_)
                                src = t3
                            ts("dve", t1i, src, 1.0 / (2 * PI), ALU.mult, RW, W_)
                            cp("dve", t2, t1i, W_, W_)
                            stt("dve", t3, t2, -2 * PI, src, ALU.mult, ALU.add, RW, W_)
                            ts("dve", t3, t3, -PI, ALU.max, W_, W_, s2=PI, op1=ALU.min)
                            act(o_, t3, AF.Sin, W_, W_)

                    def coef_math(a, b, ls, tmp, R_, W_, extra=None):
                        dlt, rr, th, sn, cs, t1, t2, t3 = tmp
                        RW = R_ + W_
                        act(dlt, ls, AF.Exp, R_, W_)
                        tt("dve", rr, dlt, a, ALU.mult, RW, W_)
                        if extra is not None:
                            act(extra[0], rr, AF.Exp, W_, W_, scale=128.0)
                        act(rr, rr, AF.Exp, W_, W_)
                        tt("dve", th, dlt, b, ALU.mult, RW, W_)
                        if extra is not None:
                            ts("dve", extra[1], th, 128.0, ALU.mult, W_, W_)
                        sincos(sn, cs, th, t1, t2, t3, W_, W_)
                        tt("dve", cs, cs, rr, ALU.mult, W_, W_)
                        tt("dve", sn, sn, rr, ALU.mult, W_, W_)
                        tt("dve", t1, a, a, ALU.mult, RW, W_)
                        tt("dve", t2, b, b, ALU.mult, RW, W_)
                        tt("dve", t1, t1, t2, ALU.add, W_, W_)
                        recip(t1, t1, W_, W_)
                        ts("dve", t2, cs, -1.0, ALU.add, W_, W_)
                        tt("dve", t3, t2, a, ALU.mult, RW, W_)
                        tt("dve", dlt, sn, b, ALU.mult, RW, W_)
                        tt("dve", t3, t3, dlt, ALU.add, W_, W_)
                        tt("dve", t3, t3, t1, ALU.mult, W_, W_)
                        tt("dve", dlt, sn, a, ALU.mult, RW, W_)
                        tt("dve", t2, t2, b, ALU.mult, RW, W_)
                        tt("dve", dlt, dlt, t2, ALU.subtract, W_, W_)
                        tt("dve", dlt, dlt, t1, ALU.mult, W_, W_)
                        return dict(theta=th, r=rr, lbr=cs, lbi=sn, cr=t3, ci=dlt)

                    for ct in range(2):
                        ts("dve", dgd[:, ct, :], ident, dsk[:, ct:ct + 1], ALU.mult, [b_ident, b_sp], [b_dgd])
                    for d in (1, 0):
                        rev = (d == 1)
                        allb = list(b_SC)
                        W_ = [b_smv]
                        KA = smv[:, 16:18, :]; KB = smv[:, 18:20, :]; fa = smv[:, 20:22, :]; fb = smv[:, 22:24, :]
                        sc_ = s5_scratch(l, d)
                        tab_flat = flat(SC[6]).bitcast(BF16)
                        kab_flat = smv[:, 16:20, :].rearrange('p a b -> p (a b)')
                        if bi == 0:
                            for q_, slot in enumerate((0, 1, 2)):
                                dma("sp", flat(SC[slot]), dr["s5_bc"][l, d, q_].partition_broadcast(128), [], allb)
                            o = coef_math(flat(SC[0]), flat(SC[1]), flat(SC[2]), [flat(SC[i]) for i in range(3, 11)], [], allb)
                            zre, zim, w1, w2 = flat(SC[0]), flat(SC[1]), flat(SC[2]), flat(SC[4])
                            dma("sp", zre, dr["s5_bz"][l, d, 0].rearrange("p a b -> p (a b)"), [], allb)
                            dma("sp", zim, dr["s5_bz"][l, d, 1].rearrange("p a b -> p (a b)"), [], allb)
                            cr, ci = o["cr"], o["ci"]
                            tt("dve", w1, zre, cr, ALU.mult, allb, allb)
                            tt("dve", w2, zim, ci, ALU.mult, allb, allb)
                            tt("dve", flat(bzr), w1, w2, ALU.subtract, allb, [b_bz])
                            tt("dve", w1, zre, ci, ALU.mult, allb, allb)
                            tt("dve", w2, zim, cr, ALU.mult, allb, allb)
                            tt("dve", flat(bzi), w1, w2, ALU.add, allb, [b_bz])
                            dma("pool", czr, dr["s5_cz"][l, d, 0], [], [b_cz])
                            dma("pool", czi, dr["s5_cz"][l, d, 1], [], [b_cz])
                            ts("dve", flat(czi), flat(czi), -1.0, ALU.mult, [b_cz], [b_cz])
                            dma("sp", sm, dr["s5_sm"][l, d], [], [b_sm])
                            W_ = [b_smv]
                            r128, th128 = smv[:, 8, :], smv[:, 9, :]
                            so = coef_math(sm[:, 0, :], sm[:, 1, :], sm[:, 2, :], [smv[:, i, :] for i in range(8)], [b_sm], W_,
                                           extra=(r128, th128))
                            s128, c128 = smv[:, 10, :], smv[:, 11, :]
                            sincos(s128, c128, th128, smv[:, 12, :], smv[:, 13, :], smv[:, 14, :], W_, W_)
                            KA = smv[:, 16:18, :]; KB = smv[:, 18:20, :]; fa = smv[:, 20:22, :]; fb = smv[:, 22:24, :]
                            tt("dve", KA[:, 0, :], so["r"], c128, ALU.mult, W_, W_)
                            cp("dve", KA[:, 1, :], KA[:, 0, :], W_, W_)
                            tt("dve", KB[:, 1, :], so["r"], s128, ALU.mult, W_, W_)
                            ts("dve", KB[:, 0, :], KB[:, 1, :], -1.0, ALU.mult, W_, W_)
                            ANG = SC[3]
                            for st in range(8):
                                ts("dve", ANG[:, st, :], iota, so["theta"][:, st:st + 1], ALU.mult, [b_iota, b_smv] + allb, allb)
                                ts("dve", RT0[:, st, :], iota, 1.0, ALU.min, [b_iota, b_smv] + allb, allb, s2=so["r"][:, st:st + 1], op1=ALU.mult)
                            sincos(flat(SC[4]), flat(SC[7]), flat(ANG), flat(SC[0]), flat(SC[1]), flat(SC[8]), allb, allb)
                            cp("act", SINb, SC[4], allb, allb)
                            cp("act", COSb, SC[7], allb, allb)
                            dma('sp', sc_['A'][:, 0:4096], bzcz_flat, [b_bz, b_cz], [sc_['bA1']])
                            dma('sp', sc_['A'][:, 4096:6144], tab_flat, [b_tabb], [sc_['bA2']])
                            dma('sp', sc_['B'][:, 0:1024], flat(RT0), [b_RT0], [sc_['bB1']])
                            dma('sp', sc_['B'][:, 1024:1056], kab_flat, [b_smv], [sc_['bB2']])
                        else:
                            dma('sp', bzcz_flat, sc_['A'][:, 0:4096], [sc_['bA1']], [b_bz, b_cz])
                            dma('sp', tab_flat, sc_['A'][:, 4096:6144], [sc_['bA2']], [b_tabb])
                            dma('sp', flat(RT0), sc_['B'][:, 0:1024], [sc_['bB1']], [b_RT0])
                            dma('sp', kab_flat, sc_['B'][:, 1024:1056], [sc_['bB2']], [b_smv])
                        G127 = arena_pair(SC[0])[:, :, :, 127]
                        G127s = arena_pair(SC[0])[:, ::-1, :, 127]
                        Z0 = arena_pair(SC[3])[:, :, :, 0]
                        b_G2 = [b_GR, b_GI]; b_Z2 = [b_ZR, b_ZI]

                        order = ([1, 0] + list(range(17, 1, -1))) if rev else list(range(18))

                        def tokslice(k):
                            t0 = k * 128
                            if rev:
                                return slice(t0 + 127, (t0 - 1) if t0 > 0 else None, -1)
                            return slice(t0, t0 + 128)

                        def bu_stage(k):
                            tok = tokslice(k)
                            for st in range(8):
                                ct = st // 4
                                for ri, bz_ in enumerate((bzr, bzi)):
                                    bk = ri * 2 + st // 4
                                    mm(PS[bk][:, (st % 4) * 128:(st % 4 + 1) * 128], bz_[:, st, :], uT[:, ct, tok], True, True,
                                       [b_bz, b_uT], [PSb[bk]])
                            for h in range(2):
                                sl = slice(4 * h, 4 * h + 4)
                                cp("act", BUrb[:, sl, :], PS[h][:, :].rearrange("p (a b) -> p a b", a=4), [PSb[h]], [b_BUb])
                                cp("act", BUib[:, sl, :], PS[2 + h][:, :].rearrange("p (a b) -> p a b", a=4), [PSb[2 + h]], [b_BUb])

                        def y_stage(n_c, k):
                            t0 = k * 128
                            for ct in range(2):
                                bk = 4 + 2 * (n_c % 2) + ct
                                if d == 1:
                                    cp("act", y1T[:, ct, t0:t0 + 128], PS[bk][:, 0:128], [PSb[bk]], [b_y1T])
                                else:
                                    tt("dve", ytmp[:, ct, :], PS[bk][:, 0:128], y1T[:, ct, t0:t0 + 128], ALU.add, [PSb[bk], b_y1T], [b_ytmp])
                                    act(y1T[:, ct, t0:t0 + 128], ytmp[:, ct, :], AF.Gelu, [b_ytmp], [b_y1T])

                        def fwd_stage():
                            tt("dve", flat(p1), flat(BUrb), flat(COSb), ALU.mult, [b_BUb, b_tabb], [b_pp])
                            tt("dve", flat(p2), flat(BUib), flat(SINb), ALU.mult, [b_BUb, b_tabb], [b_pp])
                            tt("dve", flat(ZR), flat(p1), flat(p2), ALU.add, [b_pp], [b_ZR])
                            tt("dve", flat(p3), flat(BUib), flat(COSb), ALU.mult, [b_BUb, b_tabb], [b_pq])
                            tt("dve", flat(p4), flat(BUrb), flat(SINb), ALU.mult, [b_BUb, b_tabb], [b_pq])
                            tt("dve", flat(ZI), flat(p3), flat(p4), ALU.subtract, [b_pq], [b_ZI])

                        NC = len(order)
                        bu_stage(order[0])
                        fwd_stage()
                        if NC > 1:
                            bu_stage(order[1])
                        for n_c, k in enumerate(order):
                            if n_c > 0:
                                tt("dve", fa, G127, KA, ALU.mult, b_G2 + W_, W_)
                                tt("dve", fb, G127s, KB, ALU.mult, b_G2 + W_, W_)
                                tt("dve", fa, fa, fb, ALU.add, W_, W_)
                                tt("dve", Z0, Z0, fa, ALU.add, b_Z2 + W_, b_Z2)
                            P.op("dve", lambda e: e.tensor_tensor_scan(out=flat(GR), data0=flat(RT0), data1=flat(ZR), initial=0.0,
                                                                       op0=ALU.mult, op1=ALU.add), [b_RT0, b_ZR], [b_GR])
                            P.op("dve", lambda e: e.tensor_tensor_scan(out=flat(GI), data0=flat(RT0), data1=flat(ZI), initial=0.0,
                                                                       op0=ALU.mult, op1=ALU.add), [b_RT0, b_ZI], [b_GI])
                            cp("act", grb, GR, [b_GR], [b_gb])
                            cp("act", gib, GI, [b_GI], [b_gb])
                            if n_c + 1 < NC:
                                fwd_stage()
                                if n_c + 2 < NC:
                                    bu_stage(order[n_c + 2])
                            tt("dve", flat(p1), flat(grb), flat(COSb), ALU.mult, [b_gb, b_tabb], [b_pp])
                            tt("dve", flat(p2), flat(gib), flat(SINb), ALU.mult, [b_gb, b_tabb], [b_pp])
                            tt("dve", flat(HRb), flat(p1), flat(p2), ALU.subtract, [b_pp], [b_Hb])
                            tt("dve", flat(p3), flat(grb), flat(SINb), ALU.mult, [b_gb, b_tabb], [b_pq])
                            tt("dve", flat(p4), flat(gib), flat(COSb), ALU.mult, [b_gb, b_tabb], [b_pq])
                            tt("dve", flat(HIb), flat(p3), flat(p4), ALU.add, [b_pq], [b_Hb])
                            for ct in range(2):
                                bk = 4 + 2 * (n_c % 2) + ct
                                n = 0
                                for st in range(4 * ct, 4 * ct + 4):
                                    for (cz_, hh_) in ((czr, HRb), (czi, HIb)):
                                        rhs = hh_[:, st, ::-1] if rev else hh_[:, st, :]
                                        mm(PS[bk][:, 0:128], cz_[:, st, :], rhs, n == 0, (n == 7 and d == 1), [b_cz, b_Hb], [PSb[bk]])
                                        n += 1
                                if d == 0:
                                    mm(PS[bk][:, 0:128], dgd[:, ct, :], uT[:, ct, k * 128:(k + 1) * 128], False, True, [b_dgd, b_uT], [PSb[bk]])
                            if n_c > 0:
                                y_stage(n_c - 1, order[n_c - 1])
                        y_stage(len(order) - 1, order[-1])
                    dmp("gT", y1T, [b_y1T])
                    n = 0
                    for (t0, tn) in GROUPS:
                        for co in range(2):
                            bk = n % 4
                            s = n % 2
                            n += 1
                            for k in range(2):
                                mm(PS[bk][:, 0:tn], wglu[:, k, co * 128:(co + 1) * 128], y1T[:, k, t0:t0 + tn], k == 0, k == 1,
                                   [b_wglu, b_y1T], [PSb[bk]])
                            act(sig[s][:, 0:tn], PS[bk][:, 0:tn], AF.Sigmoid, [PSb[bk], b_sp], [b_sig], bias=bglu[:, co:co + 1])
                            tt("dve", uT[:, co, t0:t0 + tn], sig[s][:, 0:tn], y1T[:, co, t0:t0 + tn], ALU.mult, [b_sig, b_y1T], [b_uT])
                    dmp("ssmT", uT, [b_uT])
                    wout_partial(uT, b_uT, 2, wo_s, b_wo_s, wtmp, [b_wt5, b_wt5])
                    if stop_after == "s5":
                        raise Stop()

                    ph = new_phase()
                    win = ph.take([8, 256], BF16); b_win = pbuf("winf")
                    ufT = ph.take([2, T], BF16); b_ufT = pbuf("ufT")
                    dma("pool", win, wview(dr["w_in"][l])[:, :, 1792:2048], [], [b_win])
                    proj_fm(ufT, b_ufT, win, b_win, 2)
                    wo_f = ph.take([2, D], BF16); b_wo_f = pbuf("wo_f")
                    dma("pool", wo_f, wview(dr["w_out"][l])[:, 6:8, :], [], [b_wo_f])
                    wfz = ph.take([2, 256], F32); b_wfz = pbuf("wfz")
                    dma("sp", wfz, dr["w_fz"][l].rearrange("k p n -> p k n"), [], [b_wfz])
                    c64 = ph.take([128], F32); s64 = ph.take([128], F32); b_c64 = pbuf("c64")
                    dma("sp", c64, dr["c64blk"][:, :], [], [b_c64])
                    dma("sp", s64, dr["s64blk"][:, :], [], [b_c64])
                    G = ph.take([2, 512], BF16); b_G = pbuf("G")
                    for kt in range(2):
                        for ti, tb in enumerate((c64, s64)):
                            mm(PS[ti][:, 0:256], tb, wfz[:, kt, :], True, True, [b_c64, b_wfz], [PSb[ti]])
                            cp("act", G[:, kt, ti * 256:(ti + 1) * 256], PS[ti][:, 0:256], [PSb[ti]], [b_G])
                    A_tok = ph.take([NT, 512], BF16); b_A = pbuf("A_tok")
                    for i in range(NT):
                        bk = i % 4
                        for kt in range(2):
                            mm(PS[bk], ufT[:, kt, i * 128:(i + 1) * 128], G[:, kt, :], kt == 0, kt == 1, [b_ufT, b_G], [PSb[bk]])
                        if i == 0:
                            cp("act", A_tok[:, i, :], PS[bk], [PSb[bk]], [b_A])
                        else:
                            cp("act" if i % 2 == 0 else "dve", A_tok[:, i, :], PS[bk], [PSb[bk]], [], pw=[b_A])
                    ring = [ph.take([4, 512], BF16) for _ in range(4)]
                    b_ring = [pbuf("ring%d" % i) for i in range(4)]
                    ctab = [ph.take([2, 256], BF16) for _ in range(2)]; b_ctab = pbuf("ctab")
                    wtmp = [ph.take([512], F32) for _ in range(2)]; b_wtmp = [pbuf("wtmpf%d" % i) for i in range(2)]
                    fouT = ufT
                    b_fouT = pbuf("fouT", extra_old=[b_ufT])
                    tabs = (dr["dftc"], dr["dftns"])
                    nring = 0
                    for lg in range(4):
                        nmm = 0
                        for ktg in range(4):
                            for ti in range(2):
                                s = nring % 4
                                nring += 1
                                src = tabs[ti].rearrange("(k p) n -> p k n", p=128)[:, ktg * 4:(ktg + 1) * 4, lg * 512:(lg + 1) * 512]
                                dma("sp", ring[s], src, [], [b_ring[s]])
                                for kk in range(4):
                                    kt = ktg * 4 + kk
                                    for ct in range(2):
                                        mm(PS[4 + ct], A_tok[:, 2 + kt, ti * 256 + ct * 128: ti * 256 + (ct + 1) * 128], ring[s][:, kk, :],
                                           nmm < 2, nmm >= 62, [b_A, b_ring[s]], [PSb[4 + ct]])
                                        nmm += 1
                        for ct in range(2):
                            cp("act", fouT[:, ct, 256 + lg * 512: 256 + (lg + 1) * 512], PS[4 + ct], [PSb[4 + ct], b_A], [b_fouT])
                    dma("sp", ctab[0], wview(dr["dftc_c"]), [], [b_ctab])
                    dma("sp", ctab[1], wview(dr["dftns_c"]), [], [b_ctab])
                    for ct in range(2):
                        n = 0
                        for ti in range(2):
                            for kt in range(2):
                                mm(PS[ct][:, 0:256], A_tok[:, kt, ti * 256 + ct * 128: ti * 256 + (ct + 1) * 128], ctab[ti][:, kt, :],
                                   n == 0, n == 3, [b_A, b_ctab], [PSb[ct]])
                                n += 1
                        cp("act", fouT[:, ct, 0:256], PS[ct][:, 0:256], [PSb[ct], b_A], [b_fouT])
                    dmp("fouT", fouT, [b_fouT])
                    wout_partial(fouT, b_fouT, 2, wo_f, b_wo_f, wtmp, b_wtmp)
                    if stop_after == "fnet":
                        raise Stop()

                    ph = new_phase()
                    wq = [ph.take([8, 512], BF16) for _ in range(2)]
                    b_wq = [pbuf("wq%d" % i) for i in range(2)]
                    qT = ph.take([4, T], BF16); kT = ph.take([4, T], BF16)
                    b_qT = pbuf("qT"); b_kT = pbuf("kT")
                    V = ph.take([NT, 8 * 65], BF16); b_V = pbuf("V")
                    memset("pool", V.rearrange("p t (h d) -> p (t h) d", d=65)[:, :, 64:65], 1.0, [b_V])
                    sq = ph.take([512], BF16); b_sq = pbuf("sq")
                    qn_off = ph.off
                    qn = [ph.take([512], F32) for _ in range(2)]; b_qn = [pbuf("qn%d" % i) for i in range(2)]
                    tsb_off = ph.off
                    tsb = ph.take([512], F32); b_tsb = pbuf("tsb")
                    qr = [ph.take([512], BF16) for _ in range(2)]; b_qr = [pbuf("qr%d" % i) for i in range(2)]
                    s8 = ph.take([2, 8], F32); b_s8 = [pbuf("s8_%d" % i) for i in range(2)]
                    Gqk = ph.take([2, 64], F32); b_Gqk = pbuf("Gqk")
                    dma("sp", Gqk[:, 0, :], dr["qng"][l].partition_broadcast(128), [], [b_Gqk])
                    dma("sp", Gqk[:, 1, :], dr["kng"][l].partition_broadcast(128), [], [b_Gqk])
                    items = [(ci, i) for ci in range(3) for i in range(NT)]
                    MMB = [0, 1, 4, 5]

                    def load_wq(ci):
                        dma("pool", wq[ci % 2], wview(dr["w_in"][l])[:, :, ci * 512:(ci + 1) * 512], [], [b_wq[ci % 2]])

                    def stA(n):
                        ci, i = items[n]
                        s = ci % 2
                        bk = MMB[n % 4]
                        for k in range(8):
                            mm(PS[bk], HT[:, k, i * 128:(i + 1) * 128], wq[s][:, k, :], k == 0, k == 7, [HTb[i], b_wq[s]], [PSb[bk]])
                        if ci == 0 and i == NT - 1:
                            load_wq(2)

                    def stB(n):
                        ci, i = items[n]
                        bk = MMB[n % 4]
                        if ci == 2:
                            cp("act", V[:, i, :].rearrange("p (h d) -> p h d", d=65)[:, :, 0:64],
                               PS[bk].rearrange("p (h d) -> p h d", d=64), [PSb[bk]], [], pw=[b_V])
                            return
                        u = n % 2
                        s8u = s8[:, u, :]
                        act(sq, PS[bk], AF.Square, [PSb[bk]], [b_sq])
                        red(s8u, sq.rearrange("p (h d) -> p h d", h=8), ALU.add, [b_sq], [b_s8[u]])
                        act(s8u, s8u, AF.Sqrt, [b_s8[u], b_ccol], [b_s8[u]], bias=eps_col, scale=1.0 / 64)
                        recip(s8u, s8u, [b_s8[u]], [b_s8[u]])
                        qn3 = qn[u].rearrange("p (h d) -> p h d", h=8)
                        tt("dve", qn3, PS[bk].rearrange("p (h d) -> p h d", h=8), s8u.unsqueeze(2).to_broadcast([128, 8, 64]),
                           ALU.mult, [PSb[bk], b_s8[u]], [b_qn[u]])
                        tt("dve", qn3, qn3, Gqk[:, ci, :].unsqueeze(1).to_broadcast([128, 8, 64]), ALU.mult, [b_qn[u], b_Gqk], [b_qn[u]])
                        qr3 = qr[u].rearrange("p (h d) -> p h d", h=8)
                        if i >= 2:
                            j = i - 2
                            cb = ropec[:, j, :].unsqueeze(1).to_broadcast([128, 16, 32])
                            sb_ = ropes[:, j, :].unsqueeze(1).to_broadcast([128, 16, 32])
                            qn4 = qn[u].rearrange("p (g d) -> p g d", g=16)
                            tt("dve", tsb.rearrange("p (g d) -> p g d", g=16), qn4, sb_, ALU.mult, [b_qn[u], b_rope], [b_tsb])
                            tt("dve", qn4, qn4, cb, ALU.mult, [b_qn[u], b_rope], [b_qn[u]])
                            ts3 = tsb.rearrange("p (h d) -> p h d", h=8)
                            tt("pool", qr3[:, :, 0:32], qn3[:, :, 0:32], ts3[:, :, 32:64], ALU.subtract, [b_qn[u], b_tsb], [b_qr[u]])
                            tt("pool", qr3[:, :, 32:64], ts3[:, :, 0:32], qn3[:, :, 32:64], ALU.add, [b_qn[u], b_tsb], [b_qr[u]])
                        else:
                            cp("pool", qr[u], qn[u], [b_qn[u]], [b_qr[u]])

                    def stC(n):
                        ci, i = items[n]
                        if ci == 2:
                            return
                        u = n % 2
                        tb = 2 + u
                        for hp in range(4):
                            tr(PSH[tb][:, hp * 128:(hp + 1) * 128], qr[u][:, hp * 128:(hp + 1) * 128], identb,
                               [b_qr[u], b_identb], [PSb[tb]])
                        dst = qT if ci == 0 else kT
                        cp("act", dst[:, :, i * 128:(i + 1) * 128], PSH[tb][:, 0:512].rearrange("p (a b) -> p a b", a=4),
                           [PSb[tb]], [b_qT if ci == 0 else b_kT])

                    load_wq(0)
                    load_wq(1)
                    NI = len(items)
                    stA(0)
                    stA(1)
                    for n in range(NI):
                        stB(n)
                        if n + 2 < NI:
                            stA(n + 2)
                        if n >= 1:
                            stC(n - 1)
                    stC(NI - 1)
                    dmp("qT", qT, [b_qT])
                    dmp("kT", kT, [b_kT])
                    hta = Alloc(HT_OFF, HT_END)
                    NBT = 3
                    BT = [hta.take([21, 128], BF16) for _ in range(NBT)]
                    b_BT = [pbuf("BT%d" % i, extra_old=HTb) for i in range(NBT)]
                    attT = hta.take([4, T], BF16); b_attT = pbuf("attT", extra_old=HTb)
                    wo_a = wq[0].rearrange("p k n -> p (k n)").rearrange("p (k n) -> p k n", k=4)
                    b_wo_a = b_wq[0]
                    dma("pool", wo_a, wview(dr["w_out"][l])[:, 0:4, :], [], [b_wo_a])
                    w1flat = wq[1].rearrange("p k n -> p (k n)")
                    PT = [w1flat[:, s * 896:(s + 1) * 896].rearrange("p (a b) -> p a b", a=7) for s in range(2)]
                    b_PT = [pbuf("PT%d" % s, extra_old=[b_wq[1]]) for s in range(2)]
                    Ef = [w1flat[:, 1792:3072].bitcast(F32).rearrange("p (a b) -> p a b", a=5), view(qn_off, [5, 128], F32)[0]]
                    b_E = [pbuf("E0", extra_old=[b_wq[1]]), pbuf("E1", extra_old=b_qn)]
                    rinv = [view(tsb_off + 512 * i, [128], F32)[0] for i in range(2)]
                    b_rinv = [pbuf("rinv%d" % i, extra_old=[b_tsb]) for i in range(2)]
                    its = []
                    for hp in range(4):
                        for n_q, qi in enumerate(list(range(2, NT)) + [0, 1]):
                            for hh in range(2):
                                its.append((2 * hp + hh, qi, n_q))
                    att2 = [ph.take([128], BF16) for _ in range(2)]; b_att2 = [pbuf("att2_%d" % i) for i in range(2)]
                    rinv2 = ph.take([2, 2], F32); b_rinv2 = [pbuf("rinv2_%d" % i) for i in range(2)]

                    def it_info(n):
                        h, qi, n_h = its[n]
                        if qi >= 2:
                            kts, t0b = key_tiles(qi - 2)
                            lat = [2 + k for k in kts]
                        else:
                            lat, t0b = [], 0
                        return h, qi, n_h, lat, t0b

                    def st1(n):
                        h, qi, n_h, lat, t0b = it_info(n)
                        hp, hh = h // 2, h % 2
                        pr = slice(64 * hh, 64 * hh + 64)
                        keys = lat + [0, 1]
                        sb2 = (n % 2) * 2
                        qsl = slice(qi * 128, (qi + 1) * 128)
                        for m, kt_ in enumerate(keys):
                            bk = sb2 + m // 4
                            mm(PS[bk][:, (m % 4) * 128:(m % 4 + 1) * 128], kT[pr, hp, kt_ * 128:(kt_ + 1) * 128], qT[pr, hp, qsl],
                               True, True, [b_kT, b_qT], [PSb[bk]])

                    def st2(n):
                        h, qi, n_h, lat, t0b = it_info(n)
                        bs = h % NBT
                        nl = len(lat)
                        nk = nl + 2
                        sb2 = (n % 2) * 2
                        ps_ = n % 2
                        if h % 2 == 0 and n_h == 1 and h + 2 < 8:
                            dma("pool", BT[(h + 2) % NBT], dr["bt"][l, h + 2], [], [b_BT[(h + 2) % NBT]])
                        if h % 2 == 0 and n_h == 0 and h >= 2:
                            dma("pool", BT[(h + 1) % NBT], dr["bt"][l, h + 1], [], [b_BT[(h + 1) % NBT]])
                        m = 0
                        while m < nk:
                            bk = sb2 + m // 4
                            if m < nl:
                                m2 = min(nl, (m // 4 + 1) * 4)
                                src = PS[bk][:, (m % 4) * 128:(m % 4) * 128 + (m2 - m) * 128].rearrange("p (a b) -> p a b", b=128)
                                stt("dve", Ef[ps_][:, m:m2, :], src, 0.125, BT[bs][:, t0b + m:t0b + m2, :], ALU.mult, ALU.add,
                                    [PSb[bk], b_BT[bs]], [b_E[ps_]])
                                act(PT[ps_][:, m:m2, :], Ef[ps_][:, m:m2, :], AF.Exp, [b_E[ps_]], [b_PT[ps_]])
                            else:
                                m2 = min(nk, (m // 4 + 1) * 4)
                                src = PS[bk][:, (m % 4) * 128:(m % 4) * 128 + (m2 - m) * 128].rearrange("p (a b) -> p a b", b=128)
                                act(PT[ps_][:, m:m2, :], src, AF.Exp, [PSb[bk]], [b_PT[ps_]], scale=0.125)
                            m = m2

                    def st3(n):
                        h, qi, n_h, lat, t0b = it_info(n)
                        hh = h % 2
                        keys = lat + [0, 1]
                        nk = len(keys)
                        pi = n // 2
                        ob = 4 + pi % 2
                        ps_ = n % 2
                        for m, kt_ in enumerate(keys):
                            mm(PS[ob][:, hh * 128:hh * 128 + 65], PT[ps_][:, m, :], V[:, kt_, h * 65:(h + 1) * 65], m == 0, m == nk - 1,
                               [b_V, b_PT[ps_]], [PSb[ob]])

                    def fin1(pi):
                        hp, qi = its[2 * pi][0] // 2, its[2 * pi][1]
                        ob = 4 + pi % 2
                        u = pi % 2
                        o3 = PS[ob][:, 0:256].rearrange("p (h d) -> p h d", d=128)
                        recip(rinv2[:, u, :], o3[:, :, 64], [PSb[ob]], [b_rinv2[u]])
                        tt("dve", att2[u].rearrange("p (h d) -> p h d", d=64), o3[:, :, 0:64],
                           rinv2[:, u, :].unsqueeze(2).to_broadcast([128, 2, 64]), ALU.mult, [PSb[ob], b_rinv2[u]], [b_att2[u]])

                    def fin2(pi):
                        hp, qi = its[2 * pi][0] // 2, its[2 * pi][1]
                        u = pi % 2
                        tb = 6 + u
                        tr(PSH[tb][:, 0:128], att2[u], identb, [b_att2[u], b_identb], [PSb[tb]])
                        cp("act", attT[:, hp, qi * 128:(qi + 1) * 128], PSH[tb][:, 0:128], [PSb[tb]], [], pw=[b_attT])

                    memset("pool", attT[:, 0, 0:2], 0.0, [b_attT])
                    dma("pool", BT[0], dr["bt"][l, 0], [], [b_BT[0]])
                    dma("pool", BT[1], dr["bt"][l, 1], [], [b_BT[1]])
                    st1(0)
                    st1(1)
                    st2(0)
                    NIT = len(its)
                    for n in range(NIT):
                        if n + 2 < NIT:
                            st1(n + 2)
                        if n + 1 < NIT:
                            st2(n + 1)
                        st3(n)
                        if n % 2 == 1:
                            fin1(n // 2)
                            if n // 2 >= 1:
                                fin2(n // 2 - 1)
                    fin2(NIT // 2 - 1)
                    dmp("attT", attT, [b_attT])
                    b_wt = [pbuf("wtmpa%d" % i, extra_old=[b_wq[1], b_E[0]] + b_PT) for i in range(2)]
                    wout_partial_h = None
                    wtmp = [w1flat[:, 0:1024].bitcast(F32), w1flat[:, 1024:2048].bitcast(F32)]
                    wout_partial(attT, b_attT, 4, wo_a, b_wo_a, wtmp, b_wt)
                    dmp("xmix", X, Xb)
                    if stop_after == "attn":
                        raise Stop()

                    ph = new_phase()
                    for b_ in HTb:
                        P.alias([b_], [b_attT] + b_BT)
                    wgu = [None, None]; wd = [None, None]; b_wgu = [None, None]; b_wd = [None, None]
                    wgu[0] = ph.take([8, 1024], BF16); wd[0] = ph.take([4, 1024], BF16)
                    b_wgu[0] = pbuf("wgu0"); b_wd[0] = pbuf("wd0")

                    def load_expert(e):
                        s_ = e % 2
                        dma("pool", wgu[s_][:, :, 0:512], wview(dr["w_gate"][l, e]), [], [b_wgu[s_]])
                        dma("pool", wgu[s_][:, :, 512:1024], wview(dr["w_up"][l, e]), [], [b_wgu[s_]])
                        dma("pool", wd[s_], wview(dr["w_down"][l, e]), [], [b_wd[s_]])

                    load_expert(0)
                    sub_base = ph.off
                    wr = ph.take([8, 16], F32); b_wr = pbuf("wr")
                    dma("sp", wr, dr["w_router"][:, :, :], [], [b_wr])
                    h2f = [ph.take([8, 128], F32) for _ in range(2)]
                    b_h2f = [pbuf("h2f%d" % i) for i in range(2)]

                    def router(i, pb, r_i):
                        s = i % 2
                        for c in range(8):
                            bk = pb + c // 4
                            src_ = PS[bk][:, (c % 4) * 128:(c % 4 + 1) * 128]
                            wr_, pw_ = ([b_h2f[s]], ()) if c == 0 else ([], [b_h2f[s]])
                            if c < 4:
                                act(h2f[s][:, c, :], src_, AF.Identity, [PSb[bk], b_AB], wr_,
                                    bias=AB[:, r_i, 1, c:c + 1], scale=AB[:, r_i, 0, c:c + 1], pw=pw_)
                            else:
                                ts("dve", h2f[s][:, c, :], src_, AB[:, r_i, 0, c:c + 1], ALU.mult, [PSb[bk], b_AB], wr_,
                                   s2=AB[:, r_i, 1, c:c + 1], op1=ALU.add, pw=pw_)
                        cp("pool", HT[:, :, i * 128:(i + 1) * 128], h2f[s], [b_h2f[s]], [HTb[i]])
                        for c in range(8):
                            mm(PS[6][:, i * 16:(i + 1) * 16], h2f[s][:, c, :], wr[:, c, :], c == 0, c == 7, [b_h2f[s], b_wr], [PSb[6]])

                    n_before = len(state["cur"])
                    norm_to_HT(n2g, 3, 4, ph, router=router)
                    make_gb(5)
                    dmp("ht2", HT, HTb)
                    state["extra"] = [b_wr] + b_h2f + state["cur"][n_before:]
                    ph = Alloc(sub_base, PH_LIMIT)
                    wgu[1] = ph.take([8, 1024], BF16); wd[1] = ph.take([4, 1024], BF16)
                    b_wgu[1] = pbuf("wgu1"); b_wd[1] = pbuf("wd1")
                    aff = ph.take([NT, 16], F32); sel = ph.take([NT, 16], F32); w_ = ph.take([NT, 16], F32)
                    eq = ph.take([NT, 16], F32)
                    b_rt = pbuf("rt")
                    m1 = ph.take([72], F32); m2 = ph.take([72], F32); gs = ph.take([72], F32); gsel = ph.take([72], F32)
                    gmax = ph.take([NT], F32); wsum = ph.take([NT], F32)
                    brt = ph.take([16], F32); b_brt = pbuf("brt")
                    dma("sp", brt, dr["b_router"].partition_broadcast(128), [], [b_brt])
                    R = [b_rt]
                    f2 = lambda v: v.rearrange("p a b -> p (a b)")
                    v4 = lambda v: f2(v).rearrange("p (g e) -> p g e", e=4)
                    act(f2(aff), PS[6][:, 0:288], AF.Sigmoid, [PSb[6]], R)
                    dmp("aff", aff, R)
                    tt("dve", sel, aff, brt.unsqueeze(1).to_broadcast([128, NT, 16]), ALU.add, R + [b_brt], R)
                    red(m1, v4(sel), ALU.max, R, R)
                    tt("dve", v4(eq), v4(sel), m1.unsqueeze(2).to_broadcast([128, 72, 4]), ALU.is_equal, R, R)
                    stt("dve", v4(eq), v4(eq), -1.0e9, v4(sel), ALU.mult, ALU.add, R, R)
                    red(m2, v4(eq), ALU.max, R, R)
                    tt("dve", gs, m1, m2, ALU.add, R, R)
                    gs3 = gs.rearrange("p (t g) -> p t g", g=4)
                    red(gmax, gs3, ALU.max, R, R)
                    tt("dve", gsel.rearrange("p (t g) -> p t g", g=4), gs3, gmax.unsqueeze(2).to_broadcast([128, NT, 4]), ALU.is_equal, R, R)
                    tt("dve", v4(eq), v4(sel), m2.unsqueeze(2).to_broadcast([128, 72, 4]), ALU.is_ge, R, R)
                    tt("dve", v4(eq), v4(eq), gsel.unsqueeze(2).to_broadcast([128, 72, 4]), ALU.mult, R, R)
                    tt("dve", w_, aff, eq, ALU.mult, R, R)
                    red(wsum, w_, ALU.add, R, R)
                    recip(wsum, wsum, R, R)
                    tt("dve", w_, w_, wsum.unsqueeze(2).to_broadcast([128, NT, 16]), ALU.mult, R, R)
                    dmp("gates", w_, R)
                    gatesT = ph.take([T], BF16); b_gT = pbuf("gatesT")
                    for i in range(NT):
                        bk = (i // 4) % 2
                        tr(PS[bk][0:16, (i % 4) * 128:(i % 4 + 1) * 128], w_[:, i, :], ident, R + [b_ident], [PSb[bk]])
                        if i % 4 == 3 or i == NT - 1:
                            i0 = (i // 4) * 4
                            n_ = i - i0 + 1
                            cp("act", gatesT[0:16, i0 * 128:(i0 + n_) * 128], PS[bk][0:16, 0:n_ * 128], [PSb[bk]], [b_gT])
                    selc = ph.take([16 * 128], BF16); b_selc = pbuf("selc")
                    dma("pool", selc[0:16, :], dr["sel"][:, :], [], [b_selc])
                    load_expert(1)
                    h1T = [ph.take([4, 512], BF16) for _ in range(2)]; b_h1T = [pbuf("h1T%d" % i) for i in range(2)]
                    sil = [ph.take([512], F32) for _ in range(2)]; b_sil = [pbuf("sil%d" % i) for i in range(2)]
                    hmul = [ph.take([512], BF16) for _ in range(2)]; b_hmul = [pbuf("hmul%d" % i) for i in range(2)]
                    GBC = [ph.take([512], BF16) for _ in range(2)]; b_GBC = [pbuf("GBC%d" % i) for i in range(2)]
                    wtmp = [ph.take([512], F32) for _ in range(2)]; b_wtmp = [pbuf("wtmpm%d" % i) for i in range(2)]
                    cnt = dict(F=0, D=0)

                    def gateup(e, gi):
                        s = e % 2
                        t0, tn = GROUPS[gi]
                        tiles = list(range(t0 // 128, (t0 + tn) // 128))
                        hb = [HTb[i] for i in tiles]
                        gsl = (e * 5 + gi) % 2
                        mm(PS[7][:, 0:tn], selc[0:16, e * 128:(e + 1) * 128], gatesT[0:16, t0:t0 + tn], True, True,
                           [b_selc, b_gT], [PSb[7]])
                        cp("act", GBC[gsl][:, 0:tn], PS[7][:, 0:tn], [PSb[7]], [b_GBC[gsl]])
                        for fc in range(4):
                            fs = cnt["F"] % 2
                            cnt["F"] += 1
                            bg, bu = fs * 2, fs * 2 + 1
                            for k in range(8):
                                mm(PS[bg][:, 0:tn], wgu[s][:, k, fc * 128:(fc + 1) * 128], HT[:, k, t0:t0 + tn], k == 0, k == 7,
                                   [b_wgu[s]] + hb, [PSb[bg]])
                            for k in range(8):
                                mm(PS[bu][:, 0:tn], wgu[s][:, k, 512 + fc * 128:512 + (fc + 1) * 128], HT[:, k, t0:t0 + tn], k == 0, k == 7,
                                   [b_wgu[s]] + hb, [PSb[bu]])
                            act(sil[fs][:, 0:tn], PS[bg][:, 0:tn], AF.Silu, [PSb[bg]], [b_sil[fs]])
                            tt("dve", hmul[fs][:, 0:tn], sil[fs][:, 0:tn], PS[bu][:, 0:tn], ALU.mult, [b_sil[fs], PSb[bu]], [b_hmul[fs]])
                            tt("dve", h1T[gsl][:, fc, 0:tn], hmul[fs][:, 0:tn], GBC[gsl][:, 0:tn], ALU.mult,
                               [b_hmul[fs], b_GBC[gsl]], [b_h1T[gsl]])

                    def down(e, gi):
                        s = e % 2
                        t0, tn = GROUPS[gi]
                        tiles = list(range(t0 // 128, (t0 + tn) // 128))
                        gsl = (e * 5 + gi) % 2
                        for ti, i in enumerate(tiles):
                            r_i = 1 if i < 2 else 0
                            for half in range(2):
                                bk = 4 + cnt["D"] % 2
                                ws = cnt["D"] % 2
                                cnt["D"] += 1
                                for fc in range(4):
                                    mm(PS[bk], h1T[gsl][:, fc, ti * 128:(ti + 1) * 128], wd[s][:, fc, half * 512:(half + 1) * 512],
                                       fc == 0, fc == 3, [b_h1T[gsl], b_wd[s]], [PSb[bk]])
                                tt("dve", wtmp[ws], PS[bk], GB[:, r_i, half * 512:(half + 1) * 512], ALU.mult,
                                   [PSb[bk], b_GB], [b_wtmp[ws]])
                                tt("pool", X[:, i, half * 512:(half + 1) * 512], X[:, i, half * 512:(half + 1) * 512], wtmp[ws],
                                   ALU.add, [b_wtmp[ws], Xb[i]], [Xb[i]])

                    prev = None
                    for e in range(16):
                        for gi in range(len(GROUPS)):
                            gateup(e, gi)
                            if prev is not None:
                                down(*prev)
                            prev = (e, gi)
                            if gi == 0 and 1 <= e < 15:
                                load_expert(e + 1)
                    down(*prev)
                    dmp("xout", X, Xb)
                    if stop_after == "moe":
                        raise Stop()

                for i in range(2, NT):
                    dma("sp", out_d[bi, (i - 2) * 128:(i - 1) * 128, :], X[:, i, :], [Xb[i]], [], semb=Xb[i])
        except Stop:
            pass
        P.emit()
    return nc


_PROG_CACHE = {}


def _shapes(m):
    return {k: tuple(v.shape) for k, v in m.items()}


def kernel(**inputs):
    n_cores = 8
    shared = _prep_shared(inputs)
    x = np.ascontiguousarray(np.asarray(inputs["x"], dtype=np.float32))
    ctx = np.ascontiguousarray(np.asarray(inputs["ctx"], dtype=np.float32))
    c = np.asarray(inputs["c"], dtype=np.float32)
    c_ctx = np.asarray(inputs["c_ctx"], dtype=np.float32)
    in_maps = []
    for core in range(n_cores):
        m = dict(shared)
        b0 = 2 * core
        m["x"] = x[b0:b0 + 2]
        m["ctx"] = ctx[b0:b0 + 2]
        rows = np.stack([c[b0], c[b0 + 1], c_ctx], axis=0)
        m["crowT"] = np.ascontiguousarray(rows.reshape(3, 8, 128).transpose(2, 1, 0))
        in_maps.append(m)
    key = "main"
    if key not in _PROG_CACHE:
        _PROG_CACHE[key] = build_program(_shapes(in_maps[0]))
    nc = _PROG_CACHE[key]
    res = run_bass_kernel_spmd(nc, in_maps, core_ids=list(range(n_cores)))
    out = np.concatenate([np.asarray(r["out"], dtype=np.float32) for r in res.results], axis=0)
    return out
```

```python
import math
from contextlib import ExitStack
import numpy as np
import ml_dtypes
import concourse.bass as bass
import concourse.mybir as mybir
from concourse.bass_utils import run_bass_kernel_spmd

F32 = mybir.dt.float32
BF16 = mybir.dt.bfloat16
I32 = mybir.dt.int32
AF = mybir.ActivationFunctionType
ALU = mybir.AluOpType
AX = mybir.AxisListType

D = 1024
L = 2048
LC = 256
T = L + LC
NT = T // 128
DEPTH = 2
EPS = 1e-6
NEG = -30000.0
PI = math.pi

ENGS = ("pe", "act", "dve", "pool", "sp")
BF16_INPUTS = ("dftc", "dftns", "dftc_c", "dftns_c")


class Buf:
    __slots__ = ("name", "w", "wx", "r", "dsem", "dcnt")

    def __init__(self, name):
        self.name = name
        self.w = None
        self.wx = []
        self.r = []
        self.dsem = None
        self.dcnt = 0


class Prog:
    def __init__(self, nc):
        self.nc = nc
        self.ops = {e: [] for e in ENGS}
        self.seen = {e: {} for e in ENGS}
        self.marked = {e: set() for e in ENGS}
        self.ndsem = 0
        self.dsem_final = {}
        self.dsem_names = {}

    def _deps(self, eng, reads, writes, pwrites=()):
        deps = []
        for b in reads:
            if b.w is not None:
                deps.append(b.w)
            deps.extend(b.wx)
        for b in writes:
            if b.w is not None:
                t = b.w
                if not (t[0] == "E" and t[1] == eng):
                    deps.append(t)
            for t in list(b.r) + list(b.wx):
                if not (t[0] == "E" and t[1] == eng):
                    deps.append(t)
        for b in pwrites:
            if b.w is not None:
                t = b.w
                if not (t[0] == "E" and t[1] == eng):
                    deps.append(t)
            for t in b.r:
                if not (t[0] == "E" and t[1] == eng):
                    deps.append(t)
        seen = self.seen[eng]
        best = {}
        for t in deps:
            key = (t[0], t[1])
            if seen.get(key, -1) >= t[2]:
                continue
            if best.get(key, -1) < t[2]:
                best[key] = t[2]
        out = []
        for key, v in best.items():
            seen[key] = v
            out.append((key[0], key[1], v))
            if key[0] == "E":
                self.marked[key[1]].add(v)
        return out

    def _commit(self, tok, reads, writes, pwrites=()):
        for b in pwrites:
            b.wx.append(tok)
        for b in reads:
            if len(b.r) > 24:
                last = {}
                for t in b.r:
                    k = (t[0], t[1])
                    if last.get(k, -1) < t[2]:
                        last[k] = t[2]
                b.r = [(k[0], k[1], v) for k, v in last.items()]
            b.r.append(tok)
        for b in writes:
            b.w = tok
            b.wx = []
            b.r = []

    def op(self, eng, fn, reads=(), writes=(), pw=()):
        waits = self._deps(eng, reads, writes, pw)
        seq = len(self.ops[eng])
        tok = ("E", eng, seq)
        self.ops[eng].append((waits, fn, "C", None))
        self._commit(tok, reads, writes, pw)
        return tok

    def dma(self, eng, fn, reads=(), writes=(), semb=None):
        if semb is None:
            semb = writes[0] if writes else reads[0]
        if semb.dsem is None:
            if semb.name not in self.dsem_names:
                self.dsem_names[semb.name] = self.ndsem
                self.ndsem += 1
            semb.dsem = self.dsem_names[semb.name]
        waits = self._deps(eng, reads, writes)
        semb.dcnt = self.dsem_final.get(semb.dsem, 0) + 16
        tok = ("D", semb.dsem, semb.dcnt)
        self.dsem_final[semb.dsem] = semb.dcnt
        self.ops[eng].append((waits, fn, "D", semb.dsem))
        self._commit(tok, reads, writes)
        return tok

    def alias(self, new_bufs, old_bufs):
        toks = []
        for b in old_bufs:
            if b.w is not None:
                toks.append(b.w)
            toks.extend(b.wx)
            toks.extend(b.r)
        last = {}
        for t in toks:
            k = (t[0], t[1])
            if last.get(k, -1) < t[2]:
                last[k] = t[2]
        toks = [(k[0], k[1], v) for k, v in last.items()]
        for nb in new_bufs:
            nb.r = list(nb.r) + toks

    def emit(self):
        nc = self.nc
        with ExitStack() as es:
            esem = {e: es.enter_context(nc.semaphore("sem_" + e)) for e in ENGS}
            dsem = [es.enter_context(nc.semaphore("dsem%d" % i)) for i in range(self.ndsem)]
            block = es.enter_context(nc.Block())
            mcount = {}
            for e in ENGS:
                ms = sorted(self.marked[e])
                mcount[e] = {s: i + 1 for i, s in enumerate(ms)}

            def run(e, engobj):
                mk = self.marked[e]
                for seq, (waits, fn, kind, extra) in enumerate(self.ops[e]):
                    for (k, a, v) in waits:
                        if k == "E":
                            engobj.wait_ge(esem[a], mcount[a][v])
                        else:
                            engobj.wait_ge(dsem[a], v)
                    ins = fn(engobj)
                    if kind == "D":
                        ins.then_inc(dsem[extra], 16)
                    elif seq in mk:
                        ins.then_inc(esem[e], 1)
                if e == "sp":
                    for i, cnt in self.dsem_final.items():
                        engobj.wait_ge(dsem[i], cnt)

            @block.tensor
            def _(eng):
                run("pe", eng)

            @block.scalar
            def _(eng):
                run("act", eng)

            @block.vector
            def _(eng):
                run("dve", eng)

            @block.gpsimd
            def _(eng):
                run("pool", eng)

            @block.sync
            def _(eng):
                run("sp", eng)


def _row_start(r):
    return min(max(r - 4, 0), 24)


def _col_start(c):
    return min(max(c - 8, 0), 48)


BIAS_TILES = [(5, 3), (5, 4), (5, 5), (5, 6), (5, 7)] + [(0, k) for k in range(4)] + [(1, k) for k in range(4)] \
    + [(14, k) for k in range(12, 16)] + [(15, k) for k in range(12, 16)]


def key_tiles(j):
    if j == 0:
        return [0, 1, 2, 3], 5
    if j == 1:
        return [0, 1, 2, 3], 9
    if j == 14:
        return [12, 13, 14, 15], 13
    if j == 15:
        return [12, 13, 14, 15], 17
    return [j - 2, j - 1, j, j + 1, j + 2], 0


def _bias_index():
    idx = np.full((21, 128, 128), 15 * 31, dtype=np.int64)
    for t, (j, jk) in enumerate(BIAS_TILES):
        for qr in range(2):
            r = 2 * j + qr
            rs = _row_start(r)
            for kr in range(2):
                r2 = 2 * jk + kr
                if not (rs <= r2 < rs + 8):
                    continue
                for c in range(64):
                    cs = _col_start(c)
                    c2 = np.arange(cs, cs + 16)
                    idx[t, kr * 64 + c2, qr * 64 + c] = (r2 - r + 7) * 31 + (c2 - c + 15)
    return idx


_CONST_CACHE = {}


def _constants():
    if _CONST_CACHE:
        return _CONST_CACHE
    c = {}
    c["ident"] = np.eye(128, dtype=np.float32)
    c["iota"] = np.tile(np.arange(128, dtype=np.float32)[None, :], (128, 1))
    t = np.arange(L)
    row = (t // 64).astype(np.float32)
    col = (t % 64).astype(np.float32)
    inv = (100.0 ** (-np.arange(16, dtype=np.float32) / 16)).astype(np.float32)
    ang = np.concatenate([row[:, None] * inv, col[:, None] * inv], axis=-1).astype(np.float32)
    c["ropec"] = np.ascontiguousarray(np.cos(ang).astype(np.float32).reshape(16, 128, 32).transpose(1, 0, 2))
    c["ropes"] = np.ascontiguousarray(np.sin(ang).astype(np.float32).reshape(16, 128, 32).transpose(1, 0, 2))
    def dft(n, scale):
        k = np.arange(n, dtype=np.int64)
        m = (k[:, None] * k[None, :]) % n
        a = 2.0 * np.pi * m.astype(np.float64) / n
        return (np.cos(a) * scale).astype(np.float32), (-np.sin(a) * scale).astype(np.float32)
    c["dftc"], c["dftns"] = dft(L, 1.0 / math.sqrt(L * 64))
    c["dftc_c"], c["dftns_c"] = dft(LC, 1.0 / math.sqrt(LC * 64))
    for k_ in BF16_INPUTS:
        c[k_] = np.ascontiguousarray(c[k_].astype(ml_dtypes.bfloat16))
    c64, ns64 = dft(64, 1.0)
    z = np.zeros((128, 128), np.float32)
    z[:64, :64] = c64
    z[64:, 64:] = c64
    c["c64blk"] = z.copy()
    z[:64, :64] = -ns64
    z[64:, 64:] = -ns64
    c["s64blk"] = z.copy()
    sel = np.zeros((16, 16, 128), np.float32)
    for e in range(16):
        sel[e, e, :] = 1.0
    c["sel"] = sel.reshape(16, 16 * 128)
    c["bias_idx"] = _bias_index()
    _CONST_CACHE.update(c)
    return c


def _prep_shared(inp):
    cst = _constants()
    f = lambda a: np.ascontiguousarray(np.asarray(a, dtype=np.float32))
    m = {}
    for k in ("ident", "iota", "ropec", "ropes", "dftc", "dftns", "dftc_c", "dftns_c", "c64blk", "s64blk", "sel"):
        m[k] = cst[k]
    m["w_ada"] = f(inp["w_ada"])
    m["b_adaT"] = f(np.asarray(inp["b_ada"]).reshape(DEPTH, 48, 128).transpose(0, 2, 1))
    m["n1gT"] = f(np.asarray(inp["norm1_g"]).reshape(DEPTH, 8, 128).transpose(0, 2, 1))
    m["n2gT"] = f(np.asarray(inp["norm2_g"]).reshape(DEPTH, 8, 128).transpose(0, 2, 1))
    m["w_in"] = f(inp["w_in"])
    m["w_out"] = f(inp["w_out"])
    m["qng"] = f(inp["q_norm_g"])
    m["kng"] = f(inp["k_norm_g"])
    rpb = np.asarray(inp["rpb"], dtype=np.float32).reshape(DEPTH, 8, 15 * 31)
    rpbp = np.concatenate([rpb, np.full((DEPTH, 8, 1), NEG, np.float32)], axis=-1)
    bt = rpbp[:, :, cst["bias_idx"]]
    m["bt"] = f(bt.transpose(0, 1, 3, 2, 4))
    lre = np.asarray(inp["s5_lam_re"], np.float32)
    lim = np.asarray(inp["s5_lam_im"], np.float32)
    lst = np.asarray(inp["s5_log_step"], np.float32)
    sm = lambda a: a.reshape(DEPTH, 2, 8, 2, 64).transpose(0, 1, 3, 4, 2).reshape(DEPTH, 2, 128, 8)
    lsr = np.repeat(lst[..., None], 64, axis=-1)
    m["s5_sm"] = f(np.stack([sm(lre), sm(lim), sm(lsr)], axis=3))
    m["s5_bc"] = f(np.stack([lre.reshape(DEPTH, 2, 1024), lim.reshape(DEPTH, 2, 1024),
                             lsr.reshape(DEPTH, 2, 1024)], axis=2))
    bre = np.asarray(inp["s5_b_re"], np.float32)
    bim = np.asarray(inp["s5_b_im"], np.float32)
    cre = np.asarray(inp["s5_c_re"], np.float32)
    cim = np.asarray(inp["s5_c_im"], np.float32)
    bz = np.zeros((DEPTH, 2, 2, 128, 8, 128), np.float32)
    cz = np.zeros((DEPTH, 2, 2, 128, 8, 128), np.float32)
    for g in range(16):
        st, two, ch0 = g // 2, g % 2, (g % 8) * 16
        bz[:, :, 0, ch0:ch0 + 16, st, two * 64:(two + 1) * 64] = bre[:, :, g].transpose(0, 1, 3, 2)
        bz[:, :, 1, ch0:ch0 + 16, st, two * 64:(two + 1) * 64] = bim[:, :, g].transpose(0, 1, 3, 2)
        cz[:, :, 0, two * 64:(two + 1) * 64, st, ch0:ch0 + 16] = cre[:, :, g].transpose(0, 1, 3, 2)
        cz[:, :, 1, two * 64:(two + 1) * 64, st, ch0:ch0 + 16] = cim[:, :, g].transpose(0, 1, 3, 2)
    m["s5_bz"] = bz
    m["s5_cz"] = cz
    m["s5_dT"] = f(np.asarray(inp["s5_d"]).reshape(DEPTH, 2, 128).transpose(0, 2, 1))
    m["w_glu"] = f(inp["w_glu"])
    m["b_gluT"] = f(np.asarray(inp["b_glu"]).reshape(DEPTH, 2, 128).transpose(0, 2, 1))
    wf = np.asarray(inp["w_fnet"], np.float32)
    wfz = np.zeros((DEPTH, 2, 128, 256), np.float32)
    for h in range(4):
        kt, hh = h // 2, h % 2
        wfz[:, kt, hh * 64:(hh + 1) * 64, h * 64:(h + 1) * 64] = wf[:, h]
    m["w_fz"] = wfz
    m["w_router"] = f(np.asarray(inp["w_router"]).reshape(8, 128, 16).transpose(1, 0, 2))
    m["b_router"] = f(inp["b_router"])
    m["w_gate"] = f(inp["w_gate"])
    m["w_up"] = f(inp["w_up"])
    m["w_down"] = f(inp["w_down"])
    return m


GROUPS = [(g * 512, 512) for g in range(4)] + [(2048, 256)]


def build_program(shapes, n_batch=2, layers=(0, 1), dbg=None, stop_after=None):
    nc = bass.Bass("TRN2", target_bir_lowering=False)
    P = Prog(nc)
    dr = {}
    for name, shp in shapes.items():
        dr[name] = nc.dram_tensor(name, list(shp), BF16 if name in BF16_INPUTS else F32, kind="ExternalInput").ap()
    out_d = nc.dram_tensor("out", [n_batch, L, D], F32, kind="ExternalOutput").ap()
    dbg_d = {}
    if dbg:
        for name, (shp, dt) in dbg.items():
            dbg_d[name] = nc.dram_tensor("dbg_" + name, list(shp), dt, kind="ExternalOutput").ap()

    class Stop(Exception):
        pass

    es = ExitStack()
    with es:
        ARENA_BYTES = 212000
        arena_t = es.enter_context(nc.sbuf_tensor("arena", [128, ARENA_BYTES // 4], F32))
        ps_t = [es.enter_context(nc.psum_tensor("ps%d" % i, [128, 512], F32)) for i in range(8)]
        PS = [t[:, :] for t in ps_t]
        PSH = [t[:, :].bitcast(BF16) for t in ps_t]
        PSb = [Buf("ps%d" % i) for i in range(8)]

        def view(off, shape, dt):
            n = int(np.prod(shape))
            assert off % 4 == 0
            if dt == F32:
                v = arena_t[:, off // 4: off // 4 + n]
                nb = n * 4
            else:
                assert n % 2 == 0
                v = arena_t[:, off // 4: off // 4 + n // 2].bitcast(BF16)
                nb = n * 2
            if len(shape) == 2:
                v = v.rearrange("p (a b) -> p a b", a=shape[0])
            elif len(shape) == 3:
                v = v.rearrange("p (a b c) -> p a b c", a=shape[0], b=shape[1])
            return v, nb

        class Alloc:
            def __init__(self, base, limit):
                self.off = base
                self.limit = limit

            def take(self, shape, dt):
                v, nb = view(self.off, shape, dt)
                self.off += (nb + 3) // 4 * 4
                assert self.off <= self.limit, (self.off, self.limit)
                return v

        pers = Alloc(0, ARENA_BYTES)
        X = pers.take([NT, D], F32)
        Xb = [Buf("x%d" % i) for i in range(NT)]
        HT_OFF = pers.off
        HT = pers.take([8, T], BF16)
        HT_END = pers.off
        HTb = [Buf("ht%d" % i) for i in range(NT)]
        ident = pers.take([128], F32); b_ident = Buf("ident")
        identb = pers.take([128], BF16); b_identb = Buf("identb")
        ones_f = pers.take([128], F32); b_ones = Buf("ones")
        ones_b = pers.take([128], BF16)
        iota = pers.take([128], F32); b_iota = Buf("iota")
        ropec = pers.take([16, 32], F32); ropes = pers.take([16, 32], F32); b_rope = Buf("rope")
        modT = pers.take([DEPTH * 48, 3], F32); b_modT = Buf("modT")
        n1g = pers.take([DEPTH, 8], F32); n2g = pers.take([DEPTH, 8], F32); b_ng = Buf("ng")
        AB = pers.take([2, 2, 8], F32); b_AB = Buf("AB")
        ssq = pers.take([NT], F32); b_ssq = Buf("ssq")
        rstd = pers.take([NT], F32); b_rstd = Buf("rstd")
        ccol = pers.take([4], F32); b_ccol = Buf("ccol")
        GB = pers.take([2, D], F32); b_GB = Buf("GB")
        diag = pers.take([128], F32); b_diag = Buf("diag")
        PH_BASE = pers.off
        PH_LIMIT = ARENA_BYTES
        state = dict(old=[], cur=[])

        def new_phase():
            state["old"] = state["old"] + state["cur"]
            summ = Buf("summ")
            P.alias([summ], state["old"])
            state["old"] = [summ]
            state["cur"] = []
            state["extra"] = []
            return Alloc(PH_BASE, PH_LIMIT)

        def pbuf(name, extra_old=()):
            b = Buf(name)
            P.alias([b], state["old"] + list(extra_old) + list(state.get("extra", [])))
            state["cur"].append(b)
            return b

        def mm(out, lhsT, rhs, start, stop, reads, writes):
            P.op("pe", lambda e: e.matmul(out=out, lhsT=lhsT, rhs=rhs, start=start, stop=stop), reads, writes)

        def tr(out, in_, idn, reads, writes):
            P.op("pe", lambda e: e.transpose(out=out, in_=in_, identity=idn), reads, writes)

        def act(out, in_, func, reads, writes, bias=None, scale=None, accum=None, pw=()):
            kw = {}
            if bias is not None:
                kw["bias"] = bias
            if scale is not None:
                kw["scale"] = scale
            if accum is not None:
                kw["accum_out"] = accum
            P.op("act", lambda e: e.activation(out=out, in_=in_, func=func, **kw), reads, writes, pw)

        def tt(eng, out, in0, in1, op, reads, writes):
            P.op(eng, lambda e: e.tensor_tensor(out=out, in0=in0, in1=in1, op=op), reads, writes)

        def ts(eng, out, in0, s1, op0, reads, writes, s2=None, op1=None, pw=()):
            if op1 is None:
                P.op(eng, lambda e: e.tensor_scalar(out=out, in0=in0, scalar1=s1, scalar2=None, op0=op0), reads, writes, pw)
            else:
                P.op(eng, lambda e: e.tensor_scalar(out=out, in0=in0, scalar1=s1, scalar2=s2, op0=op0, op1=op1), reads, writes, pw)

        def stt(eng, out, in0, scalar, in1, op0, op1, reads, writes):
            P.op(eng, lambda e: e.scalar_tensor_tensor(out=out, in0=in0, scalar=scalar, in1=in1, op0=op0, op1=op1), reads, writes)

        def red(out, in_, op, reads, writes):
            P.op("dve", lambda e: e.tensor_reduce(out=out, in_=in_, axis=AX.X, op=op), reads, writes)

        def recip(out, in_, reads, writes):
            P.op("dve", lambda e: e.reciprocal(out=out, in_=in_), reads, writes)

        def cp(eng, out, in_, reads, writes, pw=()):
            if eng == "act":
                act(out, in_, AF.Copy, reads, writes, pw=pw)
            else:
                P.op(eng, lambda e: e.tensor_copy(out=out, in_=in_), reads, writes, pw)

        def memset(eng, ap, val, writes):
            P.op(eng, lambda e: e.memset(ap, val), (), writes)

        def dma(q, out, in_, reads, writes, semb=None):
            P.dma(q, lambda e: e.dma_start(out=out, in_=in_), reads, writes, semb)

        def dump(name, src, reads):
            if name in dbg_d:
                dma("sp", dbg_d[name], src, reads, [], semb=reads[0])

        def flat(v):
            return v.rearrange("p a b -> p (a b)")

        def wview(ap2d):
            return ap2d.rearrange("(k p) n -> p k n", p=128)

        dma("sp", ident, dr["ident"][:, :], [], [b_ident])
        dma("sp", iota, dr["iota"][:, :], [], [b_iota])
        dma("sp", ropec, dr["ropec"][:, :, :], [], [b_rope])
        dma("sp", ropes, dr["ropes"][:, :, :], [], [b_rope])
        dma("sp", n1g, dr["n1gT"].rearrange("l p c -> p l c"), [], [b_ng])
        dma("sp", n2g, dr["n2gT"].rearrange("l p c -> p l c"), [], [b_ng])
        cp("dve", identb, ident, [b_ident], [b_identb])
        memset("dve", ones_f, 1.0, [b_ones])
        memset("dve", ones_b, 1.0, [b_ones])
        memset("dve", ccol[:, 0:1], EPS, [b_ccol])
        memset("dve", ccol[:, 1:2], -PI, [b_ccol])
        eps_col = ccol[:, 0:1]
        negpi_col = ccol[:, 1:2]

        ph = new_phase()
        cT = ph.take([8, 3], F32); b_cT = pbuf("cT")
        sT = ph.take([8, 3], BF16); b_sT = pbuf("sT")
        badaT = ph.take([DEPTH, 48], F32); b_bada = pbuf("bada")
        wch = [ph.take([8, 512], BF16) for _ in range(2)]
        b_wch = [pbuf("wch%d" % i) for i in range(2)]
        dma("sp", cT, dr["crowT"][:, :, :], [], [b_cT])
        dma("sp", badaT, dr["b_adaT"].rearrange("l p c -> p l c"), [], [b_bada])
        act(sT, cT, AF.Silu, [b_cT], [b_sT])
        for l in range(DEPTH):
            wv = wview(dr["w_ada"][l])
            for cc in range(12):
                s = (l * 12 + cc) % 2
                dma("pool", wch[s], wv[:, :, cc * 512:(cc + 1) * 512], [], [b_wch[s]])
                for j in range(4):
                    col = (cc * 4 + j) * 3
                    for k in range(8):
                        mm(PS[0][:, col:col + 3], wch[s][:, k, j * 128:(j + 1) * 128], sT[:, k, :], k == 0, k == 7,
                           [b_wch[s], b_sT], [PSb[0]])
            tt("dve", modT[:, l * 48:(l + 1) * 48, :], PS[0][:, 0:144].rearrange("p (c r) -> p c r", r=3),
               badaT[:, l, :].unsqueeze(2).to_broadcast([128, 48, 3]), ALU.add, [PSb[0], b_bada], [b_modT])
        dump("modT", modT, [b_modT])

        s5_scr = {}

        def s5_scratch(l, d):
            if (l, d) not in s5_scr:
                a = nc.dram_tensor("s5scrA_%d_%d" % (l, d), [128, 6144], BF16, kind="Internal").ap()
                b = nc.dram_tensor("s5scrB_%d_%d" % (l, d), [128, 1056], F32, kind="Internal").ap()
                s5_scr[(l, d)] = dict(A=a, B=b, bA1=Buf("s5A1"), bA2=Buf("s5A2"), bB1=Buf("s5B1"), bB2=Buf("s5B2"))
            return s5_scr[(l, d)]

        def modcol(l, vec, chunk, row):
            i = l * 48 + vec * 8 + chunk
            return modT[:, i, row:row + 1]

        try:
            for bi in range(n_batch):
                for i in range(NT):
                    if i < 2:
                        src = dr["ctx"][bi, i * 128:(i + 1) * 128, :]
                    else:
                        src = dr["x"][bi, (i - 2) * 128:(i - 1) * 128, :]
                    dma("sp", X[:, i, :], src, [], [Xb[i]])

                for l in layers:
                    first = (bi == 0 and l == layers[0])

                    def dmp(name, src, reads):
                        if first:
                            dump(name, src, reads)

                    def norm_to_HT(gT, vshift, vscale, ph, router=None):
                        junk = ph.take([D], BF16); b_junk = pbuf("junk")
                        xs = [ph.take([D], F32) for _ in range(2)]
                        b_xs = [pbuf("xs%d" % i) for i in range(2)]
                        for r_i, row in enumerate((bi, 2)):
                            for c in range(8):
                                ts("dve", AB[:, r_i, 0, c:c + 1], modcol(l, vscale, c, row), 1.0, ALU.add,
                                   [b_modT, b_ng], [b_AB], s2=gT[:, l, c:c + 1], op1=ALU.mult)
                                cp("dve", AB[:, r_i, 1, c:c + 1], modcol(l, vshift, c, row), [b_modT], [b_AB])
                        for i in range(NT):
                            act(junk, X[:, i, :], AF.Square, [Xb[i]], [b_junk, b_ssq], accum=ssq[:, i:i + 1])
                        act(rstd, ssq, AF.Sqrt, [b_ssq, b_ccol], [b_rstd], bias=eps_col, scale=1.0 / D)
                        recip(rstd, rstd, [b_rstd], [b_rstd])
                        def mk_xs(i):
                            ts("dve", xs[i % 2], X[:, i, :], rstd[:, i:i + 1], ALU.mult, [Xb[i], b_rstd], [b_xs[i % 2]])

                        mk_xs(0)
                        for i in range(NT):
                            s = i % 2
                            r_i = 1 if i < 2 else 0
                            pb = (i % 2) * 2
                            for c in range(8):
                                bk = pb + c // 4
                                tr(PS[bk][:, (c % 4) * 128:(c % 4 + 1) * 128], xs[s][:, c * 128:(c + 1) * 128], ident,
                                   [b_xs[s], b_ident], [PSb[bk]])
                            if i + 1 < NT:
                                mk_xs(i + 1)
                            if router is not None:
                                router(i, pb, r_i)
                                continue
                            for c in range(8):
                                bk = pb + c // 4
                                src_ = PS[bk][:, (c % 4) * 128:(c % 4 + 1) * 128]
                                dst_ = HT[:, c, i * 128:(i + 1) * 128]
                                wr_, pw_ = ([HTb[i]], ()) if c == 0 else ([], [HTb[i]])
                                if c < 4:
                                    act(dst_, src_, AF.Identity, [PSb[bk], b_AB], wr_,
                                        bias=AB[:, r_i, 1, c:c + 1], scale=AB[:, r_i, 0, c:c + 1], pw=pw_)
                                else:
                                    ts("dve", dst_, src_, AB[:, r_i, 0, c:c + 1], ALU.mult, [PSb[bk], b_AB], wr_,
                                       s2=AB[:, r_i, 1, c:c + 1], op1=ALU.add, pw=pw_)

                    def make_gb(vec):
                        for r_i, row in enumerate((bi, 2)):
                            for c in range(8):
                                ts("dve", diag, ident, modcol(l, vec, c, row), ALU.mult, [b_ident, b_modT], [b_diag])
                                mm(PS[7][:, 0:128], ones_f, diag, True, True, [b_ones, b_diag], [PSb[7]])
                                cp("act", GB[:, r_i, c * 128:(c + 1) * 128], PS[7][:, 0:128], [PSb[7]], [b_GB])

                    def wout_partial(srcT, b_src, nk, wo, b_wo, tmp, b_tmp):
                        for i in range(NT):
                            r_i = 1 if i < 2 else 0
                            for half in range(2):
                                bk = 4 + (i * 2 + half) % 4
                                for kk in range(nk):
                                    mm(PS[bk], srcT[:, kk, i * 128:(i + 1) * 128], wo[:, kk, half * 512:(half + 1) * 512],
                                       kk == 0, kk == nk - 1, [b_src, b_wo], [PSb[bk]])
                                s = (i * 2 + half) % 2
                                tt("dve", tmp[s], PS[bk], GB[:, r_i, half * 512:(half + 1) * 512], ALU.mult,
                                   [PSb[bk], b_GB], [b_tmp[s]])
                                tt("pool", X[:, i, half * 512:(half + 1) * 512], X[:, i, half * 512:(half + 1) * 512], tmp[s],
                                   ALU.add, [b_tmp[s], Xb[i]], [Xb[i]])

                    def proj_fm(dstT, b_dst, wchunk, b_w, ncol_tiles):
                        n = 0
                        for (t0, tn) in GROUPS:
                            tiles = list(range(t0 // 128, (t0 + tn) // 128))
                            for ct in range(ncol_tiles):
                                bk = n % 4
                                n += 1
                                for k in range(8):
                                    mm(PS[bk][:, 0:tn], wchunk[:, k, ct * 128:(ct + 1) * 128], HT[:, k, t0:t0 + tn], k == 0, k == 7,
                                       [b_w] + [HTb[i] for i in tiles], [PSb[bk]])
                                cp("act", dstT[:, ct, t0:t0 + tn], PS[bk][:, 0:tn], [PSb[bk]], [b_dst])

                    ph = new_phase()
                    norm_to_HT(n1g, 0, 1, ph)
                    make_gb(2)
                    dmp("ht1", HT, HTb)
                    if stop_after == "norm1":
                        raise Stop()

                    ph = new_phase()
                    win = ph.take([8, 256], BF16); b_win = pbuf("win")
                    uT = ph.take([2, T], BF16); b_uT = pbuf("uT")
                    dma("pool", win, wview(dr["w_in"][l])[:, :, 1536:1792], [], [b_win])
                    proj_fm(uT, b_uT, win, b_win, 2)
                    dmp("uT", uT, [b_uT])
                    y1T = ph.take([2, T], BF16); b_y1T = pbuf("y1T")
                    wglu = ph.take([2, 256], BF16); b_wglu = pbuf("wglu")
                    dma("pool", wglu, wview(dr["w_glu"][l]), [], [b_wglu])
                    bglu = ph.take([2], F32); dsk = ph.take([2], F32); b_sp = pbuf("s5small")
                    dma("sp", bglu, dr["b_gluT"][l], [], [b_sp])
                    dma("sp", dsk, dr["s5_dT"][l], [], [b_sp])
                    wo_s = ph.take([2, D], BF16); b_wo_s = pbuf("wo_s")
                    dma("pool", wo_s, wview(dr["w_out"][l])[:, 4:6, :], [], [b_wo_s])
                    bz_off = ph.off
                    bzr = ph.take([8, 128], BF16); bzi = ph.take([8, 128], BF16); b_bz = pbuf("bz")
                    czr = ph.take([8, 128], BF16); czi = ph.take([8, 128], BF16); b_cz = pbuf("cz")
                    bzcz_flat = view(bz_off, [4096], BF16)[0]
                    assert ph.off == bz_off + 4 * 2048
                    sm = ph.take([3, 8], F32); b_sm = pbuf("sm")
                    smv = ph.take([40, 8], F32); b_smv = pbuf("smv")
                    dgd = ph.take([2, 128], BF16); b_dgd = pbuf("dgd")
                    carry = ph.take([2, 8], F32); b_carry = pbuf("carry")
                    ytmp = ph.take([2, 128], F32); b_ytmp = pbuf("ytmp")
                    SC = [ph.take([8, 128], F32) for _ in range(11)]
                    b_SC = [pbuf("sc%d" % i) for i in range(11)]
                    def arena_pair(v0):
                        off = v0.offset - arena_t[:, :].offset
                        return arena_t[:, off:off + 2048].rearrange("p (t a b) -> p t a b", t=2, a=8)

                    def halves(v):
                        f = flat(v).bitcast(BF16)
                        return [f[:, 0:1024].rearrange("p (a b) -> p a b", a=8), f[:, 1024:2048].rearrange("p (a b) -> p a b", a=8)]
                    GR, GI, RT0, ZR, ZI = SC[0:5]
                    b_GR, b_GI, b_RT0, b_ZR, b_ZI = b_SC[0:5]
                    BUrb, BUib = halves(SC[5]); b_BUb = b_SC[5]
                    COSb, SINb = halves(SC[6]); b_tabb = b_SC[6]
                    p1, p2 = halves(SC[7]); b_pp = b_SC[7]
                    grb, gib = halves(SC[8]); b_gb = b_SC[8]
                    HRb, HIb = halves(SC[9]); b_Hb = b_SC[9]
                    p3, p4 = halves(SC[10]); b_pq = b_SC[10]
                    sig = [flat(ZR)[:, 0:512], flat(ZR)[:, 512:1024]]; b_sig = b_ZR
                    wtmp = [flat(ZI)[:, 0:512], flat(ZI)[:, 512:1024]]; b_wt5 = b_ZI
                    c127 = ph.take([2, 8], F32); b_c127 = pbuf("c127")

                    def sincos(o_sin, o_cos, th, t1, t2, t3, R_, W_):
                        RW = R_ + W_
                        t1i = t1.bitcast(I32)
                        for (o_, shift) in ((o_sin, 0.0), (o_cos, 0.5 * PI)):
                            if shift == 0.0:
                                src = th
                            else:
                                ts("dve", t3, th, shift, ALU.add, RW, W_)
                                src = t3
                            ts("dve", t1i, src, 1.0 / (2 * PI), ALU.mult, RW, W_)
                            cp("dve", t2, t1i, W_, W_)
                            stt("dve", t3, t2, -2 * PI, src, ALU.mult, ALU.add, RW, W_)
                            ts("dve", t3, t3, -PI, ALU.max, W_, W_, s2=PI, op1=ALU.min)
                            act(o_, t3, AF.Sin, W_, W_)

                    def coef_math(a, b, ls, tmp, R_, W_, extra=None):
                        dlt, rr, th, sn, cs, t1, t2, t3 = tmp
                        RW = R_ + W_
                        act(dlt, ls, AF.Exp, R_, W_)
                        tt("dve", rr, dlt, a, ALU.mult, RW, W_)
                        if extra is not None:
                            act(extra[0], rr, AF.Exp, W_, W_, scale=128.0)
                        act(rr, rr, AF.Exp, W_, W_)
                        tt("dve", th, dlt, b, ALU.mult, RW, W_)
                        if extra is not None:
                            ts("dve", extra[1], th, 128.0, ALU.mult, W_, W_)
                        sincos(sn, cs, th, t1, t2, t3, W_, W_)
                        tt("dve", cs, cs, rr, ALU.mult, W_, W_)
                        tt("dve", sn, sn, rr, ALU.mult, W_, W_)
                        tt("dve", t1, a, a, ALU.mult, RW, W_)
                        tt("dve", t2, b, b, ALU.mult, RW, W_)
                        tt("dve", t1, t1, t2, ALU.add, W_, W_)
                        recip(t1, t1, W_, W_)
                        ts("dve", t2, cs, -1.0, ALU.add, W_, W_)
                        tt("dve", t3, t2, a, ALU.mult, RW, W_)
                        tt("dve", dlt, sn, b, ALU.mult, RW, W_)
                        tt("dve", t3, t3, dlt, ALU.add, W_, W_)
                        tt("dve", t3, t3, t1, ALU.mult, W_, W_)
                        tt("dve", dlt, sn, a, ALU.mult, RW, W_)
                        tt("dve", t2, t2, b, ALU.mult, RW, W_)
                        tt("dve", dlt, dlt, t2, ALU.subtract, W_, W_)
                        tt("dve", dlt, dlt, t1, ALU.mult, W_, W_)
                        return dict(theta=th, r=rr, lbr=cs, lbi=sn, cr=t3, ci=dlt)

                    for ct in range(2):
                        ts("dve", dgd[:, ct, :], ident, dsk[:, ct:ct + 1], ALU.mult, [b_ident, b_sp], [b_dgd])
                    for d in (1, 0):
                        rev = (d == 1)
                        allb = list(b_SC)
                        W_ = [b_smv]
                        KA = smv[:, 16:18, :]; KB = smv[:, 18:20, :]; fa = smv[:, 20:22, :]; fb = smv[:, 22:24, :]
                        sc_ = s5_scratch(l, d)
                        tab_flat = flat(SC[6]).bitcast(BF16)
                        kab_flat = smv[:, 16:20, :].rearrange('p a b -> p (a b)')
                        if bi == 0:
                            for q_, slot in enumerate((0, 1, 2)):
                                dma("sp", flat(SC[slot]), dr["s5_bc"][l, d, q_].partition_broadcast(128), [], allb)
                            o = coef_math(flat(SC[0]), flat(SC[1]), flat(SC[2]), [flat(SC[i]) for i in range(3, 11)], [], allb)
                            zre, zim, w1, w2 = flat(SC[0]), flat(SC[1]), flat(SC[2]), flat(SC[4])
                            dma("sp", zre, dr["s5_bz"][l, d, 0].rearrange("p a b -> p (a b)"), [], allb)
                            dma("sp", zim, dr["s5_bz"][l, d, 1].rearrange("p a b -> p (a b)"), [], allb)
                            cr, ci = o["cr"], o["ci"]
                            tt("dve", w1, zre, cr, ALU.mult, allb, allb)
                            tt("dve", w2, zim, ci, ALU.mult, allb, allb)
                            tt("dve", flat(bzr), w1, w2, ALU.subtract, allb, [b_bz])
                            tt("dve", w1, zre, ci, ALU.mult, allb, allb)
                            tt("dve", w2, zim, cr, ALU.mult, allb, allb)
                            tt("dve", flat(bzi), w1, w2, ALU.add, allb, [b_bz])
                            dma("pool", czr, dr["s5_cz"][l, d, 0], [], [b_cz])
                            dma("pool", czi, dr["s5_cz"][l, d, 1], [], [b_cz])
                            ts("dve", flat(czi), flat(czi), -1.0, ALU.mult, [b_cz], [b_cz])
                            dma("sp", sm, dr["s5_sm"][l, d], [], [b_sm])
                            W_ = [b_smv]
                            r128, th128 = smv[:, 8, :], smv[:, 9, :]
                            so = coef_math(sm[:, 0, :], sm[:, 1, :], sm[:, 2, :], [smv[:, i, :] for i in range(8)], [b_sm], W_,
                                           extra=(r128, th128))
                            s128, c128 = smv[:, 10, :], smv[:, 11, :]
                            sincos(s128, c128, th128, smv[:, 12, :], smv[:, 13, :], smv[:, 14, :], W_, W_)
                            KA = smv[:, 16:18, :]; KB = smv[:, 18:20, :]; fa = smv[:, 20:22, :]; fb = smv[:, 22:24, :]
                            tt("dve", KA[:, 0, :], so["r"], c128, ALU.mult, W_, W_)
                            cp("dve", KA[:, 1, :], KA[:, 0, :], W_, W_)
                            tt("dve", KB[:, 1, :], so["r"], s128, ALU.mult, W_, W_)
                            ts("dve", KB[:, 0, :], KB[:, 1, :], -1.0, ALU.mult, W_, W_)
                            ANG = SC[3]
                            for st in range(8):
                                ts("dve", ANG[:, st, :], iota, so["theta"][:, st:st + 1], ALU.mult, [b_iota, b_smv] + allb, allb)
                                ts("dve", RT0[:, st, :], iota, 1.0, ALU.min, [b_iota, b_smv] + allb, allb, s2=so["r"][:, st:st + 1], op1=ALU.mult)
                            sincos(flat(SC[4]), flat(SC[7]), flat(ANG), flat(SC[0]), flat(SC[1]), flat(SC[8]), allb, allb)
                            cp("act", SINb, SC[4], allb, allb)
                            cp("act", COSb, SC[7], allb, allb)
                            dma('sp', sc_['A'][:, 0:4096], bzcz_flat, [b_bz, b_cz], [sc_['bA1']])
                            dma('sp', sc_['A'][:, 4096:6144], tab_flat, [b_tabb], [sc_['bA2']])
                            dma('sp', sc_['B'][:, 0:1024], flat(RT0), [b_RT0], [sc_['bB1']])
                            dma('sp', sc_['B'][:, 1024:1056], kab_flat, [b_smv], [sc_['bB2']])
                        else:
                            dma('sp', bzcz_flat, sc_['A'][:, 0:4096], [sc_['bA1']], [b_bz, b_cz])
                            dma('sp', tab_flat, sc_['A'][:, 4096:6144], [sc_['bA2']], [b_tabb])
                            dma('sp', flat(RT0), sc_['B'][:, 0:1024], [sc_['bB1']], [b_RT0])
                            dma('sp', kab_flat, sc_['B'][:, 1024:1056], [sc_['bB2']], [b_smv])
                        G127 = arena_pair(SC[0])[:, :, :, 127]
                        G127s = arena_pair(SC[0])[:, ::-1, :, 127]
                        Z0 = arena_pair(SC[3])[:, :, :, 0]
                        b_G2 = [b_GR, b_GI]; b_Z2 = [b_ZR, b_ZI]

                        order = ([1, 0] + list(range(17, 1, -1))) if rev else list(range(18))

                        def tokslice(k):
                            t0 = k * 128
                            if rev:
                                return slice(t0 + 127, (t0 - 1) if t0 > 0 else None, -1)
                            return slice(t0, t0 + 128)

                        def bu_stage(k):
                            tok = tokslice(k)
                            for st in range(8):
                                ct = st // 4
                                for ri, bz_ in enumerate((bzr, bzi)):
                                    bk = ri * 2 + st // 4
                                    mm(PS[bk][:, (st % 4) * 128:(st % 4 + 1) * 128], bz_[:, st, :], uT[:, ct, tok], True, True,
                                       [b_bz, b_uT], [PSb[bk]])
                            for h in range(2):
                                sl = slice(4 * h, 4 * h + 4)
                                cp("act", BUrb[:, sl, :], PS[h][:, :].rearrange("p (a b) -> p a b", a=4), [PSb[h]], [b_BUb])
                                cp("act", BUib[:, sl, :], PS[2 + h][:, :].rearrange("p (a b) -> p a b", a=4), [PSb[2 + h]], [b_BUb])

                        def y_stage(n_c, k):
                            t0 = k * 128
                            for ct in range(2):
                                bk = 4 + 2 * (n_c % 2) + ct
                                if d == 1:
                                    cp("act", y1T[:, ct, t0:t0 + 128], PS[bk][:, 0:128], [PSb[bk]], [b_y1T])
                                else:
                                    tt("dve", ytmp[:, ct, :], PS[bk][:, 0:128], y1T[:, ct, t0:t0 + 128], ALU.add, [PSb[bk], b_y1T], [b_ytmp])
                                    act(y1T[:, ct, t0:t0 + 128], ytmp[:, ct, :], AF.Gelu, [b_ytmp], [b_y1T])

                        def fwd_stage():
                            tt("dve", flat(p1), flat(BUrb), flat(COSb), ALU.mult, [b_BUb, b_tabb], [b_pp])
                            tt("dve", flat(p2), flat(BUib), flat(SINb), ALU.mult, [b_BUb, b_tabb], [b_pp])
                            tt("dve", flat(ZR), flat(p1), flat(p2), ALU.add, [b_pp], [b_ZR])
                            tt("dve", flat(p3), flat(BUib), flat(COSb), ALU.mult, [b_BUb, b_tabb], [b_pq])
                            tt("dve", flat(p4), flat(BUrb), flat(SINb), ALU.mult, [b_BUb, b_tabb], [b_pq])
                            tt("dve", flat(ZI), flat(p3), flat(p4), ALU.subtract, [b_pq], [b_ZI])

                        NC = len(order)
                        bu_stage(order[0])
                        fwd_stage()
                        if NC > 1:
                            bu_stage(order[1])
                        for n_c, k in enumerate(order):
                            if n_c > 0:
                                tt("dve", fa, G127, KA, ALU.mult, b_G2 + W_, W_)
                                tt("dve", fb, G127s, KB, ALU.mult, b_G2 + W_, W_)
                                tt("dve", fa, fa, fb, ALU.add, W_, W_)
                                tt("dve", Z0, Z0, fa, ALU.add, b_Z2 + W_, b_Z2)
                            P.op("dve", lambda e: e.tensor_tensor_scan(out=flat(GR), data0=flat(RT0), data1=flat(ZR), initial=0.0,
                                                                       op0=ALU.mult, op1=ALU.add), [b_RT0, b_ZR], [b_GR])
                            P.op("dve", lambda e: e.tensor_tensor_scan(out=flat(GI), data0=flat(RT0), data1=flat(ZI), initial=0.0,
                                                                       op0=ALU.mult, op1=ALU.add), [b_RT0, b_ZI], [b_GI])
                            cp("act", grb, GR, [b_GR], [b_gb])
                            cp("act", gib, GI, [b_GI], [b_gb])
                            if n_c + 1 < NC:
                                fwd_stage()
                                if n_c + 2 < NC:
                                    bu_stage(order[n_c + 2])
                            tt("dve", flat(p1), flat(grb), flat(COSb), ALU.mult, [b_gb, b_tabb], [b_pp])
                            tt("dve", flat(p2), flat(gib), flat(SINb), ALU.mult, [b_gb, b_tabb], [b_pp])
                            tt("dve", flat(HRb), flat(p1), flat(p2), ALU.subtract, [b_pp], [b_Hb])
                            tt("dve", flat(p3), flat(grb), flat(SINb), ALU.mult, [b_gb, b_tabb], [b_pq])
                            tt("dve", flat(p4), flat(gib), flat(COSb), ALU.mult, [b_gb, b_tabb], [b_pq])
                            tt("dve", flat(HIb), flat(p3), flat(p4), ALU.add, [b_pq], [b_Hb])
                            for ct in range(2):
                                bk = 4 + 2 * (n_c % 2) + ct
                                n = 0
                                for st in range(4 * ct, 4 * ct + 4):
                                    for (cz_, hh_) in ((czr, HRb), (czi, HIb)):
                                        rhs = hh_[:, st, ::-1] if rev else hh_[:, st, :]
                                        mm(PS[bk][:, 0:128], cz_[:, st, :], rhs, n == 0, (n == 7 and d == 1), [b_cz, b_Hb], [PSb[bk]])
                                        n += 1
                                if d == 0:
                                    mm(PS[bk][:, 0:128], dgd[:, ct, :], uT[:, ct, k * 128:(k + 1) * 128], False, True, [b_dgd, b_uT], [PSb[bk]])
                            if n_c > 0:
                                y_stage(n_c - 1, order[n_c - 1])
                        y_stage(len(order) - 1, order[-1])
                    dmp("gT", y1T, [b_y1T])
                    n = 0
                    for (t0, tn) in GROUPS:
                        for co in range(2):
                            bk = n % 4
                            s = n % 2
                            n += 1
                            for k in range(2):
                                mm(PS[bk][:, 0:tn], wglu[:, k, co * 128:(co + 1) * 128], y1T[:, k, t0:t0 + tn], k == 0, k == 1,
                                   [b_wglu, b_y1T], [PSb[bk]])
                            act(sig[s][:, 0:tn], PS[bk][:, 0:tn], AF.Sigmoid, [PSb[bk], b_sp], [b_sig], bias=bglu[:, co:co + 1])
                            tt("dve", uT[:, co, t0:t0 + tn], sig[s][:, 0:tn], y1T[:, co, t0:t0 + tn], ALU.mult, [b_sig, b_y1T], [b_uT])
                    dmp("ssmT", uT, [b_uT])
                    wout_partial(uT, b_uT, 2, wo_s, b_wo_s, wtmp, [b_wt5, b_wt5])
                    if stop_after == "s5":
                        raise Stop()

                    ph = new_phase()
                    win = ph.take([8, 256], BF16); b_win = pbuf("winf")
                    ufT = ph.take([2, T], BF16); b_ufT = pbuf("ufT")
                    dma("pool", win, wview(dr["w_in"][l])[:, :, 1792:2048], [], [b_win])
                    proj_fm(ufT, b_ufT, win, b_win, 2)
                    wo_f = ph.take([2, D], BF16); b_wo_f = pbuf("wo_f")
                    dma("pool", wo_f, wview(dr["w_out"][l])[:, 6:8, :], [], [b_wo_f])
                    wfz = ph.take([2, 256], F32); b_wfz = pbuf("wfz")
                    dma("sp", wfz, dr["w_fz"][l].rearrange("k p n -> p k n"), [], [b_wfz])
                    c64 = ph.take([128], F32); s64 = ph.take([128], F32); b_c64 = pbuf("c64")
                    dma("sp", c64, dr["c64blk"][:, :], [], [b_c64])
                    dma("sp", s64, dr["s64blk"][:, :], [], [b_c64])
                    G = ph.take([2, 512], BF16); b_G = pbuf("G")
                    for kt in range(2):
                        for ti, tb in enumerate((c64, s64)):
                            mm(PS[ti][:, 0:256], tb, wfz[:, kt, :], True, True, [b_c64, b_wfz], [PSb[ti]])
                            cp("act", G[:, kt, ti * 256:(ti + 1) * 256], PS[ti][:, 0:256], [PSb[ti]], [b_G])
                    A_tok = ph.take([NT, 512], BF16); b_A = pbuf("A_tok")
                    for i in range(NT):
                        bk = i % 4
                        for kt in range(2):
                            mm(PS[bk], ufT[:, kt, i * 128:(i + 1) * 128], G[:, kt, :], kt == 0, kt == 1, [b_ufT, b_G], [PSb[bk]])
                        if i == 0:
                            cp("act", A_tok[:, i, :], PS[bk], [PSb[bk]], [b_A])
                        else:
                            cp("act" if i % 2 == 0 else "dve", A_tok[:, i, :], PS[bk], [PSb[bk]], [], pw=[b_A])
                    ring = [ph.take([4, 512], BF16) for _ in range(4)]
                    b_ring = [pbuf("ring%d" % i) for i in range(4)]
                    ctab = [ph.take([2, 256], BF16) for _ in range(2)]; b_ctab = pbuf("ctab")
                    wtmp = [ph.take([512], F32) for _ in range(2)]; b_wtmp = [pbuf("wtmpf%d" % i) for i in range(2)]
                    fouT = ufT
                    b_fouT = pbuf("fouT", extra_old=[b_ufT])
                    tabs = (dr["dftc"], dr["dftns"])
                    nring = 0
                    for lg in range(4):
                        nmm = 0
                        for ktg in range(4):
                            for ti in range(2):
                                s = nring % 4
                                nring += 1
                                src = tabs[ti].rearrange("(k p) n -> p k n", p=128)[:, ktg * 4:(ktg + 1) * 4, lg * 512:(lg + 1) * 512]
                                dma("sp", ring[s], src, [], [b_ring[s]])
                                for kk in range(4):
                                    kt = ktg * 4 + kk
                                    for ct in range(2):
                                        mm(PS[4 + ct], A_tok[:, 2 + kt, ti * 256 + ct * 128: ti * 256 + (ct + 1) * 128], ring[s][:, kk, :],
                                           nmm < 2, nmm >= 62, [b_A, b_ring[s]], [PSb[4 + ct]])
                                        nmm += 1
                        for ct in range(2):
                            cp("act", fouT[:, ct, 256 + lg * 512: 256 + (lg + 1) * 512], PS[4 + ct], [PSb[4 + ct], b_A], [b_fouT])
                    dma("sp", ctab[0], wview(dr["dftc_c"]), [], [b_ctab])
                    dma("sp", ctab[1], wview(dr["dftns_c"]), [], [b_ctab])
                    for ct in range(2):
                        n = 0
                        for ti in range(2):
                            for kt in range(2):
                                mm(PS[ct][:, 0:256], A_tok[:, kt, ti * 256 + ct * 128: ti * 256 + (ct + 1) * 128], ctab[ti][:, kt, :],
                                   n == 0, n == 3, [b_A, b_ctab], [PSb[ct]])
                                n += 1
                        cp("act", fouT[:, ct, 0:256], PS[ct][:, 0:256], [PSb[ct], b_A], [b_fouT])
                    dmp("fouT", fouT, [b_fouT])
                    wout_partial(fouT, b_fouT, 2, wo_f, b_wo_f, wtmp, b_wtmp)
                    if stop_after == "fnet":
                        raise Stop()

                    ph = new_phase()
                    wq = [ph.take([8, 512], BF16) for _ in range(2)]
                    b_wq = [pbuf("wq%d" % i) for i in range(2)]
                    qT = ph.take([4, T], BF16); kT = ph.take([4, T], BF16)
                    b_qT = pbuf("qT"); b_kT = pbuf("kT")
                    V = ph.take([NT, 8 * 65], BF16); b_V = pbuf("V")
                    memset("pool", V.rearrange("p t (h d) -> p (t h) d", d=65)[:, :, 64:65], 1.0, [b_V])
                    sq = ph.take([512], BF16); b_sq = pbuf("sq")
                    qn_off = ph.off
                    qn = [ph.take([512], F32) for _ in range(2)]; b_qn = [pbuf("qn%d" % i) for i in range(2)]
                    tsb_off = ph.off
                    tsb = ph.take([512], F32); b_tsb = pbuf("tsb")
                    qr = [ph.take([512], BF16) for _ in range(2)]; b_qr = [pbuf("qr%d" % i) for i in range(2)]
                    s8 = ph.take([2, 8], F32); b_s8 = [pbuf("s8_%d" % i) for i in range(2)]
                    Gqk = ph.take([2, 64], F32); b_Gqk = pbuf("Gqk")
                    dma("sp", Gqk[:, 0, :], dr["qng"][l].partition_broadcast(128), [], [b_Gqk])
                    dma("sp", Gqk[:, 1, :], dr["kng"][l].partition_broadcast(128), [], [b_Gqk])
                    items = [(ci, i) for ci in range(3) for i in range(NT)]
                    MMB = [0, 1, 4, 5]

                    def load_wq(ci):
                        dma("pool", wq[ci % 2], wview(dr["w_in"][l])[:, :, ci * 512:(ci + 1) * 512], [], [b_wq[ci % 2]])

                    def stA(n):
                        ci, i = items[n]
                        s = ci % 2
                        bk = MMB[n % 4]
                        for k in range(8):
                            mm(PS[bk], HT[:, k, i * 128:(i + 1) * 128], wq[s][:, k, :], k == 0, k == 7, [HTb[i], b_wq[s]], [PSb[bk]])
                        if ci == 0 and i == NT - 1:
                            load_wq(2)

                    def stB(n):
                        ci, i = items[n]
                        bk = MMB[n % 4]
                        if ci == 2:
                            cp("act", V[:, i, :].rearrange("p (h d) -> p h d", d=65)[:, :, 0:64],
                               PS[bk].rearrange("p (h d) -> p h d", d=64), [PSb[bk]], [], pw=[b_V])
                            return
                        u = n % 2
                        s8u = s8[:, u, :]
                        act(sq, PS[bk], AF.Square, [PSb[bk]], [b_sq])
                        red(s8u, sq.rearrange("p (h d) -> p h d", h=8), ALU.add, [b_sq], [b_s8[u]])
                        act(s8u, s8u, AF.Sqrt, [b_s8[u], b_ccol], [b_s8[u]], bias=eps_col, scale=1.0 / 64)
                        recip(s8u, s8u, [b_s8[u]], [b_s8[u]])
                        qn3 = qn[u].rearrange("p (h d) -> p h d", h=8)
                        tt("dve", qn3, PS[bk].rearrange("p (h d) -> p h d", h=8), s8u.unsqueeze(2).to_broadcast([128, 8, 64]),
                           ALU.mult, [PSb[bk], b_s8[u]], [b_qn[u]])
                        tt("dve", qn3, qn3, Gqk[:, ci, :].unsqueeze(1).to_broadcast([128, 8, 64]), ALU.mult, [b_qn[u], b_Gqk], [b_qn[u]])
                        qr3 = qr[u].rearrange("p (h d) -> p h d", h=8)
                        if i >= 2:
                            j = i - 2
                            cb = ropec[:, j, :].unsqueeze(1).to_broadcast([128, 16, 32])
                            sb_ = ropes[:, j, :].unsqueeze(1).to_broadcast([128, 16, 32])
                            qn4 = qn[u].rearrange("p (g d) -> p g d", g=16)
                            tt("dve", tsb.rearrange("p (g d) -> p g d", g=16), qn4, sb_, ALU.mult, [b_qn[u], b_rope], [b_tsb])
                            tt("dve", qn4, qn4, cb, ALU.mult, [b_qn[u], b_rope], [b_qn[u]])
                            ts3 = tsb.rearrange("p (h d) -> p h d", h=8)
                            tt("pool", qr3[:, :, 0:32], qn3[:, :, 0:32], ts3[:, :, 32:64], ALU.subtract, [b_qn[u], b_tsb], [b_qr[u]])
                            tt("pool", qr3[:, :, 32:64], ts3[:, :, 0:32], qn3[:, :, 32:64], ALU.add, [b_qn[u], b_tsb], [b_qr[u]])
                        else:
                            cp("pool", qr[u], qn[u], [b_qn[u]], [b_qr[u]])

                    def stC(n):
                        ci, i = items[n]
                        if ci == 2:
                            return
                        u = n % 2
                        tb = 2 + u
                        for hp in range(4):
                            tr(PSH[tb][:, hp * 128:(hp + 1) * 128], qr[u][:, hp * 128:(hp + 1) * 128], identb,
                               [b_qr[u], b_identb], [PSb[tb]])
                        dst = qT if ci == 0 else kT
                        cp("act", dst[:, :, i * 128:(i + 1) * 128], PSH[tb][:, 0:512].rearrange("p (a b) -> p a b", a=4),
                           [PSb[tb]], [b_qT if ci == 0 else b_kT])

                    load_wq(0)
                    load_wq(1)
                    NI = len(items)
                    stA(0)
                    stA(1)
                    for n in range(NI):
                        stB(n)
                        if n + 2 < NI:
                            stA(n + 2)
                        if n >= 1:
                            stC(n - 1)
                    stC(NI - 1)
                    dmp("qT", qT, [b_qT])
                    dmp("kT", kT, [b_kT])
                    hta = Alloc(HT_OFF, HT_END)
                    NBT = 3
                    BT = [hta.take([21, 128], BF16) for _ in range(NBT)]
                    b_BT = [pbuf("BT%d" % i, extra_old=HTb) for i in range(NBT)]
                    attT = hta.take([4, T], BF16); b_attT = pbuf("attT", extra_old=HTb)
                    wo_a = wq[0].rearrange("p k n -> p (k n)").rearrange("p (k n) -> p k n", k=4)
                    b_wo_a = b_wq[0]
                    dma("pool", wo_a, wview(dr["w_out"][l])[:, 0:4, :], [], [b_wo_a])
                    w1flat = wq[1].rearrange("p k n -> p (k n)")
                    PT = [w1flat[:, o_:o_ + 896].rearrange("p (a b) -> p a b", a=7) for o_ in (0, 896, 3072)]
                    b_PT = [pbuf("PT%d" % s, extra_old=[b_wq[1]]) for s in range(3)]
                    Ef = [w1flat[:, 1792:3072].bitcast(F32).rearrange("p (a b) -> p a b", a=5), view(qn_off, [5, 128], F32)[0]]
                    b_E = [pbuf("E0", extra_old=[b_wq[1]]), pbuf("E1", extra_old=b_qn)]
                    rinv = [view(tsb_off + 512 * i, [128], F32)[0] for i in range(2)]
                    b_rinv = [pbuf("rinv%d" % i, extra_old=[b_tsb]) for i in range(2)]
                    its = []
                    for hp in range(4):
                        for n_q, qi in enumerate(list(range(2, NT)) + [0, 1]):
                            for hh in range(2):
                                its.append((2 * hp + hh, qi, n_q))
                    att2 = [ph.take([128], BF16) for _ in range(2)]; b_att2 = [pbuf("att2_%d" % i) for i in range(2)]
                    rinv2 = ph.take([2, 2], F32); b_rinv2 = [pbuf("rinv2_%d" % i) for i in range(2)]

                    def it_info(n):
                        h, qi, n_h = its[n]
                        if qi >= 2:
                            kts, t0b = key_tiles(qi - 2)
                            lat = [2 + k for k in kts]
                        else:
                            lat, t0b = [], 0
                        return h, qi, n_h, lat, t0b

                    def st1(n):
                        h, qi, n_h, lat, t0b = it_info(n)
                        hp, hh = h // 2, h % 2
                        pr = slice(64 * hh, 64 * hh + 64)
                        keys = lat + [0, 1]
                        sb2 = (n % 2) * 2
                        qsl = slice(qi * 128, (qi + 1) * 128)
                        for m, kt_ in enumerate(keys):
                            bk = sb2 + m // 4
                            mm(PS[bk][:, (m % 4) * 128:(m % 4 + 1) * 128], kT[pr, hp, kt_ * 128:(kt_ + 1) * 128], qT[pr, hp, qsl],
                               True, True, [b_kT, b_qT], [PSb[bk]])

                    def st2a(n):
                        h, qi, n_h, lat, t0b = it_info(n)
                        bs = h % NBT
                        nl = len(lat)
                        nk = nl + 2
                        sb2 = (n % 2) * 2
                        pe_ = n % 2
                        pt_ = n % 3
                        if h % 2 == 0 and n_h == 1 and h + 2 < 8:
                            dma("pool", BT[(h + 2) % NBT], dr["bt"][l, h + 2], [], [b_BT[(h + 2) % NBT]])
                        if h % 2 == 0 and n_h == 0 and h >= 2:
                            dma("pool", BT[(h + 1) % NBT], dr["bt"][l, h + 1], [], [b_BT[(h + 1) % NBT]])
                        m = 0
                        while m < nk:
                            bk = sb2 + m // 4
                            if m < nl:
                                m2 = min(nl, (m // 4 + 1) * 4)
                                src = PS[bk][:, (m % 4) * 128:(m % 4) * 128 + (m2 - m) * 128].rearrange("p (a b) -> p a b", b=128)
                                stt("dve", Ef[pe_][:, m:m2, :], src, 0.125, BT[bs][:, t0b + m:t0b + m2, :], ALU.mult, ALU.add,
                                    [PSb[bk], b_BT[bs]], [b_E[pe_]])
                            else:
                                m2 = min(nk, (m // 4 + 1) * 4)
                                src = PS[bk][:, (m % 4) * 128:(m % 4) * 128 + (m2 - m) * 128].rearrange("p (a b) -> p a b", b=128)
                                act(PT[pt_][:, m:m2, :], src, AF.Exp, [PSb[bk], b_E[pe_]], [b_PT[pt_]], scale=0.125)
                            m = m2

                    def st2b(n):
                        h, qi, n_h, lat, t0b = it_info(n)
                        nl = len(lat)
                        pe_ = n % 2
                        pt_ = n % 3
                        m = 0
                        while m < nl:
                            m2 = min(nl, (m // 4 + 1) * 4)
                            act(PT[pt_][:, m:m2, :], Ef[pe_][:, m:m2, :], AF.Exp, [b_E[pe_]], [b_PT[pt_]])
                            m = m2

                    def st3(n):
                        h, qi, n_h, lat, t0b = it_info(n)
                        hh = h % 2
                        keys = lat + [0, 1]
                        nk = len(keys)
                        pi = n // 2
                        ob = 4 + pi % 2
                        ps_ = n % 3
                        for m, kt_ in enumerate(keys):
                            mm(PS[ob][:, hh * 128:hh * 128 + 65], PT[ps_][:, m, :], V[:, kt_, h * 65:(h + 1) * 65], m == 0, m == nk - 1,
                               [b_V, b_PT[ps_]], [PSb[ob]])

                    def fin1(pi):
                        hp, qi = its[2 * pi][0] // 2, its[2 * pi][1]
                        ob = 4 + pi % 2
                        u = pi % 2
                        o3 = PS[ob][:, 0:256].rearrange("p (h d) -> p h d", d=128)
                        recip(rinv2[:, u, :], o3[:, :, 64], [PSb[ob]], [b_rinv2[u]])
                        tt("dve", att2[u].rearrange("p (h d) -> p h d", d=64), o3[:, :, 0:64],
                           rinv2[:, u, :].unsqueeze(2).to_broadcast([128, 2, 64]), ALU.mult, [PSb[ob], b_rinv2[u]], [b_att2[u]])

                    def fin2(pi):
                        hp, qi = its[2 * pi][0] // 2, its[2 * pi][1]
                        u = pi % 2
                        tb = 6 + u
                        tr(PSH[tb][:, 0:128], att2[u], identb, [b_att2[u], b_identb], [PSb[tb]])
                        cp("act", attT[:, hp, qi * 128:(qi + 1) * 128], PSH[tb][:, 0:128], [PSb[tb]], [], pw=[b_attT])

                    memset("pool", attT[:, 0, 0:2], 0.0, [b_attT])
                    dma("pool", BT[0], dr["bt"][l, 0], [], [b_BT[0]])
                    dma("pool", BT[1], dr["bt"][l, 1], [], [b_BT[1]])
                    NIT = len(its)
                    st1(0)
                    st1(1)
                    st2a(0)
                    st1(2)
                    st2a(1)
                    st2b(0)
                    for n in range(NIT):
                        if n + 3 < NIT:
                            st1(n + 3)
                        if n + 1 < NIT:
                            st2b(n + 1)
                        if n + 2 < NIT:
                            st2a(n + 2)
                        st3(n)
                        if n % 2 == 1:
                            fin1(n // 2)
                            if n // 2 >= 1:
                                fin2(n // 2 - 1)
                    fin2(NIT // 2 - 1)
                    dmp("attT", attT, [b_attT])
                    b_wt = [pbuf("wtmpa%d" % i, extra_old=[b_wq[1], b_E[0]] + b_PT) for i in range(2)]
                    wout_partial_h = None
                    wtmp = [w1flat[:, 0:1024].bitcast(F32), w1flat[:, 1024:2048].bitcast(F32)]
                    wout_partial(attT, b_attT, 4, wo_a, b_wo_a, wtmp, b_wt)
                    dmp("xmix", X, Xb)
                    if stop_after == "attn":
                        raise Stop()

                    ph = new_phase()
                    for b_ in HTb:
                        P.alias([b_], [b_attT] + b_BT)
                    wgu = [None, None]; wd = [None, None]; b_wgu = [None, None]; b_wd = [None, None]
                    wgu[0] = ph.take([8, 1024], BF16); wd[0] = ph.take([4, 1024], BF16)
                    b_wgu[0] = pbuf("wgu0"); b_wd[0] = pbuf("wd0")

                    def load_expert(e):
                        s_ = e % 2
                        dma("pool", wgu[s_][:, :, 0:512], wview(dr["w_gate"][l, e]), [], [b_wgu[s_]])
                        dma("pool", wgu[s_][:, :, 512:1024], wview(dr["w_up"][l, e]), [], [b_wgu[s_]])
                        dma("pool", wd[s_], wview(dr["w_down"][l, e]), [], [b_wd[s_]])

                    load_expert(0)
                    sub_base = ph.off
                    wr = ph.take([8, 16], F32); b_wr = pbuf("wr")
                    dma("sp", wr, dr["w_router"][:, :, :], [], [b_wr])
                    h2f = [ph.take([8, 128], F32) for _ in range(2)]
                    b_h2f = [pbuf("h2f%d" % i) for i in range(2)]

                    def router(i, pb, r_i):
                        s = i % 2
                        for c in range(8):
                            bk = pb + c // 4
                            src_ = PS[bk][:, (c % 4) * 128:(c % 4 + 1) * 128]
                            wr_, pw_ = ([b_h2f[s]], ()) if c == 0 else ([], [b_h2f[s]])
                            if c < 4:
                                act(h2f[s][:, c, :], src_, AF.Identity, [PSb[bk], b_AB], wr_,
                                    bias=AB[:, r_i, 1, c:c + 1], scale=AB[:, r_i, 0, c:c + 1], pw=pw_)
                            else:
                                ts("dve", h2f[s][:, c, :], src_, AB[:, r_i, 0, c:c + 1], ALU.mult, [PSb[bk], b_AB], wr_,
                                   s2=AB[:, r_i, 1, c:c + 1], op1=ALU.add, pw=pw_)
                        cp("pool", HT[:, :, i * 128:(i + 1) * 128], h2f[s], [b_h2f[s]], [HTb[i]])
                        for c in range(8):
                            mm(PS[6][:, i * 16:(i + 1) * 16], h2f[s][:, c, :], wr[:, c, :], c == 0, c == 7, [b_h2f[s], b_wr], [PSb[6]])

                    n_before = len(state["cur"])
                    norm_to_HT(n2g, 3, 4, ph, router=router)
                    make_gb(5)
                    dmp("ht2", HT, HTb)
                    state["extra"] = [b_wr] + b_h2f + state["cur"][n_before:]
                    ph = Alloc(sub_base, PH_LIMIT)
                    wgu[1] = ph.take([8, 1024], BF16); wd[1] = ph.take([4, 1024], BF16)
                    b_wgu[1] = pbuf("wgu1"); b_wd[1] = pbuf("wd1")
                    aff = ph.take([NT, 16], F32); sel = ph.take([NT, 16], F32); w_ = ph.take([NT, 16], F32)
                    eq = ph.take([NT, 16], F32)
                    b_rt = pbuf("rt")
                    m1 = ph.take([72], F32); m2 = ph.take([72], F32); gs = ph.take([72], F32); gsel = ph.take([72], F32)
                    gmax = ph.take([NT], F32); wsum = ph.take([NT], F32)
                    brt = ph.take([16], F32); b_brt = pbuf("brt")
                    dma("sp", brt, dr["b_router"].partition_broadcast(128), [], [b_brt])
                    R = [b_rt]
                    f2 = lambda v: v.rearrange("p a b -> p (a b)")
                    v4 = lambda v: f2(v).rearrange("p (g e) -> p g e", e=4)
                    act(f2(aff), PS[6][:, 0:288], AF.Sigmoid, [PSb[6]], R)
                    dmp("aff", aff, R)
                    tt("dve", sel, aff, brt.unsqueeze(1).to_broadcast([128, NT, 16]), ALU.add, R + [b_brt], R)
                    red(m1, v4(sel), ALU.max, R, R)
                    tt("dve", v4(eq), v4(sel), m1.unsqueeze(2).to_broadcast([128, 72, 4]), ALU.is_equal, R, R)
                    stt("dve", v4(eq), v4(eq), -1.0e9, v4(sel), ALU.mult, ALU.add, R, R)
                    red(m2, v4(eq), ALU.max, R, R)
                    tt("dve", gs, m1, m2, ALU.add, R, R)
                    gs3 = gs.rearrange("p (t g) -> p t g", g=4)
                    red(gmax, gs3, ALU.max, R, R)
                    tt("dve", gsel.rearrange("p (t g) -> p t g", g=4), gs3, gmax.unsqueeze(2).to_broadcast([128, NT, 4]), ALU.is_equal, R, R)
                    tt("dve", v4(eq), v4(sel), m2.unsqueeze(2).to_broadcast([128, 72, 4]), ALU.is_ge, R, R)
                    tt("dve", v4(eq), v4(eq), gsel.unsqueeze(2).to_broadcast([128, 72, 4]), ALU.mult, R, R)
                    tt("dve", w_, aff, eq, ALU.mult, R, R)
                    red(wsum, w_, ALU.add, R, R)
                    recip(wsum, wsum, R, R)
                    tt("dve", w_, w_, wsum.unsqueeze(2).to_broadcast([128, NT, 16]), ALU.mult, R, R)
                    dmp("gates", w_, R)
                    gatesT = ph.take([T], BF16); b_gT = pbuf("gatesT")
                    for i in range(NT):
                        bk = (i // 4) % 2
                        tr(PS[bk][0:16, (i % 4) * 128:(i % 4 + 1) * 128], w_[:, i, :], ident, R + [b_ident], [PSb[bk]])
                        if i % 4 == 3 or i == NT - 1:
                            i0 = (i // 4) * 4
                            n_ = i - i0 + 1
                            cp("act", gatesT[0:16, i0 * 128:(i0 + n_) * 128], PS[bk][0:16, 0:n_ * 128], [PSb[bk]], [b_gT])
                    selc = ph.take([16 * 128], BF16); b_selc = pbuf("selc")
                    dma("pool", selc[0:16, :], dr["sel"][:, :], [], [b_selc])
                    load_expert(1)
                    h1T = [ph.take([4, 512], BF16) for _ in range(2)]; b_h1T = [pbuf("h1T%d" % i) for i in range(2)]
                    sil = [ph.take([512], F32) for _ in range(2)]; b_sil = [pbuf("sil%d" % i) for i in range(2)]
                    hmul = [ph.take([512], BF16) for _ in range(2)]; b_hmul = [pbuf("hmul%d" % i) for i in range(2)]
                    GBC = [ph.take([512], BF16) for _ in range(2)]; b_GBC = [pbuf("GBC%d" % i) for i in range(2)]
                    wtmp = [ph.take([512], F32) for _ in range(2)]; b_wtmp = [pbuf("wtmpm%d" % i) for i in range(2)]
                    cnt = dict(F=0, D=0)

                    def gateup(e, gi):
                        s = e % 2
                        t0, tn = GROUPS[gi]
                        tiles = list(range(t0 // 128, (t0 + tn) // 128))
                        hb = [HTb[i] for i in tiles]
                        gsl = (e * 5 + gi) % 2
                        mm(PS[7][:, 0:tn], selc[0:16, e * 128:(e + 1) * 128], gatesT[0:16, t0:t0 + tn], True, True,
                           [b_selc, b_gT], [PSb[7]])
                        cp("act", GBC[gsl][:, 0:tn], PS[7][:, 0:tn], [PSb[7]], [b_GBC[gsl]])
                        for fc in range(4):
                            fs = cnt["F"] % 2
                            cnt["F"] += 1
                            bg, bu = fs * 2, fs * 2 + 1
                            for k in range(8):
                                mm(PS[bg][:, 0:tn], wgu[s][:, k, fc * 128:(fc + 1) * 128], HT[:, k, t0:t0 + tn], k == 0, k == 7,
                                   [b_wgu[s]] + hb, [PSb[bg]])
                            for k in range(8):
                                mm(PS[bu][:, 0:tn], wgu[s][:, k, 512 + fc * 128:512 + (fc + 1) * 128], HT[:, k, t0:t0 + tn], k == 0, k == 7,
                                   [b_wgu[s]] + hb, [PSb[bu]])
                            act(sil[fs][:, 0:tn], PS[bg][:, 0:tn], AF.Silu, [PSb[bg]], [b_sil[fs]])
                            tt("dve", hmul[fs][:, 0:tn], sil[fs][:, 0:tn], PS[bu][:, 0:tn], ALU.mult, [b_sil[fs], PSb[bu]], [b_hmul[fs]])
                            tt("dve", h1T[gsl][:, fc, 0:tn], hmul[fs][:, 0:tn], GBC[gsl][:, 0:tn], ALU.mult,
                               [b_hmul[fs], b_GBC[gsl]], [b_h1T[gsl]])

                    def down(e, gi):
                        s = e % 2
                        t0, tn = GROUPS[gi]
                        tiles = list(range(t0 // 128, (t0 + tn) // 128))
                        gsl = (e * 5 + gi) % 2
                        for ti, i in enumerate(tiles):
                            r_i = 1 if i < 2 else 0
                            for half in range(2):
                                bk = 4 + cnt["D"] % 2
                                ws = cnt["D"] % 2
                                cnt["D"] += 1
                                for fc in range(4):
                                    mm(PS[bk], h1T[gsl][:, fc, ti * 128:(ti + 1) * 128], wd[s][:, fc, half * 512:(half + 1) * 512],
                                       fc == 0, fc == 3, [b_h1T[gsl], b_wd[s]], [PSb[bk]])
                                tt("dve", wtmp[ws], PS[bk], GB[:, r_i, half * 512:(half + 1) * 512], ALU.mult,
                                   [PSb[bk], b_GB], [b_wtmp[ws]])
                                tt("pool", X[:, i, half * 512:(half + 1) * 512], X[:, i, half * 512:(half + 1) * 512], wtmp[ws],
                                   ALU.add, [b_wtmp[ws], Xb[i]], [Xb[i]])

                    prev = None
                    for e in range(16):
                        for gi in range(len(GROUPS)):
                            gateup(e, gi)
                            if prev is not None:
                                down(*prev)
                            prev = (e, gi)
                            if gi == 0 and 1 <= e < 15:
                                load_expert(e + 1)
                    down(*prev)
                    dmp("xout", X, Xb)
                    if stop_after == "moe":
                        raise Stop()

                for i in range(2, NT):
                    dma("sp", out_d[bi, (i - 2) * 128:(i - 1) * 128, :], X[:, i, :], [Xb[i]], [], semb=Xb[i])
        except Stop:
            pass
        P.emit()
    return nc


_PROG_CACHE = {}


def _shapes(m):
    return {k: tuple(v.shape) for k, v in m.items()}


def kernel(**inputs):
    n_cores = 8
    shared = _prep_shared(inputs)
    x = np.ascontiguousarray(np.asarray(inputs["x"], dtype=np.float32))
    ctx = np.ascontiguousarray(np.asarray(inputs["ctx"], dtype=np.float32))
    c = np.asarray(inputs["c"], dtype=np.float32)
    c_ctx = np.asarray(inputs["c_ctx"], dtype=np.float32)
    in_maps = []
    for core in range(n_cores):
        m = dict(shared)
        b0 = 2 * core
        m["x"] = x[b0:b0 + 2]
        m["ctx"] = ctx[b0:b0 + 2]
        rows = np.stack([c[b0], c[b0 + 1], c_ctx], axis=0)
        m["crowT"] = np.ascontiguousarray(rows.reshape(3, 8, 128).transpose(2, 1, 0))
        in_maps.append(m)
    key = "main"
    if key not in _PROG_CACHE:
        _PROG_CACHE[key] = build_program(_shapes(in_maps[0]))
    nc = _PROG_CACHE[key]
    res = run_bass_kernel_spmd(nc, in_maps, core_ids=list(range(n_cores)))
    out = np.concatenate([np.asarray(r["out"], dtype=np.float32) for r in res.results], axis=0)
    return out
```

```python
import math
from contextlib import ExitStack
import numpy as np
import ml_dtypes
import concourse.bass as bass
import concourse.mybir as mybir
from concourse.bass_utils import run_bass_kernel_spmd

F32 = mybir.dt.float32
BF16 = mybir.dt.bfloat16
I32 = mybir.dt.int32
AF = mybir.ActivationFunctionType
ALU = mybir.AluOpType
AX = mybir.AxisListType

D = 1024
L = 2048
LC = 256
T = L + LC
NT = T // 128
DEPTH = 2
EPS = 1e-6
NEG = -30000.0
PI = math.pi

ENGS = ("pe", "act", "dve", "pool", "sp")
BF16_INPUTS = ("dftc", "dftns", "dftc_c", "dftns_c")


class Buf:
    __slots__ = ("name", "w", "wx", "r", "dsem", "dcnt")

    def __init__(self, name):
        self.name = name
        self.w = None
        self.wx = []
        self.r = []
        self.dsem = None
        self.dcnt = 0


class Prog:
    def __init__(self, nc):
        self.nc = nc
        self.ops = {e: [] for e in ENGS}
        self.seen = {e: {} for e in ENGS}
        self.marked = {e: set() for e in ENGS}
        self.ndsem = 0
        self.dsem_final = {}
        self.dsem_names = {}

    def _deps(self, eng, reads, writes, pwrites=()):
        deps = []
        for b in reads:
            if b.w is not None:
                deps.append(b.w)
            deps.extend(b.wx)
        for b in writes:
            if b.w is not None:
                t = b.w
                if not (t[0] == "E" and t[1] == eng):
                    deps.append(t)
            for t in list(b.r) + list(b.wx):
                if not (t[0] == "E" and t[1] == eng):
                    deps.append(t)
        for b in pwrites:
            if b.w is not None:
                t = b.w
                if not (t[0] == "E" and t[1] == eng):
                    deps.append(t)
            for t in b.r:
                if not (t[0] == "E" and t[1] == eng):
                    deps.append(t)
        seen = self.seen[eng]
        best = {}
        for t in deps:
            key = (t[0], t[1])
            if seen.get(key, -1) >= t[2]:
                continue
            if best.get(key, -1) < t[2]:
                best[key] = t[2]
        out = []
        for key, v in best.items():
            seen[key] = v
            out.append((key[0], key[1], v))
            if key[0] == "E":
                self.marked[key[1]].add(v)
        return out

    def _commit(self, tok, reads, writes, pwrites=()):
        for b in pwrites:
            b.wx.append(tok)
        for b in reads:
            if len(b.r) > 24:
                last = {}
                for t in b.r:
                    k = (t[0], t[1])
                    if last.get(k, -1) < t[2]:
                        last[k] = t[2]
                b.r = [(k[0], k[1], v) for k, v in last.items()]
            b.r.append(tok)
        for b in writes:
            b.w = tok
            b.wx = []
            b.r = []

    def op(self, eng, fn, reads=(), writes=(), pw=()):
        waits = self._deps(eng, reads, writes, pw)
        seq = len(self.ops[eng])
        tok = ("E", eng, seq)
        self.ops[eng].append((waits, fn, "C", None))
        self._commit(tok, reads, writes, pw)
        return tok

    def dma(self, eng, fn, reads=(), writes=(), semb=None):
        if semb is None:
            semb = writes[0] if writes else reads[0]
        if semb.dsem is None:
            if semb.name not in self.dsem_names:
                self.dsem_names[semb.name] = self.ndsem
                self.ndsem += 1
            semb.dsem = self.dsem_names[semb.name]
        waits = self._deps(eng, reads, writes)
        semb.dcnt = self.dsem_final.get(semb.dsem, 0) + 16
        tok = ("D", semb.dsem, semb.dcnt)
        self.dsem_final[semb.dsem] = semb.dcnt
        self.ops[eng].append((waits, fn, "D", semb.dsem))
        self._commit(tok, reads, writes)
        return tok

    def alias(self, new_bufs, old_bufs):
        toks = []
        for b in old_bufs:
            if b.w is not None:
                toks.append(b.w)
            toks.extend(b.wx)
            toks.extend(b.r)
        last = {}
        for t in toks:
            k = (t[0], t[1])
            if last.get(k, -1) < t[2]:
                last[k] = t[2]
        toks = [(k[0], k[1], v) for k, v in last.items()]
        for nb in new_bufs:
            nb.r = list(nb.r) + toks

    def emit(self):
        nc = self.nc
        with ExitStack() as es:
            esem = {e: es.enter_context(nc.semaphore("sem_" + e)) for e in ENGS}
            dsem = [es.enter_context(nc.semaphore("dsem%d" % i)) for i in range(self.ndsem)]
            block = es.enter_context(nc.Block())
            mcount = {}
            for e in ENGS:
                ms = sorted(self.marked[e])
                mcount[e] = {s: i + 1 for i, s in enumerate(ms)}

            def run(e, engobj):
                mk = self.marked[e]
                for seq, (waits, fn, kind, extra) in enumerate(self.ops[e]):
                    for (k, a, v) in waits:
                        if k == "E":
                            engobj.wait_ge(esem[a], mcount[a][v])
                        else:
                            engobj.wait_ge(dsem[a], v)
                    ins = fn(engobj)
                    if kind == "D":
                        ins.then_inc(dsem[extra], 16)
                    elif seq in mk:
                        ins.then_inc(esem[e], 1)
                if e == "sp":
                    for i, cnt in self.dsem_final.items():
                        engobj.wait_ge(dsem[i], cnt)

            @block.tensor
            def _(eng):
                run("pe", eng)

            @block.scalar
            def _(eng):
                run("act", eng)

            @block.vector
            def _(eng):
                run("dve", eng)

            @block.gpsimd
            def _(eng):
                run("pool", eng)

            @block.sync
            def _(eng):
                run("sp", eng)


def _row_start(r):
    return min(max(r - 4, 0), 24)


def _col_start(c):
    return min(max(c - 8, 0), 48)


BIAS_TILES = [(5, 3), (5, 4), (5, 5), (5, 6), (5, 7)] + [(0, k) for k in range(4)] + [(1, k) for k in range(4)] \
    + [(14, k) for k in range(12, 16)] + [(15, k) for k in range(12, 16)]


def key_tiles(j):
    if j == 0:
        return [0, 1, 2, 3], 5
    if j == 1:
        return [0, 1, 2, 3], 9
    if j == 14:
        return [12, 13, 14, 15], 13
    if j == 15:
        return [12, 13, 14, 15], 17
    return [j - 2, j - 1, j, j + 1, j + 2], 0


def _bias_index():
    idx = np.full((21, 128, 128), 15 * 31, dtype=np.int64)
    for t, (j, jk) in enumerate(BIAS_TILES):
        for qr in range(2):
            r = 2 * j + qr
            rs = _row_start(r)
            for kr in range(2):
                r2 = 2 * jk + kr
                if not (rs <= r2 < rs + 8):
                    continue
                for c in range(64):
                    cs = _col_start(c)
                    c2 = np.arange(cs, cs + 16)
                    idx[t, kr * 64 + c2, qr * 64 + c] = (r2 - r + 7) * 31 + (c2 - c + 15)
    return idx


_CONST_CACHE = {}


def _constants():
    if _CONST_CACHE:
        return _CONST_CACHE
    c = {}
    c["ident"] = np.eye(128, dtype=np.float32)
    c["iota"] = np.tile(np.arange(128, dtype=np.float32)[None, :], (128, 1))
    t = np.arange(L)
    row = (t // 64).astype(np.float32)
    col = (t % 64).astype(np.float32)
    inv = (100.0 ** (-np.arange(16, dtype=np.float32) / 16)).astype(np.float32)
    ang = np.concatenate([row[:, None] * inv, col[:, None] * inv], axis=-1).astype(np.float32)
    c["ropec"] = np.ascontiguousarray(np.cos(ang).astype(np.float32).reshape(16, 128, 32).transpose(1, 0, 2))
    c["ropes"] = np.ascontiguousarray(np.sin(ang).astype(np.float32).reshape(16, 128, 32).transpose(1, 0, 2))
    def dft(n, scale):
        k = np.arange(n, dtype=np.int64)
        m = (k[:, None] * k[None, :]) % n
        a = 2.0 * np.pi * m.astype(np.float64) / n
        return (np.cos(a) * scale).astype(np.float32), (-np.sin(a) * scale).astype(np.float32)
    c["dftc"], c["dftns"] = dft(L, 1.0 / math.sqrt(L * 64))
    c["dftc_c"], c["dftns_c"] = dft(LC, 1.0 / math.sqrt(LC * 64))
    for k_ in BF16_INPUTS:
        c[k_] = np.ascontiguousarray(c[k_].astype(ml_dtypes.bfloat16))
    c64, ns64 = dft(64, 1.0)
    z = np.zeros((128, 128), np.float32)
    z[:64, :64] = c64
    z[64:, 64:] = c64
    c["c64blk"] = z.copy()
    z[:64, :64] = -ns64
    z[64:, 64:] = -ns64
    c["s64blk"] = z.copy()
    sel = np.zeros((16, 16, 128), np.float32)
    for e in range(16):
        sel[e, e, :] = 1.0
    c["sel"] = sel.reshape(16, 16 * 128)
    c["bias_idx"] = _bias_index()
    _CONST_CACHE.update(c)
    return c


def _prep_shared(inp):
    cst = _constants()
    f = lambda a: np.ascontiguousarray(np.asarray(a, dtype=np.float32))
    m = {}
    for k in ("ident", "iota", "ropec", "ropes", "dftc", "dftns", "dftc_c", "dftns_c", "c64blk", "s64blk", "sel"):
        m[k] = cst[k]
    m["w_ada"] = f(inp["w_ada"])
    m["b_adaT"] = f(np.asarray(inp["b_ada"]).reshape(DEPTH, 48, 128).transpose(0, 2, 1))
    m["n1gT"] = f(np.asarray(inp["norm1_g"]).reshape(DEPTH, 8, 128).transpose(0, 2, 1))
    m["n2gT"] = f(np.asarray(inp["norm2_g"]).reshape(DEPTH, 8, 128).transpose(0, 2, 1))
    m["w_in"] = f(inp["w_in"])
    m["w_out"] = f(inp["w_out"])
    m["qng"] = f(inp["q_norm_g"])
    m["kng"] = f(inp["k_norm_g"])
    rpb = np.asarray(inp["rpb"], dtype=np.float32).reshape(DEPTH, 8, 15 * 31)
    rpbp = np.concatenate([rpb, np.full((DEPTH, 8, 1), NEG, np.float32)], axis=-1)
    bt = rpbp[:, :, cst["bias_idx"]]
    m["bt"] = f(bt.transpose(0, 1, 3, 2, 4))
    lre = np.asarray(inp["s5_lam_re"], np.float32)
    lim = np.asarray(inp["s5_lam_im"], np.float32)
    lst = np.asarray(inp["s5_log_step"], np.float32)
    sm = lambda a: a.reshape(DEPTH, 2, 8, 2, 64).transpose(0, 1, 3, 4, 2).reshape(DEPTH, 2, 128, 8)
    lsr = np.repeat(lst[..., None], 64, axis=-1)
    m["s5_sm"] = f(np.stack([sm(lre), sm(lim), sm(lsr)], axis=3))
    m["s5_bc"] = f(np.stack([lre.reshape(DEPTH, 2, 1024), lim.reshape(DEPTH, 2, 1024),
                             lsr.reshape(DEPTH, 2, 1024)], axis=2))
    bre = np.asarray(inp["s5_b_re"], np.float32)
    bim = np.asarray(inp["s5_b_im"], np.float32)
    cre = np.asarray(inp["s5_c_re"], np.float32)
    cim = np.asarray(inp["s5_c_im"], np.float32)
    bz = np.zeros((DEPTH, 2, 2, 128, 8, 128), np.float32)
    cz = np.zeros((DEPTH, 2, 2, 128, 8, 128), np.float32)
    for g in range(16):
        st, two, ch0 = g // 2, g % 2, (g % 8) * 16
        bz[:, :, 0, ch0:ch0 + 16, st, two * 64:(two + 1) * 64] = bre[:, :, g].transpose(0, 1, 3, 2)
        bz[:, :, 1, ch0:ch0 + 16, st, two * 64:(two + 1) * 64] = bim[:, :, g].transpose(0, 1, 3, 2)
        cz[:, :, 0, two * 64:(two + 1) * 64, st, ch0:ch0 + 16] = cre[:, :, g].transpose(0, 1, 3, 2)
        cz[:, :, 1, two * 64:(two + 1) * 64, st, ch0:ch0 + 16] = cim[:, :, g].transpose(0, 1, 3, 2)
    m["s5_bz"] = bz
    m["s5_cz"] = cz
    m["s5_dT"] = f(np.asarray(inp["s5_d"]).reshape(DEPTH, 2, 128).transpose(0, 2, 1))
    m["w_glu"] = f(inp["w_glu"])
    m["b_gluT"] = f(np.asarray(inp["b_glu"]).reshape(DEPTH, 2, 128).transpose(0, 2, 1))
    wf = np.asarray(inp["w_fnet"], np.float32)
    wfz = np.zeros((DEPTH, 2, 128, 256), np.float32)
    for h in range(4):
        kt, hh = h // 2, h % 2
        wfz[:, kt, hh * 64:(hh + 1) * 64, h * 64:(h + 1) * 64] = wf[:, h]
    m["w_fz"] = wfz
    m["w_router"] = f(np.asarray(inp["w_router"]).reshape(8, 128, 16).transpose(1, 0, 2))
    m["b_router"] = f(inp["b_router"])
    m["w_gate"] = f(inp["w_gate"])
    m["w_up"] = f(inp["w_up"])
    m["w_down"] = f(inp["w_down"])
    return m


GROUPS = [(g * 512, 512) for g in range(4)] + [(2048, 256)]


def build_program(shapes, n_batch=2, layers=(0, 1), dbg=None, stop_after=None):
    nc = bass.Bass("TRN2", target_bir_lowering=False)
    P = Prog(nc)
    dr = {}
    for name, shp in shapes.items():
        dr[name] = nc.dram_tensor(name, list(shp), BF16 if name in BF16_INPUTS else F32, kind="ExternalInput").ap()
    out_d = nc.dram_tensor("out", [n_batch, L, D], F32, kind="ExternalOutput").ap()
    dbg_d = {}
    if dbg:
        for name, (shp, dt) in dbg.items():
            dbg_d[name] = nc.dram_tensor("dbg_" + name, list(shp), dt, kind="ExternalOutput").ap()

    class Stop(Exception):
        pass

    es = ExitStack()
    with es:
        ARENA_BYTES = 212000
        arena_t = es.enter_context(nc.sbuf_tensor("arena", [128, ARENA_BYTES // 4], F32))
        ps_t = [es.enter_context(nc.psum_tensor("ps%d" % i, [128, 512], F32)) for i in range(8)]
        PS = [t[:, :] for t in ps_t]
        PSH = [t[:, :].bitcast(BF16) for t in ps_t]
        PSb = [Buf("ps%d" % i) for i in range(8)]

        def view(off, shape, dt):
            n = int(np.prod(shape))
            assert off % 4 == 0
            if dt == F32:
                v = arena_t[:, off // 4: off // 4 + n]
                nb = n * 4
            else:
                assert n % 2 == 0
                v = arena_t[:, off // 4: off // 4 + n // 2].bitcast(BF16)
                nb = n * 2
            if len(shape) == 2:
                v = v.rearrange("p (a b) -> p a b", a=shape[0])
            elif len(shape) == 3:
                v = v.rearrange("p (a b c) -> p a b c", a=shape[0], b=shape[1])
            return v, nb

        class Alloc:
            def __init__(self, base, limit):
                self.off = base
                self.limit = limit

            def take(self, shape, dt):
                v, nb = view(self.off, shape, dt)
                self.off += (nb + 3) // 4 * 4
                assert self.off <= self.limit, (self.off, self.limit)
                return v

        pers = Alloc(0, ARENA_BYTES)
        X = pers.take([NT, D], F32)
        Xb = [Buf("x%d" % i) for i in range(NT)]
        HT_OFF = pers.off
        HT = pers.take([8, T], BF16)
        HT_END = pers.off
        HTb = [Buf("ht%d" % i) for i in range(NT)]
        ident = pers.take([128], F32); b_ident = Buf("ident")
        identb = pers.take([128], BF16); b_identb = Buf("identb")
        ones_f = pers.take([128], F32); b_ones = Buf("ones")
        ones_b = pers.take([128], BF16)
        iota = pers.take([128], F32); b_iota = Buf("iota")
        ropec = pers.take([16, 32], F32); ropes = pers.take([16, 32], F32); b_rope = Buf("rope")
        modT = pers.take([DEPTH * 48, 3], F32); b_modT = Buf("modT")
        n1g = pers.take([DEPTH, 8], F32); n2g = pers.take([DEPTH, 8], F32); b_ng = Buf("ng")
        AB = pers.take([2, 2, 8], F32); b_AB = Buf("AB")
        ssq = pers.take([NT], F32); b_ssq = Buf("ssq")
        rstd = pers.take([NT], F32); b_rstd = Buf("rstd")
        ccol = pers.take([4], F32); b_ccol = Buf("ccol")
        GB = pers.take([2, D], F32); b_GB = Buf("GB")
        diag = pers.take([128], F32); b_diag = Buf("diag")
        PH_BASE = pers.off
        PH_LIMIT = ARENA_BYTES
        state = dict(old=[], cur=[])

        def new_phase():
            state["old"] = state["old"] + state["cur"]
            summ = Buf("summ")
            P.alias([summ], state["old"])
            state["old"] = [summ]
            state["cur"] = []
            state["extra"] = []
            return Alloc(PH_BASE, PH_LIMIT)

        def pbuf(name, extra_old=()):
            b = Buf(name)
            P.alias([b], state["old"] + list(extra_old) + list(state.get("extra", [])))
            state["cur"].append(b)
            return b

        def mm(out, lhsT, rhs, start, stop, reads, writes):
            P.op("pe", lambda e: e.matmul(out=out, lhsT=lhsT, rhs=rhs, start=start, stop=stop), reads, writes)

        def tr(out, in_, idn, reads, writes):
            P.op("pe", lambda e: e.transpose(out=out, in_=in_, identity=idn), reads, writes)

        def act(out, in_, func, reads, writes, bias=None, scale=None, accum=None, pw=()):
            kw = {}
            if bias is not None:
                kw["bias"] = bias
            if scale is not None:
                kw["scale"] = scale
            if accum is not None:
                kw["accum_out"] = accum
            P.op("act", lambda e: e.activation(out=out, in_=in_, func=func, **kw), reads, writes, pw)

        def tt(eng, out, in0, in1, op, reads, writes):
            P.op(eng, lambda e: e.tensor_tensor(out=out, in0=in0, in1=in1, op=op), reads, writes)

        def ts(eng, out, in0, s1, op0, reads, writes, s2=None, op1=None, pw=()):
            if op1 is None:
                P.op(eng, lambda e: e.tensor_scalar(out=out, in0=in0, scalar1=s1, scalar2=None, op0=op0), reads, writes, pw)
            else:
                P.op(eng, lambda e: e.tensor_scalar(out=out, in0=in0, scalar1=s1, scalar2=s2, op0=op0, op1=op1), reads, writes, pw)

        def stt(eng, out, in0, scalar, in1, op0, op1, reads, writes):
            P.op(eng, lambda e: e.scalar_tensor_tensor(out=out, in0=in0, scalar=scalar, in1=in1, op0=op0, op1=op1), reads, writes)

        def red(out, in_, op, reads, writes):
            P.op("dve", lambda e: e.tensor_reduce(out=out, in_=in_, axis=AX.X, op=op), reads, writes)

        def recip(out, in_, reads, writes):
            P.op("dve", lambda e: e.reciprocal(out=out, in_=in_), reads, writes)

        def cp(eng, out, in_, reads, writes, pw=()):
            if eng == "act":
                act(out, in_, AF.Copy, reads, writes, pw=pw)
            else:
                P.op(eng, lambda e: e.tensor_copy(out=out, in_=in_), reads, writes, pw)

        def memset(eng, ap, val, writes):
            P.op(eng, lambda e: e.memset(ap, val), (), writes)

        def dma(q, out, in_, reads, writes, semb=None):
            P.dma(q, lambda e: e.dma_start(out=out, in_=in_), reads, writes, semb)

        def dump(name, src, reads):
            if name in dbg_d:
                dma("sp", dbg_d[name], src, reads, [], semb=reads[0])

        def flat(v):
            return v.rearrange("p a b -> p (a b)")

        def wview(ap2d):
            return ap2d.rearrange("(k p) n -> p k n", p=128)

        dma("sp", ident, dr["ident"][:, :], [], [b_ident])
        dma("sp", iota, dr["iota"][:, :], [], [b_iota])
        dma("sp", ropec, dr["ropec"][:, :, :], [], [b_rope])
        dma("sp", ropes, dr["ropes"][:, :, :], [], [b_rope])
        dma("sp", n1g, dr["n1gT"].rearrange("l p c -> p l c"), [], [b_ng])
        dma("sp", n2g, dr["n2gT"].rearrange("l p c -> p l c"), [], [b_ng])
        cp("dve", identb, ident, [b_ident], [b_identb])
        memset("dve", ones_f, 1.0, [b_ones])
        memset("dve", ones_b, 1.0, [b_ones])
        memset("dve", ccol[:, 0:1], EPS, [b_ccol])
        memset("dve", ccol[:, 1:2], -PI, [b_ccol])
        eps_col = ccol[:, 0:1]
        negpi_col = ccol[:, 1:2]

        ph = new_phase()
        cT = ph.take([8, 3], F32); b_cT = pbuf("cT")
        sT = ph.take([8, 3], BF16); b_sT = pbuf("sT")
        badaT = ph.take([DEPTH, 48], F32); b_bada = pbuf("bada")
        wch = [ph.take([8, 512], BF16) for _ in range(2)]
        b_wch = [pbuf("wch%d" % i) for i in range(2)]
        dma("sp", cT, dr["crowT"][:, :, :], [], [b_cT])
        dma("sp", badaT, dr["b_adaT"].rearrange("l p c -> p l c"), [], [b_bada])
        act(sT, cT, AF.Silu, [b_cT], [b_sT])
        for l in range(DEPTH):
            wv = wview(dr["w_ada"][l])
            for cc in range(12):
                s = (l * 12 + cc) % 2
                dma("pool", wch[s], wv[:, :, cc * 512:(cc + 1) * 512], [], [b_wch[s]])
                for j in range(4):
                    col = (cc * 4 + j) * 3
                    for k in range(8):
                        mm(PS[0][:, col:col + 3], wch[s][:, k, j * 128:(j + 1) * 128], sT[:, k, :], k == 0, k == 7,
                           [b_wch[s], b_sT], [PSb[0]])
            tt("dve", modT[:, l * 48:(l + 1) * 48, :], PS[0][:, 0:144].rearrange("p (c r) -> p c r", r=3),
               badaT[:, l, :].unsqueeze(2).to_broadcast([128, 48, 3]), ALU.add, [PSb[0], b_bada], [b_modT])
        dump("modT", modT, [b_modT])

        s5_scr = {}

        def s5_scratch(l, d):
            if (l, d) not in s5_scr:
                a = nc.dram_tensor("s5scrA_%d_%d" % (l, d), [128, 6144], BF16, kind="Internal").ap()
                b = nc.dram_tensor("s5scrB_%d_%d" % (l, d), [128, 1056], F32, kind="Internal").ap()
                s5_scr[(l, d)] = dict(A=a, B=b, bA1=Buf("s5A1"), bA2=Buf("s5A2"), bB1=Buf("s5B1"), bB2=Buf("s5B2"))
            return s5_scr[(l, d)]

        def modcol(l, vec, chunk, row):
            i = l * 48 + vec * 8 + chunk
            return modT[:, i, row:row + 1]

        try:
            for bi in range(n_batch):
                for i in range(NT):
                    if i < 2:
                        src = dr["ctx"][bi, i * 128:(i + 1) * 128, :]
                    else:
                        src = dr["x"][bi, (i - 2) * 128:(i - 1) * 128, :]
                    dma("sp", X[:, i, :], src, [], [Xb[i]])

                for l in layers:
                    first = (bi == 0 and l == layers[0])
                    last = (l == DEPTH - 1)
                    moe_groups = [(256 + g * 512, 512) for g in range(4)] if last else GROUPS

                    def dmp(name, src, reads):
                        if first:
                            dump(name, src, reads)

                    def norm_to_HT(gT, vshift, vscale, ph, router=None):
                        junk = ph.take([D], BF16); b_junk = pbuf("junk")
                        xs = [ph.take([D], F32) for _ in range(2)]
                        b_xs = [pbuf("xs%d" % i) for i in range(2)]
                        for r_i, row in enumerate((bi, 2)):
                            for c in range(8):
                                ts("dve", AB[:, r_i, 0, c:c + 1], modcol(l, vscale, c, row), 1.0, ALU.add,
                                   [b_modT, b_ng], [b_AB], s2=gT[:, l, c:c + 1], op1=ALU.mult)
                                cp("dve", AB[:, r_i, 1, c:c + 1], modcol(l, vshift, c, row), [b_modT], [b_AB])
                        for i in range(NT):
                            act(junk, X[:, i, :], AF.Square, [Xb[i]], [b_junk, b_ssq], accum=ssq[:, i:i + 1])
                        act(rstd, ssq, AF.Sqrt, [b_ssq, b_ccol], [b_rstd], bias=eps_col, scale=1.0 / D)
                        recip(rstd, rstd, [b_rstd], [b_rstd])
                        def mk_xs(i):
                            ts("dve", xs[i % 2], X[:, i, :], rstd[:, i:i + 1], ALU.mult, [Xb[i], b_rstd], [b_xs[i % 2]])

                        mk_xs(0)
                        for i in range(NT):
                            s = i % 2
                            r_i = 1 if i < 2 else 0
                            pb = (i % 2) * 2
                            for c in range(8):
                                bk = pb + c // 4
                                tr(PS[bk][:, (c % 4) * 128:(c % 4 + 1) * 128], xs[s][:, c * 128:(c + 1) * 128], ident,
                                   [b_xs[s], b_ident], [PSb[bk]])
                            if i + 1 < NT:
                                mk_xs(i + 1)
                            if router is not None:
                                router(i, pb, r_i)
                                continue
                            for c in range(8):
                                bk = pb + c // 4
                                src_ = PS[bk][:, (c % 4) * 128:(c % 4 + 1) * 128]
                                dst_ = HT[:, c, i * 128:(i + 1) * 128]
                                wr_, pw_ = ([HTb[i]], ()) if c == 0 else ([], [HTb[i]])
                                if c < 4:
                                    act(dst_, src_, AF.Identity, [PSb[bk], b_AB], wr_,
                                        bias=AB[:, r_i, 1, c:c + 1], scale=AB[:, r_i, 0, c:c + 1], pw=pw_)
                                else:
                                    ts("dve", dst_, src_, AB[:, r_i, 0, c:c + 1], ALU.mult, [PSb[bk], b_AB], wr_,
                                       s2=AB[:, r_i, 1, c:c + 1], op1=ALU.add, pw=pw_)

                    def make_gb(vec):
                        for r_i, row in enumerate((bi, 2)):
                            for c in range(8):
                                ts("dve", diag, ident, modcol(l, vec, c, row), ALU.mult, [b_ident, b_modT], [b_diag])
                                mm(PS[7][:, 0:128], ones_f, diag, True, True, [b_ones, b_diag], [PSb[7]])
                                cp("act", GB[:, r_i, c * 128:(c + 1) * 128], PS[7][:, 0:128], [PSb[7]], [b_GB])

                    def wout_partial(srcT, b_src, nk, wo, b_wo, tmp, b_tmp):
                        for i in range(2 if last else 0, NT):
                            r_i = 1 if i < 2 else 0
                            for half in range(2):
                                bk = 4 + (i * 2 + half) % 4
                                for kk in range(nk):
                                    mm(PS[bk], srcT[:, kk, i * 128:(i + 1) * 128], wo[:, kk, half * 512:(half + 1) * 512],
                                       kk == 0, kk == nk - 1, [b_src, b_wo], [PSb[bk]])
                                s = (i * 2 + half) % 2
                                tt("dve", tmp[s], PS[bk], GB[:, r_i, half * 512:(half + 1) * 512], ALU.mult,
                                   [PSb[bk], b_GB], [b_tmp[s]])
                                tt("pool", X[:, i, half * 512:(half + 1) * 512], X[:, i, half * 512:(half + 1) * 512], tmp[s],
                                   ALU.add, [b_tmp[s], Xb[i]], [Xb[i]])

                    def proj_fm(dstT, b_dst, wchunk, b_w, ncol_tiles):
                        n = 0
                        for (t0, tn) in GROUPS:
                            tiles = list(range(t0 // 128, (t0 + tn) // 128))
                            for ct in range(ncol_tiles):
                                bk = n % 4
                                n += 1
                                for k in range(8):
                                    mm(PS[bk][:, 0:tn], wchunk[:, k, ct * 128:(ct + 1) * 128], HT[:, k, t0:t0 + tn], k == 0, k == 7,
                                       [b_w] + [HTb[i] for i in tiles], [PSb[bk]])
                                cp("act", dstT[:, ct, t0:t0 + tn], PS[bk][:, 0:tn], [PSb[bk]], [b_dst])

                    ph = new_phase()
                    norm_to_HT(n1g, 0, 1, ph)
                    make_gb(2)
                    dmp("ht1", HT, HTb)
                    if stop_after == "norm1":
                        raise Stop()

                    ph = new_phase()
                    win = ph.take([8, 256], BF16); b_win = pbuf("win")
                    uT = ph.take([2, T], BF16); b_uT = pbuf("uT")
                    dma("pool", win, wview(dr["w_in"][l])[:, :, 1536:1792], [], [b_win])
                    proj_fm(uT, b_uT, win, b_win, 2)
                    dmp("uT", uT, [b_uT])
                    y1T = ph.take([2, T], BF16); b_y1T = pbuf("y1T")
                    wglu = ph.take([2, 256], BF16); b_wglu = pbuf("wglu")
                    dma("pool", wglu, wview(dr["w_glu"][l]), [], [b_wglu])
                    bglu = ph.take([2], F32); dsk = ph.take([2], F32); b_sp = pbuf("s5small")
                    dma("sp", bglu, dr["b_gluT"][l], [], [b_sp])
                    dma("sp", dsk, dr["s5_dT"][l], [], [b_sp])
                    wo_s = ph.take([2, D], BF16); b_wo_s = pbuf("wo_s")
                    dma("pool", wo_s, wview(dr["w_out"][l])[:, 4:6, :], [], [b_wo_s])
                    bz_off = ph.off
                    bzr = ph.take([8, 128], BF16); bzi = ph.take([8, 128], BF16); b_bz = pbuf("bz")
                    czr = ph.take([8, 128], BF16); czi = ph.take([8, 128], BF16); b_cz = pbuf("cz")
                    bzcz_flat = view(bz_off, [4096], BF16)[0]
                    assert ph.off == bz_off + 4 * 2048
                    sm = ph.take([3, 8], F32); b_sm = pbuf("sm")
                    smv = ph.take([40, 8], F32); b_smv = pbuf("smv")
                    dgd = ph.take([2, 128], BF16); b_dgd = pbuf("dgd")
                    carry = ph.take([2, 8], F32); b_carry = pbuf("carry")
                    ytmp = ph.take([2, 128], F32); b_ytmp = pbuf("ytmp")
                    SC = [ph.take([8, 128], F32) for _ in range(11)]
                    b_SC = [pbuf("sc%d" % i) for i in range(11)]
                    def arena_pair(v0):
                        off = v0.offset - arena_t[:, :].offset
                        return arena_t[:, off:off + 2048].rearrange("p (t a b) -> p t a b", t=2, a=8)

                    def halves(v):
                        f = flat(v).bitcast(BF16)
                        return [f[:, 0:1024].rearrange("p (a b) -> p a b", a=8), f[:, 1024:2048].rearrange("p (a b) -> p a b", a=8)]
                    GR, GI, RT0, ZR, ZI = SC[0:5]
                    b_GR, b_GI, b_RT0, b_ZR, b_ZI = b_SC[0:5]
                    BUrb, BUib = halves(SC[5]); b_BUb = b_SC[5]
                    COSb, SINb = halves(SC[6]); b_tabb = b_SC[6]
                    p1, p2 = halves(SC[7]); b_pp = b_SC[7]
                    grb, gib = halves(SC[8]); b_gb = b_SC[8]
                    HRb, HIb = halves(SC[9]); b_Hb = b_SC[9]
                    p3, p4 = halves(SC[10]); b_pq = b_SC[10]
                    sig = [flat(ZR)[:, 0:512], flat(ZR)[:, 512:1024]]; b_sig = b_ZR
                    wtmp = [flat(ZI)[:, 0:512], flat(ZI)[:, 512:1024]]; b_wt5 = b_ZI
                    c127 = ph.take([2, 8], F32); b_c127 = pbuf("c127")

                    def sincos(o_sin, o_cos, th, t1, t2, t3, R_, W_):
                        RW = R_ + W_
                        t1i = t1.bitcast(I32)
                        for (o_, shift) in ((o_sin, 0.0), (o_cos, 0.5 * PI)):
                            if shift == 0.0:
                                src = th
                            else:
                                ts("dve", t3, th, shift, ALU.add, RW, W_)
                                src = t3
                            ts("dve", t1i, src, 1.0 / (2 * PI), ALU.mult, RW, W_)
                            cp("dve", t2, t1i, W_, W_)
                            stt("dve", t3, t2, -2 * PI, src, ALU.mult, ALU.add, RW, W_)
                            ts("dve", t3, t3, -PI, ALU.max, W_, W_, s2=PI, op1=ALU.min)
                            act(o_, t3, AF.Sin, W_, W_)

                    def coef_math(a, b, ls, tmp, R_, W_, extra=None):
                        dlt, rr, th, sn, cs, t1, t2, t3 = tmp
                        RW = R_ + W_
                        act(dlt, ls, AF.Exp, R_, W_)
                        tt("dve", rr, dlt, a, ALU.mult, RW, W_)
                        if extra is not None:
                            act(extra[0], rr, AF.Exp, W_, W_, scale=128.0)
                        act(rr, rr, AF.Exp, W_, W_)
                        tt("dve", th, dlt, b, ALU.mult, RW, W_)
                        if extra is not None:
                            ts("dve", extra[1], th, 128.0, ALU.mult, W_, W_)
                        sincos(sn, cs, th, t1, t2, t3, W_, W_)
                        tt("dve", cs, cs, rr, ALU.mult, W_, W_)
                        tt("dve", sn, sn, rr, ALU.mult, W_, W_)
                        tt("dve", t1, a, a, ALU.mult, RW, W_)
                        tt("dve", t2, b, b, ALU.mult, RW, W_)
                        tt("dve", t1, t1, t2, ALU.add, W_, W_)
                        recip(t1, t1, W_, W_)
                        ts("dve", t2, cs, -1.0, ALU.add, W_, W_)
                        tt("dve", t3, t2, a, ALU.mult, RW, W_)
                        tt("dve", dlt, sn, b, ALU.mult, RW, W_)
                        tt("dve", t3, t3, dlt, ALU.add, W_, W_)
                        tt("dve", t3, t3, t1, ALU.mult, W_, W_)
                        tt("dve", dlt, sn, a, ALU.mult, RW, W_)
                        tt("dve", t2, t2, b, ALU.mult, RW, W_)
                        tt("dve", dlt, dlt, t2, ALU.subtract, W_, W_)
                        tt("dve", dlt, dlt, t1, ALU.mult, W_, W_)
                        return dict(theta=th, r=rr, lbr=cs, lbi=sn, cr=t3, ci=dlt)

                    for ct in range(2):
                        ts("dve", dgd[:, ct, :], ident, dsk[:, ct:ct + 1], ALU.mult, [b_ident, b_sp], [b_dgd])
                    for d in (1, 0):
                        rev = (d == 1)
                        allb = list(b_SC)
                        W_ = [b_smv]
                        KA = smv[:, 16:18, :]; KB = smv[:, 18:20, :]; fa = smv[:, 20:22, :]; fb = smv[:, 22:24, :]
                        sc_ = s5_scratch(l, d)
                        tab_flat = flat(SC[6]).bitcast(BF16)
                        kab_flat = smv[:, 16:20, :].rearrange('p a b -> p (a b)')
                        if bi == 0:
                            for q_, slot in enumerate((0, 1, 2)):
                                dma("sp", flat(SC[slot]), dr["s5_bc"][l, d, q_].partition_broadcast(128), [], allb)
                            o = coef_math(flat(SC[0]), flat(SC[1]), flat(SC[2]), [flat(SC[i]) for i in range(3, 11)], [], allb)
                            zre, zim, w1, w2 = flat(SC[0]), flat(SC[1]), flat(SC[2]), flat(SC[4])
                            dma("sp", zre, dr["s5_bz"][l, d, 0].rearrange("p a b -> p (a b)"), [], allb)
                            dma("sp", zim, dr["s5_bz"][l, d, 1].rearrange("p a b -> p (a b)"), [], allb)
                            cr, ci = o["cr"], o["ci"]
                            tt("dve", w1, zre, cr, ALU.mult, allb, allb)
                            tt("dve", w2, zim, ci, ALU.mult, allb, allb)
                            tt("dve", flat(bzr), w1, w2, ALU.subtract, allb, [b_bz])
                            tt("dve", w1, zre, ci, ALU.mult, allb, allb)
                            tt("dve", w2, zim, cr, ALU.mult, allb, allb)
                            tt("dve", flat(bzi), w1, w2, ALU.add, allb, [b_bz])
                            dma("pool", czr, dr["s5_cz"][l, d, 0], [], [b_cz])
                            dma("pool", czi, dr["s5_cz"][l, d, 1], [], [b_cz])
                            ts("dve", flat(czi), flat(czi), -1.0, ALU.mult, [b_cz], [b_cz])
                            dma("sp", sm, dr["s5_sm"][l, d], [], [b_sm])
                            W_ = [b_smv]
                            r128, th128 = smv[:, 8, :], smv[:, 9, :]
                            so = coef_math(sm[:, 0, :], sm[:, 1, :], sm[:, 2, :], [smv[:, i, :] for i in range(8)], [b_sm], W_,
                                           extra=(r128, th128))
                            s128, c128 = smv[:, 10, :], smv[:, 11, :]
                            sincos(s128, c128, th128, smv[:, 12, :], smv[:, 13, :], smv[:, 14, :], W_, W_)
                            KA = smv[:, 16:18, :]; KB = smv[:, 18:20, :]; fa = smv[:, 20:22, :]; fb = smv[:, 22:24, :]
                            tt("dve", KA[:, 0, :], so["r"], c128, ALU.mult, W_, W_)
                            cp("dve", KA[:, 1, :], KA[:, 0, :], W_, W_)
                            tt("dve", KB[:, 1, :], so["r"], s128, ALU.mult, W_, W_)
                            ts("dve", KB[:, 0, :], KB[:, 1, :], -1.0, ALU.mult, W_, W_)
                            ANG = SC[3]
                            for st in range(8):
                                ts("dve", ANG[:, st, :], iota, so["theta"][:, st:st + 1], ALU.mult, [b_iota, b_smv] + allb, allb)
                                ts("dve", RT0[:, st, :], iota, 1.0, ALU.min, [b_iota, b_smv] + allb, allb, s2=so["r"][:, st:st + 1], op1=ALU.mult)
                            sincos(flat(SC[4]), flat(SC[7]), flat(ANG), flat(SC[0]), flat(SC[1]), flat(SC[8]), allb, allb)
                            cp("act", SINb, SC[4], allb, allb)
                            cp("act", COSb, SC[7], allb, allb)
                            dma('sp', sc_['A'][:, 0:4096], bzcz_flat, [b_bz, b_cz], [sc_['bA1']])
                            dma('sp', sc_['A'][:, 4096:6144], tab_flat, [b_tabb], [sc_['bA2']])
                            dma('sp', sc_['B'][:, 0:1024], flat(RT0), [b_RT0], [sc_['bB1']])
                            dma('sp', sc_['B'][:, 1024:1056], kab_flat, [b_smv], [sc_['bB2']])
                        else:
                            dma('sp', bzcz_flat, sc_['A'][:, 0:4096], [sc_['bA1']], [b_bz, b_cz])
                            dma('sp', tab_flat, sc_['A'][:, 4096:6144], [sc_['bA2']], [b_tabb])
                            dma('sp', flat(RT0), sc_['B'][:, 0:1024], [sc_['bB1']], [b_RT0])
                            dma('sp', kab_flat, sc_['B'][:, 1024:1056], [sc_['bB2']], [b_smv])
                        G127 = arena_pair(SC[0])[:, :, :, 127]
                        G127s = arena_pair(SC[0])[:, ::-1, :, 127]
                        Z0 = arena_pair(SC[3])[:, :, :, 0]
                        b_G2 = [b_GR, b_GI]; b_Z2 = [b_ZR, b_ZI]

                        order = ([1, 0] + list(range(17, 1, -1))) if rev else list(range(18))

                        def tokslice(k):
                            t0 = k * 128
                            if rev:
                                return slice(t0 + 127, (t0 - 1) if t0 > 0 else None, -1)
                            return slice(t0, t0 + 128)

                        def bu_stage(k):
                            tok = tokslice(k)
                            for st in range(8):
                                ct = st // 4
                                for ri, bz_ in enumerate((bzr, bzi)):
                                    bk = ri * 2 + st // 4
                                    mm(PS[bk][:, (st % 4) * 128:(st % 4 + 1) * 128], bz_[:, st, :], uT[:, ct, tok], True, True,
                                       [b_bz, b_uT], [PSb[bk]])
                            for h in range(2):
                                sl = slice(4 * h, 4 * h + 4)
                                cp("act", BUrb[:, sl, :], PS[h][:, :].rearrange("p (a b) -> p a b", a=4), [PSb[h]], [b_BUb])
                                cp("act", BUib[:, sl, :], PS[2 + h][:, :].rearrange("p (a b) -> p a b", a=4), [PSb[2 + h]], [b_BUb])

                        def y_stage(n_c, k):
                            t0 = k * 128
                            for ct in range(2):
                                bk = 4 + 2 * (n_c % 2) + ct
                                if d == 1:
                                    cp("act", y1T[:, ct, t0:t0 + 128], PS[bk][:, 0:128], [PSb[bk]], [b_y1T])
                                else:
                                    tt("dve", ytmp[:, ct, :], PS[bk][:, 0:128], y1T[:, ct, t0:t0 + 128], ALU.add, [PSb[bk], b_y1T], [b_ytmp])
                                    act(y1T[:, ct, t0:t0 + 128], ytmp[:, ct, :], AF.Gelu, [b_ytmp], [b_y1T])

                        def fwd_stage():
                            tt("dve", flat(p1), flat(BUrb), flat(COSb), ALU.mult, [b_BUb, b_tabb], [b_pp])
                            tt("dve", flat(p2), flat(BUib), flat(SINb), ALU.mult, [b_BUb, b_tabb], [b_pp])
                            tt("dve", flat(ZR), flat(p1), flat(p2), ALU.add, [b_pp], [b_ZR])
                            tt("dve", flat(p3), flat(BUib), flat(COSb), ALU.mult, [b_BUb, b_tabb], [b_pq])
                            tt("dve", flat(p4), flat(BUrb), flat(SINb), ALU.mult, [b_BUb, b_tabb], [b_pq])
                            tt("dve", flat(ZI), flat(p3), flat(p4), ALU.subtract, [b_pq], [b_ZI])

                        NC = len(order)
                        bu_stage(order[0])
                        fwd_stage()
                        if NC > 1:
                            bu_stage(order[1])
                        for n_c, k in enumerate(order):
                            if n_c > 0:
                                tt("dve", fa, G127, KA, ALU.mult, b_G2 + W_, W_)
                                tt("dve", fb, G127s, KB, ALU.mult, b_G2 + W_, W_)
                                tt("dve", fa, fa, fb, ALU.add, W_, W_)
                                tt("dve", Z0, Z0, fa, ALU.add, b_Z2 + W_, b_Z2)
                            P.op("dve", lambda e: e.tensor_tensor_scan(out=flat(GR), data0=flat(RT0), data1=flat(ZR), initial=0.0,
                                                                       op0=ALU.mult, op1=ALU.add), [b_RT0, b_ZR], [b_GR])
                            P.op("dve", lambda e: e.tensor_tensor_scan(out=flat(GI), data0=flat(RT0), data1=flat(ZI), initial=0.0,
                                                                       op0=ALU.mult, op1=ALU.add), [b_RT0, b_ZI], [b_GI])
                            cp("act", grb, GR, [b_GR], [b_gb])
                            cp("act", gib, GI, [b_GI], [b_gb])
                            if n_c + 1 < NC:
                                fwd_stage()
                                if n_c + 2 < NC:
                                    bu_stage(order[n_c + 2])
                            tt("dve", flat(p1), flat(grb), flat(COSb), ALU.mult, [b_gb, b_tabb], [b_pp])
                            tt("dve", flat(p2), flat(gib), flat(SINb), ALU.mult, [b_gb, b_tabb], [b_pp])
                            tt("dve", flat(HRb), flat(p1), flat(p2), ALU.subtract, [b_pp], [b_Hb])
                            tt("dve", flat(p3), flat(grb), flat(SINb), ALU.mult, [b_gb, b_tabb], [b_pq])
                            tt("dve", flat(p4), flat(gib), flat(COSb), ALU.mult, [b_gb, b_tabb], [b_pq])
                            tt("dve", flat(HIb), flat(p3), flat(p4), ALU.add, [b_pq], [b_Hb])
                            for ct in range(2):
                                bk = 4 + 2 * (n_c % 2) + ct
                                n = 0
                                for st in range(4 * ct, 4 * ct + 4):
                                    for (cz_, hh_) in ((czr, HRb), (czi, HIb)):
                                        rhs = hh_[:, st, ::-1] if rev else hh_[:, st, :]
                                        mm(PS[bk][:, 0:128], cz_[:, st, :], rhs, n == 0, (n == 7 and d == 1), [b_cz, b_Hb], [PSb[bk]])
                                        n += 1
                                if d == 0:
                                    mm(PS[bk][:, 0:128], dgd[:, ct, :], uT[:, ct, k * 128:(k + 1) * 128], False, True, [b_dgd, b_uT], [PSb[bk]])
                            if n_c > 0:
                                y_stage(n_c - 1, order[n_c - 1])
                        y_stage(len(order) - 1, order[-1])
                    dmp("gT", y1T, [b_y1T])
                    n = 0
                    for (t0, tn) in GROUPS:
                        for co in range(2):
                            bk = n % 4
                            s = n % 2
                            n += 1
                            for k in range(2):
                                mm(PS[bk][:, 0:tn], wglu[:, k, co * 128:(co + 1) * 128], y1T[:, k, t0:t0 + tn], k == 0, k == 1,
                                   [b_wglu, b_y1T], [PSb[bk]])
                            act(sig[s][:, 0:tn], PS[bk][:, 0:tn], AF.Sigmoid, [PSb[bk], b_sp], [b_sig], bias=bglu[:, co:co + 1])
                            tt("dve", uT[:, co, t0:t0 + tn], sig[s][:, 0:tn], y1T[:, co, t0:t0 + tn], ALU.mult, [b_sig, b_y1T], [b_uT])
                    dmp("ssmT", uT, [b_uT])
                    wout_partial(uT, b_uT, 2, wo_s, b_wo_s, wtmp, [b_wt5, b_wt5])
                    if stop_after == "s5":
                        raise Stop()

                    ph = new_phase()
                    win = ph.take([8, 256], BF16); b_win = pbuf("winf")
                    ufT = ph.take([2, T], BF16); b_ufT = pbuf("ufT")
                    dma("pool", win, wview(dr["w_in"][l])[:, :, 1792:2048], [], [b_win])
                    proj_fm(ufT, b_ufT, win, b_win, 2)
                    wo_f = ph.take([2, D], BF16); b_wo_f = pbuf("wo_f")
                    dma("pool", wo_f, wview(dr["w_out"][l])[:, 6:8, :], [], [b_wo_f])
                    wfz = ph.take([2, 256], F32); b_wfz = pbuf("wfz")
                    dma("sp", wfz, dr["w_fz"][l].rearrange("k p n -> p k n"), [], [b_wfz])
                    c64 = ph.take([128], F32); s64 = ph.take([128], F32); b_c64 = pbuf("c64")
                    dma("sp", c64, dr["c64blk"][:, :], [], [b_c64])
                    dma("sp", s64, dr["s64blk"][:, :], [], [b_c64])
                    G = ph.take([2, 512], BF16); b_G = pbuf("G")
                    for kt in range(2):
                        for ti, tb in enumerate((c64, s64)):
                            mm(PS[ti][:, 0:256], tb, wfz[:, kt, :], True, True, [b_c64, b_wfz], [PSb[ti]])
                            cp("act", G[:, kt, ti * 256:(ti + 1) * 256], PS[ti][:, 0:256], [PSb[ti]], [b_G])
                    A_tok = ph.take([NT, 512], BF16); b_A = pbuf("A_tok")
                    for i in range(NT):
                        bk = i % 4
                        for kt in range(2):
                            mm(PS[bk], ufT[:, kt, i * 128:(i + 1) * 128], G[:, kt, :], kt == 0, kt == 1, [b_ufT, b_G], [PSb[bk]])
                        if i == 0:
                            cp("act", A_tok[:, i, :], PS[bk], [PSb[bk]], [b_A])
                        else:
                            cp("act" if i % 2 == 0 else "dve", A_tok[:, i, :], PS[bk], [PSb[bk]], [], pw=[b_A])
                    ring = [ph.take([4, 512], BF16) for _ in range(4)]
                    b_ring = [pbuf("ring%d" % i) for i in range(4)]
                    ctab = [ph.take([2, 256], BF16) for _ in range(2)]; b_ctab = pbuf("ctab")
                    wtmp = [ph.take([512], F32) for _ in range(2)]; b_wtmp = [pbuf("wtmpf%d" % i) for i in range(2)]
                    fouT = ufT
                    b_fouT = pbuf("fouT", extra_old=[b_ufT])
                    tabs = (dr["dftc"], dr["dftns"])
                    nring = 0
                    for lg in range(4):
                        nmm = 0
                        for ktg in range(4):
                            for ti in range(2):
                                s = nring % 4
                                nring += 1
                                src = tabs[ti].rearrange("(k p) n -> p k n", p=128)[:, ktg * 4:(ktg + 1) * 4, lg * 512:(lg + 1) * 512]
                                dma("sp", ring[s], src, [], [b_ring[s]])
                                for kk in range(4):
                                    kt = ktg * 4 + kk
                                    for ct in range(2):
                                        mm(PS[4 + ct], A_tok[:, 2 + kt, ti * 256 + ct * 128: ti * 256 + (ct + 1) * 128], ring[s][:, kk, :],
                                           nmm < 2, nmm >= 62, [b_A, b_ring[s]], [PSb[4 + ct]])
                                        nmm += 1
                        for ct in range(2):
                            cp("act", fouT[:, ct, 256 + lg * 512: 256 + (lg + 1) * 512], PS[4 + ct], [PSb[4 + ct], b_A], [b_fouT])
                    dma("sp", ctab[0], wview(dr["dftc_c"]), [], [b_ctab])
                    dma("sp", ctab[1], wview(dr["dftns_c"]), [], [b_ctab])
                    for ct in range(2):
                        n = 0
                        for ti in range(2):
                            for kt in range(2):
                                mm(PS[ct][:, 0:256], A_tok[:, kt, ti * 256 + ct * 128: ti * 256 + (ct + 1) * 128], ctab[ti][:, kt, :],
                                   n == 0, n == 3, [b_A, b_ctab], [PSb[ct]])
                                n += 1
                        cp("act", fouT[:, ct, 0:256], PS[ct][:, 0:256], [PSb[ct], b_A], [b_fouT])
                    dmp("fouT", fouT, [b_fouT])
                    wout_partial(fouT, b_fouT, 2, wo_f, b_wo_f, wtmp, b_wtmp)
                    if stop_after == "fnet":
                        raise Stop()

                    ph = new_phase()
                    wq = [ph.take([8, 512], BF16) for _ in range(2)]
                    b_wq = [pbuf("wq%d" % i) for i in range(2)]
                    qT = ph.take([4, T], BF16); kT = ph.take([4, T], BF16)
                    b_qT = pbuf("qT"); b_kT = pbuf("kT")
                    V = ph.take([NT, 8 * 65], BF16); b_V = pbuf("V")
                    memset("pool", V.rearrange("p t (h d) -> p (t h) d", d=65)[:, :, 64:65], 1.0, [b_V])
                    sq = ph.take([512], BF16); b_sq = pbuf("sq")
                    qn_off = ph.off
                    qn = [ph.take([512], F32) for _ in range(2)]; b_qn = [pbuf("qn%d" % i) for i in range(2)]
                    tsb_off = ph.off
                    tsb = ph.take([512], F32); b_tsb = pbuf("tsb")
                    qr = [ph.take([512], BF16) for _ in range(2)]; b_qr = [pbuf("qr%d" % i) for i in range(2)]
                    s8 = ph.take([2, 8], F32); b_s8 = [pbuf("s8_%d" % i) for i in range(2)]
                    Gqk = ph.take([2, 64], F32); b_Gqk = pbuf("Gqk")
                    dma("sp", Gqk[:, 0, :], dr["qng"][l].partition_broadcast(128), [], [b_Gqk])
                    dma("sp", Gqk[:, 1, :], dr["kng"][l].partition_broadcast(128), [], [b_Gqk])
                    items = [(ci, i) for ci in range(3) for i in range(NT) if not (last and ci == 0 and i < 2)]
                    MMB = [0, 1, 4, 5]

                    def load_wq(ci):
                        dma("pool", wq[ci % 2], wview(dr["w_in"][l])[:, :, ci * 512:(ci + 1) * 512], [], [b_wq[ci % 2]])

                    def stA(n):
                        ci, i = items[n]
                        s = ci % 2
                        bk = MMB[n % 4]
                        for k in range(8):
                            mm(PS[bk], HT[:, k, i * 128:(i + 1) * 128], wq[s][:, k, :], k == 0, k == 7, [HTb[i], b_wq[s]], [PSb[bk]])
                        if ci == 0 and i == NT - 1:
                            load_wq(2)

                    def stB(n):
                        ci, i = items[n]
                        bk = MMB[n % 4]
                        if ci == 2:
                            cp("act", V[:, i, :].rearrange("p (h d) -> p h d", d=65)[:, :, 0:64],
                               PS[bk].rearrange("p (h d) -> p h d", d=64), [PSb[bk]], [], pw=[b_V])
                            return
                        u = n % 2
                        s8u = s8[:, u, :]
                        act(sq, PS[bk], AF.Square, [PSb[bk]], [b_sq])
                        red(s8u, sq.rearrange("p (h d) -> p h d", h=8), ALU.add, [b_sq], [b_s8[u]])
                        act(s8u, s8u, AF.Sqrt, [b_s8[u], b_ccol], [b_s8[u]], bias=eps_col, scale=1.0 / 64)
                        recip(s8u, s8u, [b_s8[u]], [b_s8[u]])
                        qn3 = qn[u].rearrange("p (h d) -> p h d", h=8)
                        tt("dve", qn3, PS[bk].rearrange("p (h d) -> p h d", h=8), s8u.unsqueeze(2).to_broadcast([128, 8, 64]),
                           ALU.mult, [PSb[bk], b_s8[u]], [b_qn[u]])
                        tt("dve", qn3, qn3, Gqk[:, ci, :].unsqueeze(1).to_broadcast([128, 8, 64]), ALU.mult, [b_qn[u], b_Gqk], [b_qn[u]])
                        qr3 = qr[u].rearrange("p (h d) -> p h d", h=8)
                        if i >= 2:
                            j = i - 2
                            cb = ropec[:, j, :].unsqueeze(1).to_broadcast([128, 16, 32])
                            sb_ = ropes[:, j, :].unsqueeze(1).to_broadcast([128, 16, 32])
                            qn4 = qn[u].rearrange("p (g d) -> p g d", g=16)
                            tt("dve", tsb.rearrange("p (g d) -> p g d", g=16), qn4, sb_, ALU.mult, [b_qn[u], b_rope], [b_tsb])
                            tt("dve", qn4, qn4, cb, ALU.mult, [b_qn[u], b_rope], [b_qn[u]])
                            ts3 = tsb.rearrange("p (h d) -> p h d", h=8)
                            tt("pool", qr3[:, :, 0:32], qn3[:, :, 0:32], ts3[:, :, 32:64], ALU.subtract, [b_qn[u], b_tsb], [b_qr[u]])
                            tt("pool", qr3[:, :, 32:64], ts3[:, :, 0:32], qn3[:, :, 32:64], ALU.add, [b_qn[u], b_tsb], [b_qr[u]])
                        else:
                            cp("pool", qr[u], qn[u], [b_qn[u]], [b_qr[u]])

                    def stC(n):
                        ci, i = items[n]
                        if ci == 2:
                            return
                        u = n % 2
                        tb = 2 + u
                        for hp in range(4):
                            tr(PSH[tb][:, hp * 128:(hp + 1) * 128], qr[u][:, hp * 128:(hp + 1) * 128], identb,
                               [b_qr[u], b_identb], [PSb[tb]])
                        dst = qT if ci == 0 else kT
                        cp("act", dst[:, :, i * 128:(i + 1) * 128], PSH[tb][:, 0:512].rearrange("p (a b) -> p a b", a=4),
                           [PSb[tb]], [b_qT if ci == 0 else b_kT])

                    load_wq(0)
                    load_wq(1)
                    NI = len(items)
                    stA(0)
                    stA(1)
                    for n in range(NI):
                        stB(n)
                        if n + 2 < NI:
                            stA(n + 2)
                        if n >= 1:
                            stC(n - 1)
                    stC(NI - 1)
                    dmp("qT", qT, [b_qT])
                    dmp("kT", kT, [b_kT])
                    hta = Alloc(HT_OFF, HT_END)
                    NBT = 3
                    BT = [hta.take([21, 128], BF16) for _ in range(NBT)]
                    b_BT = [pbuf("BT%d" % i, extra_old=HTb) for i in range(NBT)]
                    attT = hta.take([4, T], BF16); b_attT = pbuf("attT", extra_old=HTb)
                    wo_a = wq[0].rearrange("p k n -> p (k n)").rearrange("p (k n) -> p k n", k=4)
                    b_wo_a = b_wq[0]
                    dma("pool", wo_a, wview(dr["w_out"][l])[:, 0:4, :], [], [b_wo_a])
                    w1flat = wq[1].rearrange("p k n -> p (k n)")
                    PT = [w1flat[:, o_:o_ + 896].rearrange("p (a b) -> p a b", a=7) for o_ in (0, 896, 3072)]
                    b_PT = [pbuf("PT%d" % s, extra_old=[b_wq[1]]) for s in range(3)]
                    Ef = [w1flat[:, 1792:3072].bitcast(F32).rearrange("p (a b) -> p a b", a=5), view(qn_off, [5, 128], F32)[0]]
                    b_E = [pbuf("E0", extra_old=[b_wq[1]]), pbuf("E1", extra_old=b_qn)]
                    rinv = [view(tsb_off + 512 * i, [128], F32)[0] for i in range(2)]
                    b_rinv = [pbuf("rinv%d" % i, extra_old=[b_tsb]) for i in range(2)]
                    its = []
                    for hp in range(4):
                        for n_q, qi in enumerate(list(range(2, NT)) + ([] if last else [0, 1])):
                            for hh in range(2):
                                its.append((2 * hp + hh, qi, n_q))
                    att2 = [ph.take([128], BF16) for _ in range(2)]; b_att2 = [pbuf("att2_%d" % i) for i in range(2)]
                    rinv2 = ph.take([2, 2], F32); b_rinv2 = [pbuf("rinv2_%d" % i) for i in range(2)]

                    def it_info(n):
                        h, qi, n_h = its[n]
                        if qi >= 2:
                            kts, t0b = key_tiles(qi - 2)
                            lat = [2 + k for k in kts]
                        else:
                            lat, t0b = [], 0
                        return h, qi, n_h, lat, t0b

                    def st1(n):
                        h, qi, n_h, lat, t0b = it_info(n)
                        hp, hh = h // 2, h % 2
                        pr = slice(64 * hh, 64 * hh + 64)
                        keys = lat + [0, 1]
                        sb2 = (n % 2) * 2
                        qsl = slice(qi * 128, (qi + 1) * 128)
                        for m, kt_ in enumerate(keys):
                            bk = sb2 + m // 4
                            mm(PS[bk][:, (m % 4) * 128:(m % 4 + 1) * 128], kT[pr, hp, kt_ * 128:(kt_ + 1) * 128], qT[pr, hp, qsl],
                               True, True, [b_kT, b_qT], [PSb[bk]])

                    def st2a(n):
                        h, qi, n_h, lat, t0b = it_info(n)
                        bs = h % NBT
                        nl = len(lat)
                        nk = nl + 2
                        sb2 = (n % 2) * 2
                        pe_ = n % 2
                        pt_ = n % 3
                        if h % 2 == 0 and n_h == 1 and h + 2 < 8:
                            dma("pool", BT[(h + 2) % NBT], dr["bt"][l, h + 2], [], [b_BT[(h + 2) % NBT]])
                        if h % 2 == 0 and n_h == 0 and h >= 2:
                            dma("pool", BT[(h + 1) % NBT], dr["bt"][l, h + 1], [], [b_BT[(h + 1) % NBT]])
                        m = 0
                        while m < nk:
                            bk = sb2 + m // 4
                            if m < nl:
                                m2 = min(nl, (m // 4 + 1) * 4)
                                src = PS[bk][:, (m % 4) * 128:(m % 4) * 128 + (m2 - m) * 128].rearrange("p (a b) -> p a b", b=128)
                                stt("dve", Ef[pe_][:, m:m2, :], src, 0.125, BT[bs][:, t0b + m:t0b + m2, :], ALU.mult, ALU.add,
                                    [PSb[bk], b_BT[bs]], [b_E[pe_]])
                            else:
                                m2 = min(nk, (m // 4 + 1) * 4)
                                src = PS[bk][:, (m % 4) * 128:(m % 4) * 128 + (m2 - m) * 128].rearrange("p (a b) -> p a b", b=128)
                                act(PT[pt_][:, m:m2, :], src, AF.Exp, [PSb[bk], b_E[pe_]], [b_PT[pt_]], scale=0.125)
                            m = m2

                    def st2b(n):
                        h, qi, n_h, lat, t0b = it_info(n)
                        nl = len(lat)
                        pe_ = n % 2
                        pt_ = n % 3
                        m = 0
                        while m < nl:
                            m2 = min(nl, (m // 4 + 1) * 4)
                            act(PT[pt_][:, m:m2, :], Ef[pe_][:, m:m2, :], AF.Exp, [b_E[pe_]], [b_PT[pt_]])
                            m = m2

                    def st3(n):
                        h, qi, n_h, lat, t0b = it_info(n)
                        hh = h % 2
                        keys = lat + [0, 1]
                        nk = len(keys)
                        pi = n // 2
                        ob = 4 + pi % 2
                        ps_ = n % 3
                        for m, kt_ in enumerate(keys):
                            mm(PS[ob][:, hh * 128:hh * 128 + 65], PT[ps_][:, m, :], V[:, kt_, h * 65:(h + 1) * 65], m == 0, m == nk - 1,
                               [b_V, b_PT[ps_]], [PSb[ob]])

                    def fin1(pi):
                        hp, qi = its[2 * pi][0] // 2, its[2 * pi][1]
                        ob = 4 + pi % 2
                        u = pi % 2
                        o3 = PS[ob][:, 0:256].rearrange("p (h d) -> p h d", d=128)
                        recip(rinv2[:, u, :], o3[:, :, 64], [PSb[ob]], [b_rinv2[u]])
                        tt("dve", att2[u].rearrange("p (h d) -> p h d", d=64), o3[:, :, 0:64],
                           rinv2[:, u, :].unsqueeze(2).to_broadcast([128, 2, 64]), ALU.mult, [PSb[ob], b_rinv2[u]], [b_att2[u]])

                    def fin2(pi):
                        hp, qi = its[2 * pi][0] // 2, its[2 * pi][1]
                        u = pi % 2
                        tb = 6 + u
                        tr(PSH[tb][:, 0:128], att2[u], identb, [b_att2[u], b_identb], [PSb[tb]])
                        cp("act", attT[:, hp, qi * 128:(qi + 1) * 128], PSH[tb][:, 0:128], [PSb[tb]], [], pw=[b_attT])

                    memset("pool", attT[:, 0, 0:2], 0.0, [b_attT])
                    dma("pool", BT[0], dr["bt"][l, 0], [], [b_BT[0]])
                    dma("pool", BT[1], dr["bt"][l, 1], [], [b_BT[1]])
                    NIT = len(its)
                    st1(0)
                    st1(1)
                    st2a(0)
                    st1(2)
                    st2a(1)
                    st2b(0)
                    for n in range(NIT):
                        if n + 3 < NIT:
                            st1(n + 3)
                        if n + 1 < NIT:
                            st2b(n + 1)
                        if n + 2 < NIT:
                            st2a(n + 2)
                        st3(n)
                        if n % 2 == 1:
                            fin1(n // 2)
                            if n // 2 >= 1:
                                fin2(n // 2 - 1)
                    fin2(NIT // 2 - 1)
                    dmp("attT", attT, [b_attT])
                    b_wt = [pbuf("wtmpa%d" % i, extra_old=[b_wq[1], b_E[0]] + b_PT) for i in range(2)]
                    wout_partial_h = None
                    wtmp = [w1flat[:, 0:1024].bitcast(F32), w1flat[:, 1024:2048].bitcast(F32)]
                    wout_partial(attT, b_attT, 4, wo_a, b_wo_a, wtmp, b_wt)
                    dmp("xmix", X, Xb)
                    if stop_after == "attn":
                        raise Stop()

                    ph = new_phase()
                    for b_ in HTb:
                        P.alias([b_], [b_attT] + b_BT)
                    wgu = [None, None]; wd = [None, None]; b_wgu = [None, None]; b_wd = [None, None]
                    wgu[0] = ph.take([8, 1024], BF16); wd[0] = ph.take([4, 1024], BF16)
                    b_wgu[0] = pbuf("wgu0"); b_wd[0] = pbuf("wd0")

                    def load_expert(e):
                        s_ = e % 2
                        dma("pool", wgu[s_][:, :, 0:512], wview(dr["w_gate"][l, e]), [], [b_wgu[s_]])
                        dma("pool", wgu[s_][:, :, 512:1024], wview(dr["w_up"][l, e]), [], [b_wgu[s_]])
                        dma("pool", wd[s_], wview(dr["w_down"][l, e]), [], [b_wd[s_]])

                    load_expert(0)
                    sub_base = ph.off
                    wr = ph.take([8, 16], F32); b_wr = pbuf("wr")
                    dma("sp", wr, dr["w_router"][:, :, :], [], [b_wr])
                    h2f = [ph.take([8, 128], F32) for _ in range(2)]
                    b_h2f = [pbuf("h2f%d" % i) for i in range(2)]

                    def router(i, pb, r_i):
                        s = i % 2
                        for c in range(8):
                            bk = pb + c // 4
                            src_ = PS[bk][:, (c % 4) * 128:(c % 4 + 1) * 128]
                            wr_, pw_ = ([b_h2f[s]], ()) if c == 0 else ([], [b_h2f[s]])
                            if c < 4:
                                act(h2f[s][:, c, :], src_, AF.Identity, [PSb[bk], b_AB], wr_,
                                    bias=AB[:, r_i, 1, c:c + 1], scale=AB[:, r_i, 0, c:c + 1], pw=pw_)
                            else:
                                ts("dve", h2f[s][:, c, :], src_, AB[:, r_i, 0, c:c + 1], ALU.mult, [PSb[bk], b_AB], wr_,
                                   s2=AB[:, r_i, 1, c:c + 1], op1=ALU.add, pw=pw_)
                        cp("pool", HT[:, :, i * 128:(i + 1) * 128], h2f[s], [b_h2f[s]], [HTb[i]])
                        for c in range(8):
                            mm(PS[6][:, i * 16:(i + 1) * 16], h2f[s][:, c, :], wr[:, c, :], c == 0, c == 7, [b_h2f[s], b_wr], [PSb[6]])

                    n_before = len(state["cur"])
                    norm_to_HT(n2g, 3, 4, ph, router=router)
                    make_gb(5)
                    dmp("ht2", HT, HTb)
                    state["extra"] = [b_wr] + b_h2f + state["cur"][n_before:]
                    ph = Alloc(sub_base, PH_LIMIT)
                    wgu[1] = ph.take([8, 1024], BF16); wd[1] = ph.take([4, 1024], BF16)
                    b_wgu[1] = pbuf("wgu1"); b_wd[1] = pbuf("wd1")
                    aff = ph.take([NT, 16], F32); sel = ph.take([NT, 16], F32); w_ = ph.take([NT, 16], F32)
                    eq = ph.take([NT, 16], F32)
                    b_rt = pbuf("rt")
                    m1 = ph.take([72], F32); m2 = ph.take([72], F32); gs = ph.take([72], F32); gsel = ph.take([72], F32)
                    gmax = ph.take([NT], F32); wsum = ph.take([NT], F32)
                    brt = ph.take([16], F32); b_brt = pbuf("brt")
                    dma("sp", brt, dr["b_router"].partition_broadcast(128), [], [b_brt])
                    R = [b_rt]
                    f2 = lambda v: v.rearrange("p a b -> p (a b)")
                    v4 = lambda v: f2(v).rearrange("p (g e) -> p g e", e=4)
                    act(f2(aff), PS[6][:, 0:288], AF.Sigmoid, [PSb[6]], R)
                    dmp("aff", aff, R)
                    tt("dve", sel, aff, brt.unsqueeze(1).to_broadcast([128, NT, 16]), ALU.add, R + [b_brt], R)
                    red(m1, v4(sel), ALU.max, R, R)
                    tt("dve", v4(eq), v4(sel), m1.unsqueeze(2).to_broadcast([128, 72, 4]), ALU.is_equal, R, R)
                    stt("dve", v4(eq), v4(eq), -1.0e9, v4(sel), ALU.mult, ALU.add, R, R)
                    red(m2, v4(eq), ALU.max, R, R)
                    tt("dve", gs, m1, m2, ALU.add, R, R)
                    gs3 = gs.rearrange("p (t g) -> p t g", g=4)
                    red(gmax, gs3, ALU.max, R, R)
                    tt("dve", gsel.rearrange("p (t g) -> p t g", g=4), gs3, gmax.unsqueeze(2).to_broadcast([128, NT, 4]), ALU.is_equal, R, R)
                    tt("dve", v4(eq), v4(sel), m2.unsqueeze(2).to_broadcast([128, 72, 4]), ALU.is_ge, R, R)
                    tt("dve", v4(eq), v4(eq), gsel.unsqueeze(2).to_broadcast([128, 72, 4]), ALU.mult, R, R)
                    tt("dve", w_, aff, eq, ALU.mult, R, R)
                    red(wsum, w_, ALU.add, R, R)
                    recip(wsum, wsum, R, R)
                    tt("dve", w_, w_, wsum.unsqueeze(2).to_broadcast([128, NT, 16]), ALU.mult, R, R)
                    dmp("gates", w_, R)
                    gatesT = ph.take([T], BF16); b_gT = pbuf("gatesT")
                    for i in range(NT):
                        bk = (i // 4) % 2
                        tr(PS[bk][0:16, (i % 4) * 128:(i % 4 + 1) * 128], w_[:, i, :], ident, R + [b_ident], [PSb[bk]])
                        if i % 4 == 3 or i == NT - 1:
                            i0 = (i // 4) * 4
                            n_ = i - i0 + 1
                            cp("act", gatesT[0:16, i0 * 128:(i0 + n_) * 128], PS[bk][0:16, 0:n_ * 128], [PSb[bk]], [b_gT])
                    selc = ph.take([16 * 128], BF16); b_selc = pbuf("selc")
                    dma("pool", selc[0:16, :], dr["sel"][:, :], [], [b_selc])
                    load_expert(1)
                    h1T = [ph.take([4, 512], BF16) for _ in range(2)]; b_h1T = [pbuf("h1T%d" % i) for i in range(2)]
                    sil = [ph.take([512], F32) for _ in range(2)]; b_sil = [pbuf("sil%d" % i) for i in range(2)]
                    hmul = [ph.take([512], BF16) for _ in range(2)]; b_hmul = [pbuf("hmul%d" % i) for i in range(2)]
                    GBC = [ph.take([512], BF16) for _ in range(2)]; b_GBC = [pbuf("GBC%d" % i) for i in range(2)]
                    wtmp = [ph.take([512], F32) for _ in range(2)]; b_wtmp = [pbuf("wtmpm%d" % i) for i in range(2)]
                    cnt = dict(F=0, D=0)

                    def gateup(e, gi):
                        s = e % 2
                        t0, tn = moe_groups[gi]
                        tiles = list(range(t0 // 128, (t0 + tn) // 128))
                        hb = [HTb[i] for i in tiles]
                        gsl = (e * len(moe_groups) + gi) % 2
                        mm(PS[7][:, 0:tn], selc[0:16, e * 128:(e + 1) * 128], gatesT[0:16, t0:t0 + tn], True, True,
                           [b_selc, b_gT], [PSb[7]])
                        cp("act", GBC[gsl][:, 0:tn], PS[7][:, 0:tn], [PSb[7]], [b_GBC[gsl]])
                        for fc in range(4):
                            fs = cnt["F"] % 2
                            cnt["F"] += 1
                            bg, bu = fs * 2, fs * 2 + 1
                            for k in range(8):
                                mm(PS[bg][:, 0:tn], wgu[s][:, k, fc * 128:(fc + 1) * 128], HT[:, k, t0:t0 + tn], k == 0, k == 7,
                                   [b_wgu[s]] + hb, [PSb[bg]])
                            for k in range(8):
                                mm(PS[bu][:, 0:tn], wgu[s][:, k, 512 + fc * 128:512 + (fc + 1) * 128], HT[:, k, t0:t0 + tn], k == 0, k == 7,
                                   [b_wgu[s]] + hb, [PSb[bu]])
                            act(sil[fs][:, 0:tn], PS[bg][:, 0:tn], AF.Silu, [PSb[bg]], [b_sil[fs]])
                            tt("dve", hmul[fs][:, 0:tn], sil[fs][:, 0:tn], PS[bu][:, 0:tn], ALU.mult, [b_sil[fs], PSb[bu]], [b_hmul[fs]])
                            tt("dve", h1T[gsl][:, fc, 0:tn], hmul[fs][:, 0:tn], GBC[gsl][:, 0:tn], ALU.mult,
                               [b_hmul[fs], b_GBC[gsl]], [b_h1T[gsl]])

                    def down(e, gi):
                        s = e % 2
                        t0, tn = moe_groups[gi]
                        tiles = list(range(t0 // 128, (t0 + tn) // 128))
                        gsl = (e * len(moe_groups) + gi) % 2
                        for ti, i in enumerate(tiles):
                            r_i = 1 if i < 2 else 0
                            for half in range(2):
                                bk = 4 + cnt["D"] % 2
                                ws = cnt["D"] % 2
                                cnt["D"] += 1
                                for fc in range(4):
                                    mm(PS[bk], h1T[gsl][:, fc, ti * 128:(ti + 1) * 128], wd[s][:, fc, half * 512:(half + 1) * 512],
                                       fc == 0, fc == 3, [b_h1T[gsl], b_wd[s]], [PSb[bk]])
                                tt("dve", wtmp[ws], PS[bk], GB[:, r_i, half * 512:(half + 1) * 512], ALU.mult,
                                   [PSb[bk], b_GB], [b_wtmp[ws]])
                                tt("pool", X[:, i, half * 512:(half + 1) * 512], X[:, i, half * 512:(half + 1) * 512], wtmp[ws],
                                   ALU.add, [b_wtmp[ws], Xb[i]], [Xb[i]])

                    prev = None
                    for e in range(16):
                        for gi in range(len(moe_groups)):
                            gateup(e, gi)
                            if prev is not None:
                                down(*prev)
                            prev = (e, gi)
                            if gi == 0 and 1 <= e < 15:
                                load_expert(e + 1)
                    down(*prev)
                    dmp("xout", X, Xb)
                    if stop_after == "moe":
                        raise Stop()

                for i in range(2, NT):
                    dma("sp", out_d[bi, (i - 2) * 128:(i - 1) * 128, :], X[:, i, :], [Xb[i]], [], semb=Xb[i])
        except Stop:
            pass
        P.emit()
    return nc


_PROG_CACHE = {}


def _shapes(m):
    return {k: tuple(v.shape) for k, v in m.items()}


def kernel(**inputs):
    n_cores = 8
    shared = _prep_shared(inputs)
    x = np.ascontiguousarray(np.asarray(inputs["x"], dtype=np.float32))
    ctx = np.ascontiguousarray(np.asarray(inputs["ctx"], dtype=np.float32))
    c = np.asarray(inputs["c"], dtype=np.float32)
    c_ctx = np.asarray(inputs["c_ctx"], dtype=np.float32)
    in_maps = []
    for core in range(n_cores):
        m = dict(shared)
        b0 = 2 * core
        m["x"] = x[b0:b0 + 2]
        m["ctx"] = ctx[b0:b0 + 2]
        rows = np.stack([c[b0], c[b0 + 1], c_ctx], axis=0)
        m["crowT"] = np.ascontiguousarray(rows.reshape(3, 8, 128).transpose(2, 1, 0))
        in_maps.append(m)
    key = "main"
    if key not in _PROG_CACHE:
        _PROG_CACHE[key] = build_program(_shapes(in_maps[0]))
    nc = _PROG_CACHE[key]
    res = run_bass_kernel_spmd(nc, in_maps, core_ids=list(range(n_cores)))
    out = np.concatenate([np.asarray(r["out"], dtype=np.float32) for r in res.results], axis=0)
    return out
```

```python
import math
from contextlib import ExitStack
import numpy as np
import ml_dtypes
import concourse.bass as bass
import concourse.mybir as mybir
from concourse.bass_utils import run_bass_kernel_spmd

F32 = mybir.dt.float32
BF16 = mybir.dt.bfloat16
I32 = mybir.dt.int32
AF = mybir.ActivationFunctionType
ALU = mybir.AluOpType
AX = mybir.AxisListType

D = 1024
L = 2048
LC = 256
T = L + LC
NT = T // 128
DEPTH = 2
EPS = 1e-6
NEG = -30000.0
PI = math.pi

ENGS = ("pe", "act", "dve", "pool", "sp")
BF16_INPUTS = ("dftc", "dftns", "dftc_c", "dftns_c")


class Buf:
    __slots__ = ("name", "w", "wx", "r", "dsem", "dcnt")

    def __init__(self, name):
        self.name = name
        self.w = None
        self.wx = []
        self.r = []
        self.dsem = None
        self.dcnt = 0


class Prog:
    def __init__(self, nc):
        self.nc = nc
        self.ops = {e: [] for e in ENGS}
        self.seen = {e: {} for e in ENGS}
        self.marked = {e: set() for e in ENGS}
        self.ndsem = 0
        self.dsem_final = {}
        self.dsem_names = {}

    def _deps(self, eng, reads, writes, pwrites=()):
        deps = []
        for b in reads:
            if b.w is not None:
                deps.append(b.w)
            deps.extend(b.wx)
        for b in writes:
            if b.w is not None:
                t = b.w
                if not (t[0] == "E" and t[1] == eng):
                    deps.append(t)
            for t in list(b.r) + list(b.wx):
                if not (t[0] == "E" and t[1] == eng):
                    deps.append(t)
        for b in pwrites:
            if b.w is not None:
                t = b.w
                if not (t[0] == "E" and t[1] == eng):
                    deps.append(t)
            for t in b.r:
                if not (t[0] == "E" and t[1] == eng):
                    deps.append(t)
        seen = self.seen[eng]
        best = {}
        for t in deps:
            key = (t[0], t[1])
            if seen.get(key, -1) >= t[2]:
                continue
            if best.get(key, -1) < t[2]:
                best[key] = t[2]
        out = []
        for key, v in best.items():
            seen[key] = v
            out.append((key[0], key[1], v))
            if key[0] == "E":
                self.marked[key[1]].add(v)
        return out

    def _commit(self, tok, reads, writes, pwrites=()):
        for b in pwrites:
            b.wx.append(tok)
        for b in reads:
            if len(b.r) > 24:
                last = {}
                for t in b.r:
                    k = (t[0], t[1])
                    if last.get(k, -1) < t[2]:
                        last[k] = t[2]
                b.r = [(k[0], k[1], v) for k, v in last.items()]
            b.r.append(tok)
        for b in writes:
            b.w = tok
            b.wx = []
            b.r = []

    def op(self, eng, fn, reads=(), writes=(), pw=()):
        waits = self._deps(eng, reads, writes, pw)
        seq = len(self.ops[eng])
        tok = ("E", eng, seq)
        self.ops[eng].append((waits, fn, "C", None))
        self._commit(tok, reads, writes, pw)
        return tok

    def dma(self, eng, fn, reads=(), writes=(), semb=None):
        if semb is None:
            semb = writes[0] if writes else reads[0]
        if semb.dsem is None:
            if semb.name not in self.dsem_names:
                self.dsem_names[semb.name] = self.ndsem
                self.ndsem += 1
            semb.dsem = self.dsem_names[semb.name]
        waits = self._deps(eng, reads, writes)
        semb.dcnt = self.dsem_final.get(semb.dsem, 0) + 16
        tok = ("D", semb.dsem, semb.dcnt)
        self.dsem_final[semb.dsem] = semb.dcnt
        self.ops[eng].append((waits, fn, "D", semb.dsem))
        self._commit(tok, reads, writes)
        return tok

    def alias(self, new_bufs, old_bufs):
        toks = []
        for b in old_bufs:
            if b.w is not None:
                toks.append(b.w)
            toks.extend(b.wx)
            toks.extend(b.r)
        last = {}
        for t in toks:
            k = (t[0], t[1])
            if last.get(k, -1) < t[2]:
                last[k] = t[2]
        toks = [(k[0], k[1], v) for k, v in last.items()]
        for nb in new_bufs:
            nb.r = list(nb.r) + toks

    def emit(self):
        nc = self.nc
        with ExitStack() as es:
            esem = {e: es.enter_context(nc.semaphore("sem_" + e)) for e in ENGS}
            dsem = [es.enter_context(nc.semaphore("dsem%d" % i)) for i in range(self.ndsem)]
            block = es.enter_context(nc.Block())
            mcount = {}
            for e in ENGS:
                ms = sorted(self.marked[e])
                mcount[e] = {s: i + 1 for i, s in enumerate(ms)}

            def run(e, engobj):
                mk = self.marked[e]
                for seq, (waits, fn, kind, extra) in enumerate(self.ops[e]):
                    for (k, a, v) in waits:
                        if k == "E":
                            engobj.wait_ge(esem[a], mcount[a][v])
                        else:
                            engobj.wait_ge(dsem[a], v)
                    ins = fn(engobj)
                    if kind == "D":
                        ins.then_inc(dsem[extra], 16)
                    elif seq in mk:
                        ins.then_inc(esem[e], 1)
                if e == "sp":
                    for i, cnt in self.dsem_final.items():
                        engobj.wait_ge(dsem[i], cnt)

            @block.tensor
            def _(eng):
                run("pe", eng)

            @block.scalar
            def _(eng):
                run("act", eng)

            @block.vector
            def _(eng):
                run("dve", eng)

            @block.gpsimd
            def _(eng):
                run("pool", eng)

            @block.sync
            def _(eng):
                run("sp", eng)


def _row_start(r):
    return min(max(r - 4, 0), 24)


def _col_start(c):
    return min(max(c - 8, 0), 48)


BIAS_TILES = [(5, 3), (5, 4), (5, 5), (5, 6), (5, 7)] + [(0, k) for k in range(4)] + [(1, k) for k in range(4)] \
    + [(14, k) for k in range(12, 16)] + [(15, k) for k in range(12, 16)]


def key_tiles(j):
    if j == 0:
        return [0, 1, 2, 3], 5
    if j == 1:
        return [0, 1, 2, 3], 9
    if j == 14:
        return [12, 13, 14, 15], 13
    if j == 15:
        return [12, 13, 14, 15], 17
    return [j - 2, j - 1, j, j + 1, j + 2], 0


def _bias_index():
    idx = np.full((21, 128, 128), 15 * 31, dtype=np.int64)
    for t, (j, jk) in enumerate(BIAS_TILES):
        for qr in range(2):
            r = 2 * j + qr
            rs = _row_start(r)
            for kr in range(2):
                r2 = 2 * jk + kr
                if not (rs <= r2 < rs + 8):
                    continue
                for c in range(64):
                    cs = _col_start(c)
                    c2 = np.arange(cs, cs + 16)
                    idx[t, kr * 64 + c2, qr * 64 + c] = (r2 - r + 7) * 31 + (c2 - c + 15)
    return idx


_CONST_CACHE = {}


def _constants():
    if _CONST_CACHE:
        return _CONST_CACHE
    c = {}
    c["ident"] = np.eye(128, dtype=np.float32)
    c["iota"] = np.tile(np.arange(128, dtype=np.float32)[None, :], (128, 1))
    t = np.arange(L)
    row = (t // 64).astype(np.float32)
    col = (t % 64).astype(np.float32)
    inv = (100.0 ** (-np.arange(16, dtype=np.float32) / 16)).astype(np.float32)
    ang = np.concatenate([row[:, None] * inv, col[:, None] * inv], axis=-1).astype(np.float32)
    c["ropec"] = np.ascontiguousarray(np.cos(ang).astype(np.float32).reshape(16, 128, 32).transpose(1, 0, 2))
    c["ropes"] = np.ascontiguousarray(np.sin(ang).astype(np.float32).reshape(16, 128, 32).transpose(1, 0, 2))
    def dft(n, scale):
        k = np.arange(n, dtype=np.int64)
        m = (k[:, None] * k[None, :]) % n
        a = 2.0 * np.pi * m.astype(np.float64) / n
        return (np.cos(a) * scale).astype(np.float32), (-np.sin(a) * scale).astype(np.float32)
    c["dftc"], c["dftns"] = dft(L, 1.0 / math.sqrt(L * 64))
    c["dftc_c"], c["dftns_c"] = dft(LC, 1.0 / math.sqrt(LC * 64))
    for k_ in BF16_INPUTS:
        c[k_] = np.ascontiguousarray(c[k_].astype(ml_dtypes.bfloat16))
    c64, ns64 = dft(64, 1.0)
    z = np.zeros((128, 128), np.float32)
    z[:64, :64] = c64
    z[64:, 64:] = c64
    c["c64blk"] = z.copy()
    z[:64, :64] = -ns64
    z[64:, 64:] = -ns64
    c["s64blk"] = z.copy()
    sel = np.zeros((16, 16, 128), np.float32)
    for e in range(16):
        sel[e, e, :] = 1.0
    c["sel"] = sel.reshape(16, 16 * 128)
    c["bias_idx"] = _bias_index()
    _CONST_CACHE.update(c)
    return c


def _prep_shared(inp):
    cst = _constants()
    f = lambda a: np.ascontiguousarray(np.asarray(a, dtype=np.float32))
    m = {}
    for k in ("ident", "iota", "ropec", "ropes", "dftc", "dftns", "dftc_c", "dftns_c", "c64blk", "s64blk", "sel"):
        m[k] = cst[k]
    m["w_ada"] = f(inp["w_ada"])
    m["b_adaT"] = f(np.asarray(inp["b_ada"]).reshape(DEPTH, 48, 128).transpose(0, 2, 1))
    m["n1gT"] = f(np.asarray(inp["norm1_g"]).reshape(DEPTH, 8, 128).transpose(0, 2, 1))
    m["n2gT"] = f(np.asarray(inp["norm2_g"]).reshape(DEPTH, 8, 128).transpose(0, 2, 1))
    m["w_in"] = f(inp["w_in"])
    m["w_out"] = f(inp["w_out"])
    m["qng"] = f(inp["q_norm_g"])
    m["kng"] = f(inp["k_norm_g"])
    rpb = np.asarray(inp["rpb"], dtype=np.float32).reshape(DEPTH, 8, 15 * 31)
    rpbp = np.concatenate([rpb, np.full((DEPTH, 8, 1), NEG, np.float32)], axis=-1)
    bt = rpbp[:, :, cst["bias_idx"]]
    m["bt"] = f(bt.transpose(0, 1, 3, 2, 4))
    lre = np.asarray(inp["s5_lam_re"], np.float32)
    lim = np.asarray(inp["s5_lam_im"], np.float32)
    lst = np.asarray(inp["s5_log_step"], np.float32)
    sm = lambda a: a.reshape(DEPTH, 2, 8, 2, 64).transpose(0, 1, 3, 4, 2).reshape(DEPTH, 2, 128, 8)
    lsr = np.repeat(lst[..., None], 64, axis=-1)
    m["s5_sm"] = f(np.stack([sm(lre), sm(lim), sm(lsr)], axis=3))
    m["s5_bc"] = f(np.stack([lre.reshape(DEPTH, 2, 1024), lim.reshape(DEPTH, 2, 1024),
                             lsr.reshape(DEPTH, 2, 1024)], axis=2))
    bre = np.asarray(inp["s5_b_re"], np.float32)
    bim = np.asarray(inp["s5_b_im"], np.float32)
    cre = np.asarray(inp["s5_c_re"], np.float32)
    cim = np.asarray(inp["s5_c_im"], np.float32)
    bz = np.zeros((DEPTH, 2, 2, 128, 8, 128), np.float32)
    cz = np.zeros((DEPTH, 2, 2, 128, 8, 128), np.float32)
    for g in range(16):
        st, two, ch0 = g // 2, g % 2, (g % 8) * 16
        bz[:, :, 0, ch0:ch0 + 16, st, two * 64:(two + 1) * 64] = bre[:, :, g].transpose(0, 1, 3, 2)
        bz[:, :, 1, ch0:ch0 + 16, st, two * 64:(two + 1) * 64] = bim[:, :, g].transpose(0, 1, 3, 2)
        cz[:, :, 0, two * 64:(two + 1) * 64, st, ch0:ch0 + 16] = cre[:, :, g].transpose(0, 1, 3, 2)
        cz[:, :, 1, two * 64:(two + 1) * 64, st, ch0:ch0 + 16] = cim[:, :, g].transpose(0, 1, 3, 2)
    m["s5_bz"] = bz
    m["s5_cz"] = cz
    m["s5_dT"] = f(np.asarray(inp["s5_d"]).reshape(DEPTH, 2, 128).transpose(0, 2, 1))
    m["w_glu"] = f(inp["w_glu"])
    m["b_gluT"] = f(np.asarray(inp["b_glu"]).reshape(DEPTH, 2, 128).transpose(0, 2, 1))
    wf = np.asarray(inp["w_fnet"], np.float32)
    wfz = np.zeros((DEPTH, 2, 128, 256), np.float32)
    for h in range(4):
        kt, hh = h // 2, h % 2
        wfz[:, kt, hh * 64:(hh + 1) * 64, h * 64:(h + 1) * 64] = wf[:, h]
    m["w_fz"] = wfz
    m["w_router"] = f(np.asarray(inp["w_router"]).reshape(8, 128, 16).transpose(1, 0, 2))
    m["b_router"] = f(inp["b_router"])
    m["w_gate"] = f(inp["w_gate"])
    m["w_up"] = f(inp["w_up"])
    m["w_down"] = f(inp["w_down"])
    return m


GROUPS = [(g * 512, 512) for g in range(4)] + [(2048, 256)]


def build_program(shapes, n_batch=2, layers=(0, 1), dbg=None, stop_after=None):
    nc = bass.Bass("TRN2", target_bir_lowering=False)
    P = Prog(nc)
    dr = {}
    for name, shp in shapes.items():
        dr[name] = nc.dram_tensor(name, list(shp), BF16 if name in BF16_INPUTS else F32, kind="ExternalInput").ap()
    out_d = nc.dram_tensor("out", [n_batch, L, D], F32, kind="ExternalOutput").ap()
    dbg_d = {}
    if dbg:
        for name, (shp, dt) in dbg.items():
            dbg_d[name] = nc.dram_tensor("dbg_" + name, list(shp), dt, kind="ExternalOutput").ap()

    class Stop(Exception):
        pass

    es = ExitStack()
    with es:
        ARENA_BYTES = 212000
        arena_t = es.enter_context(nc.sbuf_tensor("arena", [128, ARENA_BYTES // 4], F32))
        ps_t = [es.enter_context(nc.psum_tensor("ps%d" % i, [128, 512], F32)) for i in range(8)]
        PS = [t[:, :] for t in ps_t]
        PSH = [t[:, :].bitcast(BF16) for t in ps_t]
        PSb = [Buf("ps%d" % i) for i in range(8)]

        def view(off, shape, dt):
            n = int(np.prod(shape))
            assert off % 4 == 0
            if dt == F32:
                v = arena_t[:, off // 4: off // 4 + n]
                nb = n * 4
            else:
                assert n % 2 == 0
                v = arena_t[:, off // 4: off // 4 + n // 2].bitcast(BF16)
                nb = n * 2
            if len(shape) == 2:
                v = v.rearrange("p (a b) -> p a b", a=shape[0])
            elif len(shape) == 3:
                v = v.rearrange("p (a b c) -> p a b c", a=shape[0], b=shape[1])
            return v, nb

        class Alloc:
            def __init__(self, base, limit):
                self.off = base
                self.limit = limit

            def take(self, shape, dt):
                v, nb = view(self.off, shape, dt)
                self.off += (nb + 3) // 4 * 4
                assert self.off <= self.limit, (self.off, self.limit)
                return v

        pers = Alloc(0, ARENA_BYTES)
        X = pers.take([NT, D], F32)
        Xb = [Buf("x%d" % i) for i in range(NT)]
        HT_OFF = pers.off
        HT = pers.take([8, T], BF16)
        HT_END = pers.off
        HTb = [Buf("ht%d" % i) for i in range(NT)]
        ident = pers.take([128], F32); b_ident = Buf("ident")
        identb = pers.take([128], BF16); b_identb = Buf("identb")
        ones_f = pers.take([128], F32); b_ones = Buf("ones")
        ones_b = pers.take([128], BF16)
        iota = pers.take([128], F32); b_iota = Buf("iota")
        ropec = pers.take([16, 32], F32); ropes = pers.take([16, 32], F32); b_rope = Buf("rope")
        modT = pers.take([DEPTH * 48, 3], F32); b_modT = Buf("modT")
        n1g = pers.take([DEPTH, 8], F32); n2g = pers.take([DEPTH, 8], F32); b_ng = Buf("ng")
        AB = pers.take([2, 2, 8], F32); b_AB = Buf("AB")
        ssq = pers.take([NT], F32); b_ssq = Buf("ssq")
        rstd = pers.take([NT], F32); b_rstd = Buf("rstd")
        ccol = pers.take([4], F32); b_ccol = Buf("ccol")
        GB = pers.take([2, D], F32); b_GB = Buf("GB")
        diag = pers.take([128], F32); b_diag = Buf("diag")
        PH_BASE = pers.off
        PH_LIMIT = ARENA_BYTES
        state = dict(old=[], cur=[])

        def new_phase():
            state["old"] = state["old"] + state["cur"]
            summ = Buf("summ")
            P.alias([summ], state["old"])
            state["old"] = [summ]
            state["cur"] = []
            state["extra"] = []
            return Alloc(PH_BASE, PH_LIMIT)

        def pbuf(name, extra_old=()):
            b = Buf(name)
            P.alias([b], state["old"] + list(extra_old) + list(state.get("extra", [])))
            state["cur"].append(b)
            return b

        def mm(out, lhsT, rhs, start, stop, reads, writes):
            P.op("pe", lambda e: e.matmul(out=out, lhsT=lhsT, rhs=rhs, start=start, stop=stop), reads, writes)

        def tr(out, in_, idn, reads, writes):
            P.op("pe", lambda e: e.transpose(out=out, in_=in_, identity=idn), reads, writes)

        def act(out, in_, func, reads, writes, bias=None, scale=None, accum=None, pw=()):
            kw = {}
            if bias is not None:
                kw["bias"] = bias
            if scale is not None:
                kw["scale"] = scale
            if accum is not None:
                kw["accum_out"] = accum
            P.op("act", lambda e: e.activation(out=out, in_=in_, func=func, **kw), reads, writes, pw)

        def tt(eng, out, in0, in1, op, reads, writes):
            P.op(eng, lambda e: e.tensor_tensor(out=out, in0=in0, in1=in1, op=op), reads, writes)

        def ts(eng, out, in0, s1, op0, reads, writes, s2=None, op1=None, pw=()):
            if op1 is None:
                P.op(eng, lambda e: e.tensor_scalar(out=out, in0=in0, scalar1=s1, scalar2=None, op0=op0), reads, writes, pw)
            else:
                P.op(eng, lambda e: e.tensor_scalar(out=out, in0=in0, scalar1=s1, scalar2=s2, op0=op0, op1=op1), reads, writes, pw)

        def stt(eng, out, in0, scalar, in1, op0, op1, reads, writes):
            P.op(eng, lambda e: e.scalar_tensor_tensor(out=out, in0=in0, scalar=scalar, in1=in1, op0=op0, op1=op1), reads, writes)

        def red(out, in_, op, reads, writes):
            P.op("dve", lambda e: e.tensor_reduce(out=out, in_=in_, axis=AX.X, op=op), reads, writes)

        def recip(out, in_, reads, writes):
            P.op("dve", lambda e: e.reciprocal(out=out, in_=in_), reads, writes)

        def cp(eng, out, in_, reads, writes, pw=()):
            if eng == "act":
                act(out, in_, AF.Copy, reads, writes, pw=pw)
            else:
                P.op(eng, lambda e: e.tensor_copy(out=out, in_=in_), reads, writes, pw)

        def memset(eng, ap, val, writes):
            P.op(eng, lambda e: e.memset(ap, val), (), writes)

        def dma(q, out, in_, reads, writes, semb=None):
            P.dma(q, lambda e: e.dma_start(out=out, in_=in_), reads, writes, semb)

        def dump(name, src, reads):
            if name in dbg_d:
                dma("sp", dbg_d[name], src, reads, [], semb=reads[0])

        def flat(v):
            return v.rearrange("p a b -> p (a b)")

        def wview(ap2d):
            return ap2d.rearrange("(k p) n -> p k n", p=128)

        dma("sp", ident, dr["ident"][:, :], [], [b_ident])
        dma("sp", iota, dr["iota"][:, :], [], [b_iota])
        dma("sp", ropec, dr["ropec"][:, :, :], [], [b_rope])
        dma("sp", ropes, dr["ropes"][:, :, :], [], [b_rope])
        dma("sp", n1g, dr["n1gT"].rearrange("l p c -> p l c"), [], [b_ng])
        dma("sp", n2g, dr["n2gT"].rearrange("l p c -> p l c"), [], [b_ng])
        cp("dve", identb, ident, [b_ident], [b_identb])
        memset("dve", ones_f, 1.0, [b_ones])
        memset("dve", ones_b, 1.0, [b_ones])
        memset("dve", ccol[:, 0:1], EPS, [b_ccol])
        memset("dve", ccol[:, 1:2], -PI, [b_ccol])
        eps_col = ccol[:, 0:1]
        negpi_col = ccol[:, 1:2]

        ph = new_phase()
        cT = ph.take([8, 3], F32); b_cT = pbuf("cT")
        sT = ph.take([8, 3], BF16); b_sT = pbuf("sT")
        badaT = ph.take([DEPTH, 48], F32); b_bada = pbuf("bada")
        wch = [ph.take([8, 512], BF16) for _ in range(2)]
        b_wch = [pbuf("wch%d" % i) for i in range(2)]
        dma("sp", cT, dr["crowT"][:, :, :], [], [b_cT])
        dma("sp", badaT, dr["b_adaT"].rearrange("l p c -> p l c"), [], [b_bada])
        act(sT, cT, AF.Silu, [b_cT], [b_sT])
        for l in range(DEPTH):
            wv = wview(dr["w_ada"][l])
            for cc in range(12):
                s = (l * 12 + cc) % 2
                dma("pool", wch[s], wv[:, :, cc * 512:(cc + 1) * 512], [], [b_wch[s]])
                for j in range(4):
                    col = (cc * 4 + j) * 3
                    for k in range(8):
                        mm(PS[0][:, col:col + 3], wch[s][:, k, j * 128:(j + 1) * 128], sT[:, k, :], k == 0, k == 7,
                           [b_wch[s], b_sT], [PSb[0]])
            tt("dve", modT[:, l * 48:(l + 1) * 48, :], PS[0][:, 0:144].rearrange("p (c r) -> p c r", r=3),
               badaT[:, l, :].unsqueeze(2).to_broadcast([128, 48, 3]), ALU.add, [PSb[0], b_bada], [b_modT])
        dump("modT", modT, [b_modT])

        s5_scr = {}

        def s5_scratch(l, d):
            if (l, d) not in s5_scr:
                a = nc.dram_tensor("s5scrA_%d_%d" % (l, d), [128, 6144], BF16, kind="Internal").ap()
                b = nc.dram_tensor("s5scrB_%d_%d" % (l, d), [128, 1056], F32, kind="Internal").ap()
                s5_scr[(l, d)] = dict(A=a, B=b, bA1=Buf("s5A1"), bA2=Buf("s5A2"), bB1=Buf("s5B1"), bB2=Buf("s5B2"))
            return s5_scr[(l, d)]

        def modcol(l, vec, chunk, row):
            i = l * 48 + vec * 8 + chunk
            return modT[:, i, row:row + 1]

        try:
            for bi in range(n_batch):
                for i in range(NT):
                    if i < 2:
                        src = dr["ctx"][bi, i * 128:(i + 1) * 128, :]
                    else:
                        src = dr["x"][bi, (i - 2) * 128:(i - 1) * 128, :]
                    dma("sp", X[:, i, :], src, [], [Xb[i]])

                for l in layers:
                    first = (bi == 0 and l == layers[0])
                    last = (l == DEPTH - 1)
                    moe_groups = [(256 + g * 512, 512) for g in range(4)] if last else GROUPS

                    def dmp(name, src, reads):
                        if first:
                            dump(name, src, reads)

                    def norm_to_HT(gT, vshift, vscale, ph, router=None):
                        junk = ph.take([D], BF16); b_junk = pbuf("junk")
                        xs = [ph.take([D], F32) for _ in range(2)]
                        b_xs = [pbuf("xs%d" % i) for i in range(2)]
                        for r_i, row in enumerate((bi, 2)):
                            for c in range(8):
                                ts("dve", AB[:, r_i, 0, c:c + 1], modcol(l, vscale, c, row), 1.0, ALU.add,
                                   [b_modT, b_ng], [b_AB], s2=gT[:, l, c:c + 1], op1=ALU.mult)
                                cp("dve", AB[:, r_i, 1, c:c + 1], modcol(l, vshift, c, row), [b_modT], [b_AB])
                        for i in range(NT):
                            act(junk, X[:, i, :], AF.Square, [Xb[i]], [b_junk, b_ssq], accum=ssq[:, i:i + 1])
                        act(rstd, ssq, AF.Sqrt, [b_ssq, b_ccol], [b_rstd], bias=eps_col, scale=1.0 / D)
                        recip(rstd, rstd, [b_rstd], [b_rstd])
                        def mk_xs(i):
                            ts("dve", xs[i % 2], X[:, i, :], rstd[:, i:i + 1], ALU.mult, [Xb[i], b_rstd], [b_xs[i % 2]])

                        mk_xs(0)
                        for i in range(NT):
                            s = i % 2
                            r_i = 1 if i < 2 else 0
                            pb = (i % 2) * 2
                            for c in range(8):
                                bk = pb + c // 4
                                tr(PS[bk][:, (c % 4) * 128:(c % 4 + 1) * 128], xs[s][:, c * 128:(c + 1) * 128], ident,
                                   [b_xs[s], b_ident], [PSb[bk]])
                            if i + 1 < NT:
                                mk_xs(i + 1)
                            if router is not None:
                                router(i, pb, r_i)
                                continue
                            for c in range(8):
                                bk = pb + c // 4
                                src_ = PS[bk][:, (c % 4) * 128:(c % 4 + 1) * 128]
                                dst_ = HT[:, c, i * 128:(i + 1) * 128]
                                wr_, pw_ = ([HTb[i]], ()) if c == 0 else ([], [HTb[i]])
                                if c < 4:
                                    act(dst_, src_, AF.Identity, [PSb[bk], b_AB], wr_,
                                        bias=AB[:, r_i, 1, c:c + 1], scale=AB[:, r_i, 0, c:c + 1], pw=pw_)
                                else:
                                    ts("dve", dst_, src_, AB[:, r_i, 0, c:c + 1], ALU.mult, [PSb[bk], b_AB], wr_,
                                       s2=AB[:, r_i, 1, c:c + 1], op1=ALU.add, pw=pw_)

                    def make_gb(vec):
                        for r_i, row in enumerate((bi, 2)):
                            for c in range(8):
                                ts("dve", diag, ident, modcol(l, vec, c, row), ALU.mult, [b_ident, b_modT], [b_diag])
                                mm(PS[7][:, 0:128], ones_f, diag, True, True, [b_ones, b_diag], [PSb[7]])
                                cp("act", GB[:, r_i, c * 128:(c + 1) * 128], PS[7][:, 0:128], [PSb[7]], [b_GB])

                    def wout_partial(srcT, b_src, nk, wo, b_wo, tmp, b_tmp):
                        for i in range(2 if last else 0, NT):
                            r_i = 1 if i < 2 else 0
                            for half in range(2):
                                bk = 4 + (i * 2 + half) % 4
                                for kk in range(nk):
                                    mm(PS[bk], srcT[:, kk, i * 128:(i + 1) * 128], wo[:, kk, half * 512:(half + 1) * 512],
                                       kk == 0, kk == nk - 1, [b_src, b_wo], [PSb[bk]])
                                s = (i * 2 + half) % 2
                                tt("dve", tmp[s], PS[bk], GB[:, r_i, half * 512:(half + 1) * 512], ALU.mult,
                                   [PSb[bk], b_GB], [b_tmp[s]])
                                tt("pool" if i % 2 == 0 else "dve", X[:, i, half * 512:(half + 1) * 512],
                                   X[:, i, half * 512:(half + 1) * 512], tmp[s], ALU.add, [b_tmp[s], Xb[i]], [Xb[i]])

                    def proj_fm(dstT, b_dst, wchunk, b_w, ncol_tiles):
                        n = 0
                        for (t0, tn) in GROUPS:
                            tiles = list(range(t0 // 128, (t0 + tn) // 128))
                            for ct in range(ncol_tiles):
                                bk = n % 4
                                n += 1
                                for k in range(8):
                                    mm(PS[bk][:, 0:tn], wchunk[:, k, ct * 128:(ct + 1) * 128], HT[:, k, t0:t0 + tn], k == 0, k == 7,
                                       [b_w] + [HTb[i] for i in tiles], [PSb[bk]])
                                cp("act", dstT[:, ct, t0:t0 + tn], PS[bk][:, 0:tn], [PSb[bk]], [b_dst])

                    ph = new_phase()
                    norm_to_HT(n1g, 0, 1, ph)
                    make_gb(2)
                    dmp("ht1", HT, HTb)
                    if stop_after == "norm1":
                        raise Stop()

                    ph = new_phase()
                    win = ph.take([8, 256], BF16); b_win = pbuf("win")
                    uT = ph.take([2, T], BF16); b_uT = pbuf("uT")
                    dma("pool", win, wview(dr["w_in"][l])[:, :, 1536:1792], [], [b_win])
                    proj_fm(uT, b_uT, win, b_win, 2)
                    dmp("uT", uT, [b_uT])
                    y1T = ph.take([2, T], BF16); b_y1T = pbuf("y1T")
                    wglu = ph.take([2, 256], BF16); b_wglu = pbuf("wglu")
                    dma("pool", wglu, wview(dr["w_glu"][l]), [], [b_wglu])
                    bglu = ph.take([2], F32); dsk = ph.take([2], F32); b_sp = pbuf("s5small")
                    dma("sp", bglu, dr["b_gluT"][l], [], [b_sp])
                    dma("sp", dsk, dr["s5_dT"][l], [], [b_sp])
                    wo_s = ph.take([2, D], BF16); b_wo_s = pbuf("wo_s")
                    dma("pool", wo_s, wview(dr["w_out"][l])[:, 4:6, :], [], [b_wo_s])
                    bz_off = ph.off
                    bzr = ph.take([8, 128], BF16); bzi = ph.take([8, 128], BF16); b_bz = pbuf("bz")
                    czr = ph.take([8, 128], BF16); czi = ph.take([8, 128], BF16); b_cz = pbuf("cz")
                    bzcz_flat = view(bz_off, [4096], BF16)[0]
                    assert ph.off == bz_off + 4 * 2048
                    sm = ph.take([3, 8], F32); b_sm = pbuf("sm")
                    smv = ph.take([40, 8], F32); b_smv = pbuf("smv")
                    dgd = ph.take([2, 128], BF16); b_dgd = pbuf("dgd")
                    carry = ph.take([2, 8], F32); b_carry = pbuf("carry")
                    ytmp = ph.take([2, 128], F32); b_ytmp = pbuf("ytmp")
                    SC = [ph.take([8, 128], F32) for _ in range(11)]
                    b_SC = [pbuf("sc%d" % i) for i in range(11)]
                    def arena_pair(v0):
                        off = v0.offset - arena_t[:, :].offset
                        return arena_t[:, off:off + 2048].rearrange("p (t a b) -> p t a b", t=2, a=8)

                    def halves(v):
                        f = flat(v).bitcast(BF16)
                        return [f[:, 0:1024].rearrange("p (a b) -> p a b", a=8), f[:, 1024:2048].rearrange("p (a b) -> p a b", a=8)]
                    GR, GI, RT0, ZR, ZI = SC[0:5]
                    b_GR, b_GI, b_RT0, b_ZR, b_ZI = b_SC[0:5]
                    BUrb, BUib = halves(SC[5]); b_BUb = b_SC[5]
                    COSb, SINb = halves(SC[6]); b_tabb = b_SC[6]
                    p1, p2 = halves(SC[7]); b_pp = b_SC[7]
                    grb, gib = halves(SC[8]); b_gb = b_SC[8]
                    HRb, HIb = halves(SC[9]); b_Hb = b_SC[9]
                    p3, p4 = halves(SC[10]); b_pq = b_SC[10]
                    sig = [flat(ZR)[:, 0:512], flat(ZR)[:, 512:1024]]; b_sig = b_ZR
                    wtmp = [flat(ZI)[:, 0:512], flat(ZI)[:, 512:1024]]; b_wt5 = b_ZI
                    c127 = ph.take([2, 8], F32); b_c127 = pbuf("c127")

                    def sincos(o_sin, o_cos, th, t1, t2, t3, R_, W_):
                        RW = R_ + W_
                        t1i = t1.bitcast(I32)
                        for (o_, shift) in ((o_sin, 0.0), (o_cos, 0.5 * PI)):
                            if shift == 0.0:
                                src = th
                            else:
                                ts("dve", t3, th, shift, ALU.add, RW, W_)
                                src = t3
                            ts("dve", t1i, src, 1.0 / (2 * PI), ALU.mult, RW, W_)
                            cp("dve", t2, t1i, W_, W_)
                            stt("dve", t3, t2, -2 * PI, src, ALU.mult, ALU.add, RW, W_)
                            ts("dve", t3, t3, -PI, ALU.max, W_, W_, s2=PI, op1=ALU.min)
                            act(o_, t3, AF.Sin, W_, W_)

                    def coef_math(a, b, ls, tmp, R_, W_, extra=None):
                        dlt, rr, th, sn, cs, t1, t2, t3 = tmp
                        RW = R_ + W_
                        act(dlt, ls, AF.Exp, R_, W_)
                        tt("dve", rr, dlt, a, ALU.mult, RW, W_)
                        if extra is not None:
                            act(extra[0], rr, AF.Exp, W_, W_, scale=128.0)
                        act(rr, rr, AF.Exp, W_, W_)
                        tt("dve", th, dlt, b, ALU.mult, RW, W_)
                        if extra is not None:
                            ts("dve", extra[1], th, 128.0, ALU.mult, W_, W_)
                        sincos(sn, cs, th, t1, t2, t3, W_, W_)
                        tt("dve", cs, cs, rr, ALU.mult, W_, W_)
                        tt("dve", sn, sn, rr, ALU.mult, W_, W_)
                        tt("dve", t1, a, a, ALU.mult, RW, W_)
                        tt("dve", t2, b, b, ALU.mult, RW, W_)
                        tt("dve", t1, t1, t2, ALU.add, W_, W_)
                        recip(t1, t1, W_, W_)
                        ts("dve", t2, cs, -1.0, ALU.add, W_, W_)
                        tt("dve", t3, t2, a, ALU.mult, RW, W_)
                        tt("dve", dlt, sn, b, ALU.mult, RW, W_)
                        tt("dve", t3, t3, dlt, ALU.add, W_, W_)
                        tt("dve", t3, t3, t1, ALU.mult, W_, W_)
                        tt("dve", dlt, sn, a, ALU.mult, RW, W_)
                        tt("dve", t2, t2, b, ALU.mult, RW, W_)
                        tt("dve", dlt, dlt, t2, ALU.subtract, W_, W_)
                        tt("dve", dlt, dlt, t1, ALU.mult, W_, W_)
                        return dict(theta=th, r=rr, lbr=cs, lbi=sn, cr=t3, ci=dlt)

                    for ct in range(2):
                        ts("dve", dgd[:, ct, :], ident, dsk[:, ct:ct + 1], ALU.mult, [b_ident, b_sp], [b_dgd])
                    for d in (1, 0):
                        rev = (d == 1)
                        allb = list(b_SC)
                        W_ = [b_smv]
                        KA = smv[:, 16:18, :]; KB = smv[:, 18:20, :]; fa = smv[:, 20:22, :]; fb = smv[:, 22:24, :]
                        sc_ = s5_scratch(l, d)
                        tab_flat = flat(SC[6]).bitcast(BF16)
                        kab_flat = smv[:, 16:20, :].rearrange('p a b -> p (a b)')
                        if bi == 0:
                            for q_, slot in enumerate((0, 1, 2)):
                                dma("sp", flat(SC[slot]), dr["s5_bc"][l, d, q_].partition_broadcast(128), [], allb)
                            o = coef_math(flat(SC[0]), flat(SC[1]), flat(SC[2]), [flat(SC[i]) for i in range(3, 11)], [], allb)
                            zre, zim, w1, w2 = flat(SC[0]), flat(SC[1]), flat(SC[2]), flat(SC[4])
                            dma("sp", zre, dr["s5_bz"][l, d, 0].rearrange("p a b -> p (a b)"), [], allb)
                            dma("sp", zim, dr["s5_bz"][l, d, 1].rearrange("p a b -> p (a b)"), [], allb)
                            cr, ci = o["cr"], o["ci"]
                            tt("dve", w1, zre, cr, ALU.mult, allb, allb)
                            tt("dve", w2, zim, ci, ALU.mult, allb, allb)
                            tt("dve", flat(bzr), w1, w2, ALU.subtract, allb, [b_bz])
                            tt("dve", w1, zre, ci, ALU.mult, allb, allb)
                            tt("dve", w2, zim, cr, ALU.mult, allb, allb)
                            tt("dve", flat(bzi), w1, w2, ALU.add, allb, [b_bz])
                            dma("pool", czr, dr["s5_cz"][l, d, 0], [], [b_cz])
                            dma("pool", czi, dr["s5_cz"][l, d, 1], [], [b_cz])
                            ts("dve", flat(czi), flat(czi), -1.0, ALU.mult, [b_cz], [b_cz])
                            dma("sp", sm, dr["s5_sm"][l, d], [], [b_sm])
                            W_ = [b_smv]
                            r128, th128 = smv[:, 8, :], smv[:, 9, :]
                            so = coef_math(sm[:, 0, :], sm[:, 1, :], sm[:, 2, :], [smv[:, i, :] for i in range(8)], [b_sm], W_,
                                           extra=(r128, th128))
                            s128, c128 = smv[:, 10, :], smv[:, 11, :]
                            sincos(s128, c128, th128, smv[:, 12, :], smv[:, 13, :], smv[:, 14, :], W_, W_)
                            KA = smv[:, 16:18, :]; KB = smv[:, 18:20, :]; fa = smv[:, 20:22, :]; fb = smv[:, 22:24, :]
                            tt("dve", KA[:, 0, :], so["r"], c128, ALU.mult, W_, W_)
                            cp("dve", KA[:, 1, :], KA[:, 0, :], W_, W_)
                            tt("dve", KB[:, 1, :], so["r"], s128, ALU.mult, W_, W_)
                            ts("dve", KB[:, 0, :], KB[:, 1, :], -1.0, ALU.mult, W_, W_)
                            ANG = SC[3]
                            for st in range(8):
                                ts("dve", ANG[:, st, :], iota, so["theta"][:, st:st + 1], ALU.mult, [b_iota, b_smv] + allb, allb)
                                ts("dve", RT0[:, st, :], iota, 1.0, ALU.min, [b_iota, b_smv] + allb, allb, s2=so["r"][:, st:st + 1], op1=ALU.mult)
                            sincos(flat(SC[4]), flat(SC[7]), flat(ANG), flat(SC[0]), flat(SC[1]), flat(SC[8]), allb, allb)
                            cp("act", SINb, SC[4], allb, allb)
                            cp("act", COSb, SC[7], allb, allb)
                            dma('sp', sc_['A'][:, 0:4096], bzcz_flat, [b_bz, b_cz], [sc_['bA1']])
                            dma('sp', sc_['A'][:, 4096:6144], tab_flat, [b_tabb], [sc_['bA2']])
                            dma('sp', sc_['B'][:, 0:1024], flat(RT0), [b_RT0], [sc_['bB1']])
                            dma('sp', sc_['B'][:, 1024:1056], kab_flat, [b_smv], [sc_['bB2']])
                        else:
                            dma('sp', bzcz_flat, sc_['A'][:, 0:4096], [sc_['bA1']], [b_bz, b_cz])
                            dma('sp', tab_flat, sc_['A'][:, 4096:6144], [sc_['bA2']], [b_tabb])
                            dma('sp', flat(RT0), sc_['B'][:, 0:1024], [sc_['bB1']], [b_RT0])
                            dma('sp', kab_flat, sc_['B'][:, 1024:1056], [sc_['bB2']], [b_smv])
                        G127 = arena_pair(SC[0])[:, :, :, 127]
                        G127s = arena_pair(SC[0])[:, ::-1, :, 127]
                        Z0 = arena_pair(SC[3])[:, :, :, 0]
                        b_G2 = [b_GR, b_GI]; b_Z2 = [b_ZR, b_ZI]

                        order = ([1, 0] + list(range(17, 1, -1))) if rev else list(range(18))

                        def tokslice(k):
                            t0 = k * 128
                            if rev:
                                return slice(t0 + 127, (t0 - 1) if t0 > 0 else None, -1)
                            return slice(t0, t0 + 128)

                        def bu_stage(k):
                            tok = tokslice(k)
                            for st in range(8):
                                ct = st // 4
                                for ri, bz_ in enumerate((bzr, bzi)):
                                    bk = ri * 2 + st // 4
                                    mm(PS[bk][:, (st % 4) * 128:(st % 4 + 1) * 128], bz_[:, st, :], uT[:, ct, tok], True, True,
                                       [b_bz, b_uT], [PSb[bk]])
                            for h in range(2):
                                sl = slice(4 * h, 4 * h + 4)
                                cp("act", BUrb[:, sl, :], PS[h][:, :].rearrange("p (a b) -> p a b", a=4), [PSb[h]], [b_BUb])
                                cp("act", BUib[:, sl, :], PS[2 + h][:, :].rearrange("p (a b) -> p a b", a=4), [PSb[2 + h]], [b_BUb])

                        def y_stage(n_c, k):
                            t0 = k * 128
                            for ct in range(2):
                                bk = 4 + 2 * (n_c % 2) + ct
                                if d == 1:
                                    cp("act", y1T[:, ct, t0:t0 + 128], PS[bk][:, 0:128], [PSb[bk]], [b_y1T])
                                else:
                                    tt("dve", ytmp[:, ct, :], PS[bk][:, 0:128], y1T[:, ct, t0:t0 + 128], ALU.add, [PSb[bk], b_y1T], [b_ytmp])
                                    act(y1T[:, ct, t0:t0 + 128], ytmp[:, ct, :], AF.Gelu, [b_ytmp], [b_y1T])

                        def fwd_stage():
                            tt("dve", flat(p1), flat(BUrb), flat(COSb), ALU.mult, [b_BUb, b_tabb], [b_pp])
                            tt("dve", flat(p2), flat(BUib), flat(SINb), ALU.mult, [b_BUb, b_tabb], [b_pp])
                            tt("dve", flat(ZR), flat(p1), flat(p2), ALU.add, [b_pp], [b_ZR])
                            tt("dve", flat(p3), flat(BUib), flat(COSb), ALU.mult, [b_BUb, b_tabb], [b_pq])
                            tt("dve", flat(p4), flat(BUrb), flat(SINb), ALU.mult, [b_BUb, b_tabb], [b_pq])
                            tt("dve", flat(ZI), flat(p3), flat(p4), ALU.subtract, [b_pq], [b_ZI])

                        NC = len(order)
                        bu_stage(order[0])
                        fwd_stage()
                        if NC > 1:
                            bu_stage(order[1])
                        for n_c, k in enumerate(order):
                            if n_c > 0:
                                tt("dve", fa, G127, KA, ALU.mult, b_G2 + W_, W_)
                                tt("dve", fb, G127s, KB, ALU.mult, b_G2 + W_, W_)
                                tt("dve", fa, fa, fb, ALU.add, W_, W_)
                                tt("dve", Z0, Z0, fa, ALU.add, b_Z2 + W_, b_Z2)
                            P.op("dve", lambda e: e.tensor_tensor_scan(out=flat(GR), data0=flat(RT0), data1=flat(ZR), initial=0.0,
                                                                       op0=ALU.mult, op1=ALU.add), [b_RT0, b_ZR], [b_GR])
                            P.op("dve", lambda e: e.tensor_tensor_scan(out=flat(GI), data0=flat(RT0), data1=flat(ZI), initial=0.0,
                                                                       op0=ALU.mult, op1=ALU.add), [b_RT0, b_ZI], [b_GI])
                            cp("act", grb, GR, [b_GR], [b_gb])
                            cp("act", gib, GI, [b_GI], [b_gb])
                            if n_c + 1 < NC:
                                fwd_stage()
                                if n_c + 2 < NC:
                                    bu_stage(order[n_c + 2])
                            tt("dve", flat(p1), flat(grb), flat(COSb), ALU.mult, [b_gb, b_tabb], [b_pp])
                            tt("dve", flat(p2), flat(gib), flat(SINb), ALU.mult, [b_gb, b_tabb], [b_pp])
                            tt("dve", flat(HRb), flat(p1), flat(p2), ALU.subtract, [b_pp], [b_Hb])
                            tt("dve", flat(p3), flat(grb), flat(SINb), ALU.mult, [b_gb, b_tabb], [b_pq])
                            tt("dve", flat(p4), flat(gib), flat(COSb), ALU.mult, [b_gb, b_tabb], [b_pq])
                            tt("dve", flat(HIb), flat(p3), flat(p4), ALU.add, [b_pq], [b_Hb])
                            for ct in range(2):
                                bk = 4 + 2 * (n_c % 2) + ct
                                n = 0
                                for st in range(4 * ct, 4 * ct + 4):
                                    for (cz_, hh_) in ((czr, HRb), (czi, HIb)):
                                        rhs = hh_[:, st, ::-1] if rev else hh_[:, st, :]
                                        mm(PS[bk][:, 0:128], cz_[:, st, :], rhs, n == 0, (n == 7 and d == 1), [b_cz, b_Hb], [PSb[bk]])
                                        n += 1
                                if d == 0:
                                    mm(PS[bk][:, 0:128], dgd[:, ct, :], uT[:, ct, k * 128:(k + 1) * 128], False, True, [b_dgd, b_uT], [PSb[bk]])
                            if n_c > 0:
                                y_stage(n_c - 1, order[n_c - 1])
                        y_stage(len(order) - 1, order[-1])
                    dmp("gT", y1T, [b_y1T])
                    n = 0
                    for (t0, tn) in GROUPS:
                        for co in range(2):
                            bk = n % 4
                            s = n % 2
                            n += 1
                            for k in range(2):
                                mm(PS[bk][:, 0:tn], wglu[:, k, co * 128:(co + 1) * 128], y1T[:, k, t0:t0 + tn], k == 0, k == 1,
                                   [b_wglu, b_y1T], [PSb[bk]])
                            act(sig[s][:, 0:tn], PS[bk][:, 0:tn], AF.Sigmoid, [PSb[bk], b_sp], [b_sig], bias=bglu[:, co:co + 1])
                            tt("dve", uT[:, co, t0:t0 + tn], sig[s][:, 0:tn], y1T[:, co, t0:t0 + tn], ALU.mult, [b_sig, b_y1T], [b_uT])
                    dmp("ssmT", uT, [b_uT])
                    wout_partial(uT, b_uT, 2, wo_s, b_wo_s, wtmp, [b_wt5, b_wt5])
                    if stop_after == "s5":
                        raise Stop()

                    ph = new_phase()
                    win = ph.take([8, 256], BF16); b_win = pbuf("winf")
                    ufT = ph.take([2, T], BF16); b_ufT = pbuf("ufT")
                    dma("pool", win, wview(dr["w_in"][l])[:, :, 1792:2048], [], [b_win])
                    proj_fm(ufT, b_ufT, win, b_win, 2)
                    wo_f = ph.take([2, D], BF16); b_wo_f = pbuf("wo_f")
                    dma("pool", wo_f, wview(dr["w_out"][l])[:, 6:8, :], [], [b_wo_f])
                    wfz = ph.take([2, 256], F32); b_wfz = pbuf("wfz")
                    dma("sp", wfz, dr["w_fz"][l].rearrange("k p n -> p k n"), [], [b_wfz])
                    c64 = ph.take([128], F32); s64 = ph.take([128], F32); b_c64 = pbuf("c64")
                    dma("sp", c64, dr["c64blk"][:, :], [], [b_c64])
                    dma("sp", s64, dr["s64blk"][:, :], [], [b_c64])
                    G = ph.take([2, 512], BF16); b_G = pbuf("G")
                    for kt in range(2):
                        for ti, tb in enumerate((c64, s64)):
                            mm(PS[ti][:, 0:256], tb, wfz[:, kt, :], True, True, [b_c64, b_wfz], [PSb[ti]])
                            cp("act", G[:, kt, ti * 256:(ti + 1) * 256], PS[ti][:, 0:256], [PSb[ti]], [b_G])
                    A_tok = ph.take([NT, 512], BF16); b_A = pbuf("A_tok")
                    for i in range(NT):
                        bk = i % 4
                        for kt in range(2):
                            mm(PS[bk], ufT[:, kt, i * 128:(i + 1) * 128], G[:, kt, :], kt == 0, kt == 1, [b_ufT, b_G], [PSb[bk]])
                        if i == 0:
                            cp("act", A_tok[:, i, :], PS[bk], [PSb[bk]], [b_A])
                        else:
                            cp("act" if i % 2 == 0 else "dve", A_tok[:, i, :], PS[bk], [PSb[bk]], [], pw=[b_A])
                    ring = [ph.take([4, 512], BF16) for _ in range(4)]
                    b_ring = [pbuf("ring%d" % i) for i in range(4)]
                    ctab = [ph.take([2, 256], BF16) for _ in range(2)]; b_ctab = pbuf("ctab")
                    wtmp = [ph.take([512], F32) for _ in range(2)]; b_wtmp = [pbuf("wtmpf%d" % i) for i in range(2)]
                    fouT = ufT
                    b_fouT = pbuf("fouT", extra_old=[b_ufT])
                    tabs = (dr["dftc"], dr["dftns"])
                    nring = 0
                    for lg in range(4):
                        nmm = 0
                        for ktg in range(4):
                            for ti in range(2):
                                s = nring % 4
                                nring += 1
                                src = tabs[ti].rearrange("(k p) n -> p k n", p=128)[:, ktg * 4:(ktg + 1) * 4, lg * 512:(lg + 1) * 512]
                                dma("sp", ring[s], src, [], [b_ring[s]])
                                for kk in range(4):
                                    kt = ktg * 4 + kk
                                    for ct in range(2):
                                        mm(PS[4 + ct], A_tok[:, 2 + kt, ti * 256 + ct * 128: ti * 256 + (ct + 1) * 128], ring[s][:, kk, :],
                                           nmm < 2, nmm >= 62, [b_A, b_ring[s]], [PSb[4 + ct]])
                                        nmm += 1
                        for ct in range(2):
                            cp("act", fouT[:, ct, 256 + lg * 512: 256 + (lg + 1) * 512], PS[4 + ct], [PSb[4 + ct], b_A], [b_fouT])
                    dma("sp", ctab[0], wview(dr["dftc_c"]), [], [b_ctab])
                    dma("sp", ctab[1], wview(dr["dftns_c"]), [], [b_ctab])
                    for ct in range(2):
                        n = 0
                        for ti in range(2):
                            for kt in range(2):
                                mm(PS[ct][:, 0:256], A_tok[:, kt, ti * 256 + ct * 128: ti * 256 + (ct + 1) * 128], ctab[ti][:, kt, :],
                                   n == 0, n == 3, [b_A, b_ctab], [PSb[ct]])
                                n += 1
                        cp("act", fouT[:, ct, 0:256], PS[ct][:, 0:256], [PSb[ct], b_A], [b_fouT])
                    dmp("fouT", fouT, [b_fouT])
                    wout_partial(fouT, b_fouT, 2, wo_f, b_wo_f, wtmp, b_wtmp)
                    if stop_after == "fnet":
                        raise Stop()

                    ph = new_phase()
                    wq = [ph.take([8, 512], BF16) for _ in range(2)]
                    b_wq = [pbuf("wq%d" % i) for i in range(2)]
                    qT = ph.take([4, T], BF16); kT = ph.take([4, T], BF16)
                    b_qT = pbuf("qT"); b_kT = pbuf("kT")
                    V = ph.take([NT, 8 * 65], BF16); b_V = pbuf("V")
                    memset("pool", V.rearrange("p t (h d) -> p (t h) d", d=65)[:, :, 64:65], 1.0, [b_V])
                    sq = ph.take([512], BF16); b_sq = pbuf("sq")
                    qn_off = ph.off
                    qn = [ph.take([512], F32) for _ in range(2)]; b_qn = [pbuf("qn%d" % i) for i in range(2)]
                    tsb_off = ph.off
                    tsb = ph.take([512], F32); b_tsb = pbuf("tsb")
                    qr = [ph.take([512], BF16) for _ in range(2)]; b_qr = [pbuf("qr%d" % i) for i in range(2)]
                    s8 = ph.take([2, 8], F32); b_s8 = [pbuf("s8_%d" % i) for i in range(2)]
                    Gqk = ph.take([2, 64], F32); b_Gqk = pbuf("Gqk")
                    dma("sp", Gqk[:, 0, :], dr["qng"][l].partition_broadcast(128), [], [b_Gqk])
                    dma("sp", Gqk[:, 1, :], dr["kng"][l].partition_broadcast(128), [], [b_Gqk])
                    items = [(ci, i) for ci in range(3) for i in range(NT) if not (last and ci == 0 and i < 2)]
                    MMB = [0, 1, 4, 5]

                    def load_wq(ci):
                        dma("pool", wq[ci % 2], wview(dr["w_in"][l])[:, :, ci * 512:(ci + 1) * 512], [], [b_wq[ci % 2]])

                    def stA(n):
                        ci, i = items[n]
                        s = ci % 2
                        bk = MMB[n % 4]
                        for k in range(8):
                            mm(PS[bk], HT[:, k, i * 128:(i + 1) * 128], wq[s][:, k, :], k == 0, k == 7, [HTb[i], b_wq[s]], [PSb[bk]])
                        if ci == 0 and i == NT - 1:
                            load_wq(2)

                    def stB(n):
                        ci, i = items[n]
                        bk = MMB[n % 4]
                        if ci == 2:
                            cp("act", V[:, i, :].rearrange("p (h d) -> p h d", d=65)[:, :, 0:64],
                               PS[bk].rearrange("p (h d) -> p h d", d=64), [PSb[bk]], [], pw=[b_V])
                            return
                        u = n % 2
                        s8u = s8[:, u, :]
                        act(sq, PS[bk], AF.Square, [PSb[bk]], [b_sq])
                        red(s8u, sq.rearrange("p (h d) -> p h d", h=8), ALU.add, [b_sq], [b_s8[u]])
                        act(s8u, s8u, AF.Sqrt, [b_s8[u], b_ccol], [b_s8[u]], bias=eps_col, scale=1.0 / 64)
                        recip(s8u, s8u, [b_s8[u]], [b_s8[u]])
                        qn3 = qn[u].rearrange("p (h d) -> p h d", h=8)
                        tt("dve", qn3, PS[bk].rearrange("p (h d) -> p h d", h=8), s8u.unsqueeze(2).to_broadcast([128, 8, 64]),
                           ALU.mult, [PSb[bk], b_s8[u]], [b_qn[u]])
                        tt("dve", qn3, qn3, Gqk[:, ci, :].unsqueeze(1).to_broadcast([128, 8, 64]), ALU.mult, [b_qn[u], b_Gqk], [b_qn[u]])
                        qr3 = qr[u].rearrange("p (h d) -> p h d", h=8)
                        if i >= 2:
                            j = i - 2
                            cb = ropec[:, j, :].unsqueeze(1).to_broadcast([128, 16, 32])
                            sb_ = ropes[:, j, :].unsqueeze(1).to_broadcast([128, 16, 32])
                            qn4 = qn[u].rearrange("p (g d) -> p g d", g=16)
                            tt("dve", tsb.rearrange("p (g d) -> p g d", g=16), qn4, sb_, ALU.mult, [b_qn[u], b_rope], [b_tsb])
                            tt("dve", qn4, qn4, cb, ALU.mult, [b_qn[u], b_rope], [b_qn[u]])
                            ts3 = tsb.rearrange("p (h d) -> p h d", h=8)
                            tt("pool", qr3[:, :, 0:32], qn3[:, :, 0:32], ts3[:, :, 32:64], ALU.subtract, [b_qn[u], b_tsb], [b_qr[u]])
                            tt("pool", qr3[:, :, 32:64], ts3[:, :, 0:32], qn3[:, :, 32:64], ALU.add, [b_qn[u], b_tsb], [b_qr[u]])
                        else:
                            cp("pool", qr[u], qn[u], [b_qn[u]], [b_qr[u]])

                    def stC(n):
                        ci, i = items[n]
                        if ci == 2:
                            return
                        u = n % 2
                        tb = 2 + u
                        for hp in range(4):
                            tr(PSH[tb][:, hp * 128:(hp + 1) * 128], qr[u][:, hp * 128:(hp + 1) * 128], identb,
                               [b_qr[u], b_identb], [PSb[tb]])
                        dst = qT if ci == 0 else kT
                        cp("act", dst[:, :, i * 128:(i + 1) * 128], PSH[tb][:, 0:512].rearrange("p (a b) -> p a b", a=4),
                           [PSb[tb]], [b_qT if ci == 0 else b_kT])

                    load_wq(0)
                    load_wq(1)
                    NI = len(items)
                    stA(0)
                    stA(1)
                    for n in range(NI):
                        stB(n)
                        if n + 2 < NI:
                            stA(n + 2)
                        if n >= 1:
                            stC(n - 1)
                    stC(NI - 1)
                    dmp("qT", qT, [b_qT])
                    dmp("kT", kT, [b_kT])
                    hta = Alloc(HT_OFF, HT_END)
                    NBT = 3
                    BT = [hta.take([21, 128], BF16) for _ in range(NBT)]
                    b_BT = [pbuf("BT%d" % i, extra_old=HTb) for i in range(NBT)]
                    attT = hta.take([4, T], BF16); b_attT = pbuf("attT", extra_old=HTb)
                    wo_a = wq[0].rearrange("p k n -> p (k n)").rearrange("p (k n) -> p k n", k=4)
                    b_wo_a = b_wq[0]
                    dma("pool", wo_a, wview(dr["w_out"][l])[:, 0:4, :], [], [b_wo_a])
                    w1flat = wq[1].rearrange("p k n -> p (k n)")
                    PT = [w1flat[:, o_:o_ + 896].rearrange("p (a b) -> p a b", a=7) for o_ in (0, 896, 3072)]
                    b_PT = [pbuf("PT%d" % s, extra_old=[b_wq[1]]) for s in range(3)]
                    Ef = [w1flat[:, 1792:3072].bitcast(F32).rearrange("p (a b) -> p a b", a=5), view(qn_off, [5, 128], F32)[0]]
                    b_E = [pbuf("E0", extra_old=[b_wq[1]]), pbuf("E1", extra_old=b_qn)]
                    rinv = [view(tsb_off + 512 * i, [128], F32)[0] for i in range(2)]
                    b_rinv = [pbuf("rinv%d" % i, extra_old=[b_tsb]) for i in range(2)]
                    its = []
                    for hp in range(4):
                        for n_q, qi in enumerate(list(range(2, NT)) + ([] if last else [0, 1])):
                            for hh in range(2):
                                its.append((2 * hp + hh, qi, n_q))
                    att2 = [ph.take([128], BF16) for _ in range(2)]; b_att2 = [pbuf("att2_%d" % i) for i in range(2)]
                    rinv2 = ph.take([2, 2], F32); b_rinv2 = [pbuf("rinv2_%d" % i) for i in range(2)]

                    def it_info(n):
                        h, qi, n_h = its[n]
                        if qi >= 2:
                            kts, t0b = key_tiles(qi - 2)
                            lat = [2 + k for k in kts]
                        else:
                            lat, t0b = [], 0
                        return h, qi, n_h, lat, t0b

                    def st1(n):
                        h, qi, n_h, lat, t0b = it_info(n)
                        hp, hh = h // 2, h % 2
                        pr = slice(64 * hh, 64 * hh + 64)
                        keys = lat + [0, 1]
                        sb2 = (n % 2) * 2
                        qsl = slice(qi * 128, (qi + 1) * 128)
                        for m, kt_ in enumerate(keys):
                            bk = sb2 + m // 4
                            mm(PS[bk][:, (m % 4) * 128:(m % 4 + 1) * 128], kT[pr, hp, kt_ * 128:(kt_ + 1) * 128], qT[pr, hp, qsl],
                               True, True, [b_kT, b_qT], [PSb[bk]])

                    def st2a(n):
                        h, qi, n_h, lat, t0b = it_info(n)
                        bs = h % NBT
                        nl = len(lat)
                        nk = nl + 2
                        sb2 = (n % 2) * 2
                        pe_ = n % 2
                        pt_ = n % 3
                        if h % 2 == 0 and n_h == 1 and h + 2 < 8:
                            dma("pool", BT[(h + 2) % NBT], dr["bt"][l, h + 2], [], [b_BT[(h + 2) % NBT]])
                        if h % 2 == 0 and n_h == 0 and h >= 2:
                            dma("pool", BT[(h + 1) % NBT], dr["bt"][l, h + 1], [], [b_BT[(h + 1) % NBT]])
                        m = 0
                        while m < nk:
                            bk = sb2 + m // 4
                            if m < nl:
                                m2 = min(nl, (m // 4 + 1) * 4)
                                src = PS[bk][:, (m % 4) * 128:(m % 4) * 128 + (m2 - m) * 128].rearrange("p (a b) -> p a b", b=128)
                                stt("dve", Ef[pe_][:, m:m2, :], src, 0.125, BT[bs][:, t0b + m:t0b + m2, :], ALU.mult, ALU.add,
                                    [PSb[bk], b_BT[bs]], [b_E[pe_]])
                            else:
                                m2 = min(nk, (m // 4 + 1) * 4)
                                src = PS[bk][:, (m % 4) * 128:(m % 4) * 128 + (m2 - m) * 128].rearrange("p (a b) -> p a b", b=128)
                                act(PT[pt_][:, m:m2, :], src, AF.Exp, [PSb[bk], b_E[pe_]], [b_PT[pt_]], scale=0.125)
                            m = m2

                    def st2b(n):
                        h, qi, n_h, lat, t0b = it_info(n)
                        nl = len(lat)
                        pe_ = n % 2
                        pt_ = n % 3
                        m = 0
                        while m < nl:
                            m2 = min(nl, (m // 4 + 1) * 4)
                            act(PT[pt_][:, m:m2, :], Ef[pe_][:, m:m2, :], AF.Exp, [b_E[pe_]], [b_PT[pt_]])
                            m = m2

                    def st3(n):
                        h, qi, n_h, lat, t0b = it_info(n)
                        hh = h % 2
                        keys = lat + [0, 1]
                        nk = len(keys)
                        pi = n // 2
                        ob = 4 + pi % 2
                        ps_ = n % 3
                        for m, kt_ in enumerate(keys):
                            mm(PS[ob][:, hh * 128:hh * 128 + 65], PT[ps_][:, m, :], V[:, kt_, h * 65:(h + 1) * 65], m == 0, m == nk - 1,
                               [b_V, b_PT[ps_]], [PSb[ob]])

                    def fin1(pi):
                        hp, qi = its[2 * pi][0] // 2, its[2 * pi][1]
                        ob = 4 + pi % 2
                        u = pi % 2
                        o3 = PS[ob][:, 0:256].rearrange("p (h d) -> p h d", d=128)
                        recip(rinv2[:, u, :], o3[:, :, 64], [PSb[ob]], [b_rinv2[u]])
                        tt("dve", att2[u].rearrange("p (h d) -> p h d", d=64), o3[:, :, 0:64],
                           rinv2[:, u, :].unsqueeze(2).to_broadcast([128, 2, 64]), ALU.mult, [PSb[ob], b_rinv2[u]], [b_att2[u]])

                    def fin2(pi):
                        hp, qi = its[2 * pi][0] // 2, its[2 * pi][1]
                        u = pi % 2
                        tb = 6 + u
                        tr(PSH[tb][:, 0:128], att2[u], identb, [b_att2[u], b_identb], [PSb[tb]])
                        cp("act", attT[:, hp, qi * 128:(qi + 1) * 128], PSH[tb][:, 0:128], [PSb[tb]], [], pw=[b_attT])

                    memset("pool", attT[:, 0, 0:2], 0.0, [b_attT])
                    dma("pool", BT[0], dr["bt"][l, 0], [], [b_BT[0]])
                    dma("pool", BT[1], dr["bt"][l, 1], [], [b_BT[1]])
                    NIT = len(its)
                    st1(0)
                    st1(1)
                    st2a(0)
                    st1(2)
                    st2a(1)
                    st2b(0)
                    for n in range(NIT):
                        if n + 3 < NIT:
                            st1(n + 3)
                        if n + 1 < NIT:
                            st2b(n + 1)
                        if n + 2 < NIT:
                            st2a(n + 2)
                        st3(n)
                        if n % 2 == 1:
                            fin1(n // 2)
                            if n // 2 >= 1:
                                fin2(n // 2 - 1)
                    fin2(NIT // 2 - 1)
                    dmp("attT", attT, [b_attT])
                    b_wt = [pbuf("wtmpa%d" % i, extra_old=[b_wq[1], b_E[0]] + b_PT) for i in range(2)]
                    wout_partial_h = None
                    wtmp = [w1flat[:, 0:1024].bitcast(F32), w1flat[:, 1024:2048].bitcast(F32)]
                    wout_partial(attT, b_attT, 4, wo_a, b_wo_a, wtmp, b_wt)
                    dmp("xmix", X, Xb)
                    if stop_after == "attn":
                        raise Stop()

                    ph = new_phase()
                    for b_ in HTb:
                        P.alias([b_], [b_attT] + b_BT)
                    wgu = [None, None]; wd = [None, None]; b_wgu = [None, None]; b_wd = [None, None]
                    wgu[0] = ph.take([8, 1024], BF16); wd[0] = ph.take([4, 1024], BF16)
                    b_wgu[0] = pbuf("wgu0"); b_wd[0] = pbuf("wd0")

                    def load_expert(e):
                        s_ = e % 2
                        dma("pool", wgu[s_][:, :, 0:512], wview(dr["w_gate"][l, e]), [], [b_wgu[s_]])
                        dma("pool", wgu[s_][:, :, 512:1024], wview(dr["w_up"][l, e]), [], [b_wgu[s_]])
                        dma("pool", wd[s_], wview(dr["w_down"][l, e]), [], [b_wd[s_]])

                    load_expert(0)
                    sub_base = ph.off
                    wr = ph.take([8, 16], F32); b_wr = pbuf("wr")
                    dma("sp", wr, dr["w_router"][:, :, :], [], [b_wr])
                    h2f = [ph.take([8, 128], F32) for _ in range(2)]
                    b_h2f = [pbuf("h2f%d" % i) for i in range(2)]

                    def router(i, pb, r_i):
                        s = i % 2
                        for c in range(8):
                            bk = pb + c // 4
                            src_ = PS[bk][:, (c % 4) * 128:(c % 4 + 1) * 128]
                            wr_, pw_ = ([b_h2f[s]], ()) if c == 0 else ([], [b_h2f[s]])
                            if c < 4:
                                act(h2f[s][:, c, :], src_, AF.Identity, [PSb[bk], b_AB], wr_,
                                    bias=AB[:, r_i, 1, c:c + 1], scale=AB[:, r_i, 0, c:c + 1], pw=pw_)
                            else:
                                ts("dve", h2f[s][:, c, :], src_, AB[:, r_i, 0, c:c + 1], ALU.mult, [PSb[bk], b_AB], wr_,
                                   s2=AB[:, r_i, 1, c:c + 1], op1=ALU.add, pw=pw_)
                        cp("pool", HT[:, :, i * 128:(i + 1) * 128], h2f[s], [b_h2f[s]], [HTb[i]])
                        for c in range(8):
                            mm(PS[6][:, i * 16:(i + 1) * 16], h2f[s][:, c, :], wr[:, c, :], c == 0, c == 7, [b_h2f[s], b_wr], [PSb[6]])

                    n_before = len(state["cur"])
                    norm_to_HT(n2g, 3, 4, ph, router=router)
                    make_gb(5)
                    dmp("ht2", HT, HTb)
                    state["extra"] = [b_wr] + b_h2f + state["cur"][n_before:]
                    ph = Alloc(sub_base, PH_LIMIT)
                    wgu[1] = ph.take([8, 1024], BF16); wd[1] = ph.take([4, 1024], BF16)
                    b_wgu[1] = pbuf("wgu1"); b_wd[1] = pbuf("wd1")
                    aff = ph.take([NT, 16], F32); sel = ph.take([NT, 16], F32); w_ = ph.take([NT, 16], F32)
                    eq = ph.take([NT, 16], F32)
                    b_rt = pbuf("rt")
                    m1 = ph.take([72], F32); m2 = ph.take([72], F32); gs = ph.take([72], F32); gsel = ph.take([72], F32)
                    gmax = ph.take([NT], F32); wsum = ph.take([NT], F32)
                    brt = ph.take([16], F32); b_brt = pbuf("brt")
                    dma("sp", brt, dr["b_router"].partition_broadcast(128), [], [b_brt])
                    R = [b_rt]
                    f2 = lambda v: v.rearrange("p a b -> p (a b)")
                    v4 = lambda v: f2(v).rearrange("p (g e) -> p g e", e=4)
                    act(f2(aff), PS[6][:, 0:288], AF.Sigmoid, [PSb[6]], R)
                    dmp("aff", aff, R)
                    tt("dve", sel, aff, brt.unsqueeze(1).to_broadcast([128, NT, 16]), ALU.add, R + [b_brt], R)
                    red(m1, v4(sel), ALU.max, R, R)
                    tt("dve", v4(eq), v4(sel), m1.unsqueeze(2).to_broadcast([128, 72, 4]), ALU.is_equal, R, R)
                    stt("dve", v4(eq), v4(eq), -1.0e9, v4(sel), ALU.mult, ALU.add, R, R)
                    red(m2, v4(eq), ALU.max, R, R)
                    tt("dve", gs, m1, m2, ALU.add, R, R)
                    gs3 = gs.rearrange("p (t g) -> p t g", g=4)
                    red(gmax, gs3, ALU.max, R, R)
                    tt("dve", gsel.rearrange("p (t g) -> p t g", g=4), gs3, gmax.unsqueeze(2).to_broadcast([128, NT, 4]), ALU.is_equal, R, R)
                    tt("dve", v4(eq), v4(sel), m2.unsqueeze(2).to_broadcast([128, 72, 4]), ALU.is_ge, R, R)
                    tt("dve", v4(eq), v4(eq), gsel.unsqueeze(2).to_broadcast([128, 72, 4]), ALU.mult, R, R)
                    tt("dve", w_, aff, eq, ALU.mult, R, R)
                    red(wsum, w_, ALU.add, R, R)
                    recip(wsum, wsum, R, R)
                    tt("dve", w_, w_, wsum.unsqueeze(2).to_broadcast([128, NT, 16]), ALU.mult, R, R)
                    dmp("gates", w_, R)
                    gatesT = ph.take([T], BF16); b_gT = pbuf("gatesT")
                    for i in range(NT):
                        bk = (i // 4) % 2
                        tr(PS[bk][0:16, (i % 4) * 128:(i % 4 + 1) * 128], w_[:, i, :], ident, R + [b_ident], [PSb[bk]])
                        if i % 4 == 3 or i == NT - 1:
                            i0 = (i // 4) * 4
                            n_ = i - i0 + 1
                            cp("act", gatesT[0:16, i0 * 128:(i0 + n_) * 128], PS[bk][0:16, 0:n_ * 128], [PSb[bk]], [b_gT])
                    selc = ph.take([16 * 128], BF16); b_selc = pbuf("selc")
                    dma("pool", selc[0:16, :], dr["sel"][:, :], [], [b_selc])
                    load_expert(1)
                    h1T = [ph.take([4, 512], BF16) for _ in range(2)]; b_h1T = [pbuf("h1T%d" % i) for i in range(2)]
                    sil = [ph.take([512], F32) for _ in range(2)]; b_sil = [pbuf("sil%d" % i) for i in range(2)]
                    hmul = [ph.take([512], BF16) for _ in range(2)]; b_hmul = [pbuf("hmul%d" % i) for i in range(2)]
                    GBC = [ph.take([512], BF16) for _ in range(2)]; b_GBC = [pbuf("GBC%d" % i) for i in range(2)]
                    wtmp = [ph.take([512], F32) for _ in range(2)]; b_wtmp = [pbuf("wtmpm%d" % i) for i in range(2)]
                    cnt = dict(F=0, D=0)

                    def gateup(e, gi):
                        s = e % 2
                        t0, tn = moe_groups[gi]
                        tiles = list(range(t0 // 128, (t0 + tn) // 128))
                        hb = [HTb[i] for i in tiles]
                        gsl = (e * len(moe_groups) + gi) % 2
                        mm(PS[7][:, 0:tn], selc[0:16, e * 128:(e + 1) * 128], gatesT[0:16, t0:t0 + tn], True, True,
                           [b_selc, b_gT], [PSb[7]])
                        cp("act", GBC[gsl][:, 0:tn], PS[7][:, 0:tn], [PSb[7]], [b_GBC[gsl]])
                        for fc in range(4):
                            fs = cnt["F"] % 2
                            cnt["F"] += 1
                            bg, bu = fs * 2, fs * 2 + 1
                            for k in range(8):
                                mm(PS[bg][:, 0:tn], wgu[s][:, k, fc * 128:(fc + 1) * 128], HT[:, k, t0:t0 + tn], k == 0, k == 7,
                                   [b_wgu[s]] + hb, [PSb[bg]])
                            for k in range(8):
                                mm(PS[bu][:, 0:tn], wgu[s][:, k, 512 + fc * 128:512 + (fc + 1) * 128], HT[:, k, t0:t0 + tn], k == 0, k == 7,
                                   [b_wgu[s]] + hb, [PSb[bu]])
                            act(sil[fs][:, 0:tn], PS[bg][:, 0:tn], AF.Silu, [PSb[bg]], [b_sil[fs]])
                            tt("dve", hmul[fs][:, 0:tn], sil[fs][:, 0:tn], PS[bu][:, 0:tn], ALU.mult, [b_sil[fs], PSb[bu]], [b_hmul[fs]])
                            tt("dve", h1T[gsl][:, fc, 0:tn], hmul[fs][:, 0:tn], GBC[gsl][:, 0:tn], ALU.mult,
                               [b_hmul[fs], b_GBC[gsl]], [b_h1T[gsl]])

                    def down(e, gi):
                        s = e % 2
                        t0, tn = moe_groups[gi]
                        tiles = list(range(t0 // 128, (t0 + tn) // 128))
                        gsl = (e * len(moe_groups) + gi) % 2
                        for ti, i in enumerate(tiles):
                            r_i = 1 if i < 2 else 0
                            for half in range(2):
                                bk = (4, 5, 6)[cnt["D"] % 3]
                                ws = cnt["D"] % 2
                                cnt["D"] += 1
                                for fc in range(4):
                                    mm(PS[bk], h1T[gsl][:, fc, ti * 128:(ti + 1) * 128], wd[s][:, fc, half * 512:(half + 1) * 512],
                                       fc == 0, fc == 3, [b_h1T[gsl], b_wd[s]], [PSb[bk]])
                                tt("dve", wtmp[ws], PS[bk], GB[:, r_i, half * 512:(half + 1) * 512], ALU.mult,
                                   [PSb[bk], b_GB], [b_wtmp[ws]])
                                tt("pool" if i % 2 == 0 else "dve", X[:, i, half * 512:(half + 1) * 512],
                                   X[:, i, half * 512:(half + 1) * 512], wtmp[ws], ALU.add, [b_wtmp[ws], Xb[i]], [Xb[i]])

                    prev = None
                    for e in range(16):
                        for gi in range(len(moe_groups)):
                            gateup(e, gi)
                            if prev is not None:
                                down(*prev)
                            prev = (e, gi)
                            if gi == 0 and 1 <= e < 15:
                                load_expert(e + 1)
                    down(*prev)
                    dmp("xout", X, Xb)
                    if stop_after == "moe":
                        raise Stop()

                for i in range(2, NT):
                    dma("sp", out_d[bi, (i - 2) * 128:(i - 1) * 128, :], X[:, i, :], [Xb[i]], [], semb=Xb[i])
        except Stop:
            pass
        P.emit()
    return nc


_PROG_CACHE = {}


def _shapes(m):
    return {k: tuple(v.shape) for k, v in m.items()}


def kernel(**inputs):
    n_cores = 8
    shared = _prep_shared(inputs)
    x = np.ascontiguousarray(np.asarray(inputs["x"], dtype=np.float32))
    ctx = np.ascontiguousarray(np.asarray(inputs["ctx"], dtype=np.float32))
    c = np.asarray(inputs["c"], dtype=np.float32)
    c_ctx = np.asarray(inputs["c_ctx"], dtype=np.float32)
    in_maps = []
    for core in range(n_cores):
        m = dict(shared)
        b0 = 2 * core
        m["x"] = x[b0:b0 + 2]
        m["ctx"] = ctx[b0:b0 + 2]
        rows = np.stack([c[b0], c[b0 + 1], c_ctx], axis=0)
        m["crowT"] = np.ascontiguousarray(rows.reshape(3, 8, 128).transpose(2, 1, 0))
        in_maps.append(m)
    key = "main"
    if key not in _PROG_CACHE:
        _PROG_CACHE[key] = build_program(_shapes(in_maps[0]))
    nc = _PROG_CACHE[key]
    res = run_bass_kernel_spmd(nc, in_maps, core_ids=list(range(n_cores)))
    out = np.concatenate([np.asarray(r["out"], dtype=np.float32) for r in res.results], axis=0)
    return out
```
